# Optimizing a Trainium2 kernel written in Bass

```python
import jax
import jax.numpy as jnp
from jax import lax
import numpy as np

D_MODEL = 1024
BATCH = 8
SEQ = 8192
DEPTH = 2

GRID_W = 64
CTX_LEN = 256
HEAD_DIM = 64
N_HEADS = D_MODEL // HEAD_DIM
N_KV_HEADS = N_HEADS // 4
Q_PER_KV = N_HEADS // N_KV_HEADS
D_ATTN = N_HEADS * HEAD_DIM
D_KV = N_KV_HEADS * HEAD_DIM
WINDOW = 128
QBLK = 128
ROPE_BASE = 10000.0
ROPE_AXIS_DIM = HEAD_DIM // 2
D_RNN = D_MODEL
RG_BLOCKS = 16
RG_BW = D_RNN // RG_BLOCKS
CONV_W = 4
CONV_LEFT = 2
RG_C = 8.0
D_FOUR = D_MODEL
F_GROUPS = 4
F_GW = D_FOUR // F_GROUPS
N_BRANCH = 3
Q_OFF = 0
K_OFF = Q_OFF + D_ATTN
V_OFF = K_OFF + D_KV
XR_OFF = V_OFF + D_KV
GR_OFF = XR_OFF + D_RNN
XF_OFF = GR_OFF + D_RNN
IN_COLS = XF_OFF + D_FOUR
N_EXPERTS = 32
TOP_K = 4
D_EXPERT = D_MODEL
SWIGLU_LIMIT = 7.0
SWIGLU_ALPHA = 1.702
MOE_BLK = 128
N_MOD = 6
LN_EPS = 1e-5
DEEPNORM_ALPHA = (2 * DEPTH) ** 0.25
DEEPNORM_BETA = (8 * DEPTH) ** -0.25
NEG_INF = -1e30

kernel_name = 'hybrid_fourier_rglru_swa_moe_dit_block'


def layer_norm(x, g, b):
    xf = x.astype(jnp.float32)
    xc = xf - jnp.mean(xf, axis=-1, keepdims=True)
    var = jnp.mean(xc * xc, axis=-1, keepdims=True)
    y = xc * lax.rsqrt(var + LN_EPS) * g.astype(jnp.float32) + b.astype(jnp.float32)
    return y.astype(x.dtype)


def adaln(cond, w_mod, b_mod):
    m = jax.nn.silu(cond) @ w_mod + b_mod
    m = m.reshape(m.shape[:-1] + (N_MOD, D_MODEL))
    return tuple(jnp.expand_dims(m[..., i, :], -2) for i in range(N_MOD))


def modulate(x, shift, scale):
    return x * (1 + scale) + shift


def axial_rope_tables(rows):
    row = jnp.repeat(jnp.arange(rows, dtype=jnp.float32), GRID_W)
    col = jnp.tile(jnp.arange(GRID_W, dtype=jnp.float32), rows)
    n_freq = ROPE_AXIS_DIM // 2
    freqs = ROPE_BASE ** (-jnp.arange(n_freq, dtype=jnp.float32) / n_freq)
    ang_r = (row[:, None] * freqs)[:, None, :]
    ang_c = (col[:, None] * freqs)[:, None, :]
    return (jnp.cos(ang_r), jnp.sin(ang_r), jnp.cos(ang_c), jnp.sin(ang_c))


def rope_rotate(x, cos, sin):
    x1, x2 = jnp.split(x, 2, axis=-1)
    cos = cos.astype(x.dtype)
    sin = sin.astype(x.dtype)
    return jnp.concatenate([x1 * cos - x2 * sin, x2 * cos + x1 * sin], axis=-1)


def apply_axial_rope(x, rope):
    cos_r, sin_r, cos_c, sin_c = rope
    x_row, x_col = jnp.split(x, 2, axis=-1)
    return jnp.concatenate([rope_rotate(x_row, cos_r, sin_r), rope_rotate(x_col, cos_c, sin_c)], axis=-1)


def window_attention(q, k, v, kc, vc, sink):
    bsz, seq_len = q.shape[:2]
    nb = seq_len // QBLK
    span = QBLK + 2 * WINDOW
    scale = HEAD_DIM ** -0.5
    qb = (q * scale).reshape(bsz, nb, QBLK, N_KV_HEADS, Q_PER_KV, HEAD_DIM).swapaxes(0, 1)
    pad = ((0, 0), (WINDOW, WINDOW), (0, 0), (0, 0))
    kp = jnp.pad(k, pad)
    vp = jnp.pad(v, pad)
    sink_logit = sink.astype(jnp.float32).reshape(1, N_KV_HEADS, Q_PER_KV, 1, 1)

    def block(args):
        n, qn = args
        kn = lax.dynamic_slice_in_dim(kp, n * QBLK, span, axis=1)
        vn = lax.dynamic_slice_in_dim(vp, n * QBLK, span, axis=1)
        qpos = n * QBLK + jnp.arange(QBLK)
        kpos = n * QBLK - WINDOW + jnp.arange(span)
        valid = (jnp.abs(qpos[:, None] - kpos[None, :]) <= WINDOW) & (kpos >= 0)[None, :] & (kpos < seq_len)[None, :]
        s_loc = jnp.einsum('bqgrd,bkgd->bgrqk', qn, kn).astype(jnp.float32)
        s_loc = jnp.where(valid, s_loc, NEG_INF)
        s_ctx = jnp.einsum('bqgrd,bkgd->bgrqk', qn, kc).astype(jnp.float32)
        s_sink = jnp.broadcast_to(sink_logit, s_loc.shape[:-1] + (1,))
        prob = jax.nn.softmax(jnp.concatenate([s_loc, s_ctx, s_sink], axis=-1), axis=-1)
        p_loc = prob[..., :span].astype(v.dtype)
        p_ctx = prob[..., span:-1].astype(v.dtype)
        return jnp.einsum('bgrqk,bkgd->bqgrd', p_loc, vn) + jnp.einsum('bgrqk,bkgd->bqgrd', p_ctx, vc)

    out = lax.map(block, (jnp.arange(nb), qb))
    return out.swapaxes(0, 1).reshape(bsz, seq_len, D_ATTN)


def context_attention(qc, kc, vc, sink):
    bsz, ctx_len = qc.shape[:2]
    qg = (qc * HEAD_DIM ** -0.5).reshape(bsz, ctx_len, N_KV_HEADS, Q_PER_KV, HEAD_DIM)
    s = jnp.einsum('bqgrd,bkgd->bgrqk', qg, kc).astype(jnp.float32)
    s_sink = jnp.broadcast_to(sink.astype(jnp.float32).reshape(1, N_KV_HEADS, Q_PER_KV, 1, 1), s.shape[:-1] + (1,))
    prob = jax.nn.softmax(jnp.concatenate([s, s_sink], axis=-1), axis=-1)[..., :-1].astype(vc.dtype)
    return jnp.einsum('bgrqk,bkgd->bqgrd', prob, vc).reshape(bsz, ctx_len, D_ATTN)


def short_conv(x, w, b):
    seq_len = x.shape[1]
    xp = jnp.pad(x, ((0, 0), (CONV_LEFT, CONV_W - 1 - CONV_LEFT), (0, 0)))
    y = b
    for tap in range(CONV_W):
        y = y + xp[:, tap:tap + seq_len] * w[tap]
    return y


def rglru_coeffs(x, w_a, b_a, w_i, b_i, lam):
    bsz, seq_len, _ = x.shape
    xb = x.reshape(bsz, seq_len, RG_BLOCKS, RG_BW)
    r = jax.nn.sigmoid((jnp.einsum('blhi,hij->blhj', xb, w_a).reshape(bsz, seq_len, D_RNN) + b_a).astype(jnp.float32))
    ig = jax.nn.sigmoid((jnp.einsum('blhi,hij->blhj', xb, w_i).reshape(bsz, seq_len, D_RNN) + b_i).astype(jnp.float32))
    log_a = -RG_C * r * jax.nn.softplus(-lam.astype(jnp.float32))
    a = jnp.exp(log_a)
    b = jnp.sqrt(-jnp.expm1(2 * log_a)) * (ig * x.astype(jnp.float32))
    return a, b


def linear_scan(a, b, h0, reverse):
    first = -1 if reverse else 0
    b = b.at[:, first].add(a[:, first] * h0)

    def combine(lhs, rhs):
        a_l, b_l = lhs
        a_r, b_r = rhs
        return a_l * a_r, a_r * b_l + b_r

    _, h = lax.associative_scan(combine, (a, b), reverse=reverse, axis=1)
    return h


def rglru_bidir(x_lat, x_ctx, conv_w, conv_b, w_a, b_a, w_i, b_i, lam):
    xl = short_conv(x_lat, conv_w, conv_b)
    xc = short_conv(x_ctx, conv_w, conv_b)
    h_lat = jnp.zeros(xl.shape, jnp.float32)
    h_ctx = jnp.zeros(xc.shape, jnp.float32)
    for d, rev in ((0, False), (1, True)):
        a_c, b_c = rglru_coeffs(xc, w_a[d], b_a[d], w_i[d], b_i[d], lam[d])
        hc = linear_scan(a_c, b_c, jnp.zeros(b_c[:, 0].shape, jnp.float32), rev)
        h_end = hc[:, 0] if rev else hc[:, -1]
        a_l, b_l = rglru_coeffs(xl, w_a[d], b_a[d], w_i[d], b_i[d], lam[d])
        h_lat = h_lat + linear_scan(a_l, b_l, h_end, rev)
        h_ctx = h_ctx + hc
    return h_lat, h_ctx


def fourier_mix(xf):
    bsz, seq_len, _ = xf.shape
    xg = xf.astype(jnp.float32).reshape(bsz, seq_len, F_GROUPS, F_GW)
    y = jnp.fft.fft2(xg, axes=(1, 3), norm='ortho').real
    return y.reshape(bsz, seq_len, D_FOUR).astype(xf.dtype)


def branch_merge(u, y_attn, y_rg, y_four, w_merge, b_merge):
    g = jax.nn.sigmoid(u @ w_merge + b_merge).reshape(u.shape[:-1] + (N_BRANCH, D_MODEL))
    return g[..., 0, :] * y_attn + g[..., 1, :] * y_rg + g[..., 2, :] * y_four


def clamped_swiglu(h):
    x_glu, x_lin = jnp.split(h, 2, axis=-1)
    x_glu = jnp.minimum(x_glu, SWIGLU_LIMIT)
    x_lin = jnp.clip(x_lin, -SWIGLU_LIMIT, SWIGLU_LIMIT)
    return x_glu * jax.nn.sigmoid(SWIGLU_ALPHA * x_glu) * (x_lin + 1)


def moe_ffn(u, w_router, b_router, w_gu, b_gu, w_dn, b_dn):
    lead = u.shape[:-1]
    xt = u.reshape(-1, D_MODEL)
    n_tok = xt.shape[0]
    logits = (xt @ w_router + b_router).astype(jnp.float32)
    top_logit, top_e = lax.top_k(logits, TOP_K)
    gate = jax.nn.softmax(top_logit, axis=-1)
    n_asg = n_tok * TOP_K
    e_flat = top_e.reshape(n_asg)
    tok_flat = jnp.repeat(jnp.arange(n_tok, dtype=jnp.int32), TOP_K)
    g_flat = gate.reshape(n_asg)
    order = jnp.argsort(e_flat)
    e_s, tok_s, g_s = e_flat[order], tok_flat[order], g_flat[order]
    counts = jnp.bincount(e_flat, length=N_EXPERTS)
    padded = (counts + MOE_BLK - 1) // MOE_BLK * MOE_BLK
    start = jnp.cumsum(counts) - counts
    pend = jnp.cumsum(padded)
    pstart = pend - padded
    dest = pstart[e_s] + jnp.arange(n_asg, dtype=jnp.int32) - start[e_s]
    n_blocks = -(-n_asg // MOE_BLK) + N_EXPERTS
    n_slots = n_blocks * MOE_BLK
    slot_tok = jnp.full((n_slots,), n_tok, jnp.int32).at[dest].set(tok_s)
    slot_gate = jnp.zeros((n_slots,), jnp.float32).at[dest].set(g_s)
    block_e = jnp.minimum(jnp.searchsorted(pend, jnp.arange(n_blocks) * MOE_BLK, side='right'), N_EXPERTS - 1)
    x_pad = jnp.concatenate([xt, jnp.zeros((1, D_MODEL), xt.dtype)], axis=0)

    def expert_block(args):
        tok, e = args
        h = x_pad[tok] @ w_gu[e] + b_gu[e]
        return clamped_swiglu(h) @ w_dn[e] + b_dn[e]

    ys = lax.map(expert_block, (slot_tok.reshape(n_blocks, MOE_BLK), block_e))
    ys = ys.reshape(n_slots, D_MODEL).astype(jnp.float32) * slot_gate[:, None]
    out = jax.ops.segment_sum(ys, slot_tok, num_segments=n_tok + 1)[:n_tok]
    return out.astype(u.dtype).reshape(lead + (D_MODEL,))


def mixer_sublayer(u, u_c, rope, p, last):
    bsz, seq_len, _ = u.shape
    ctx_len = u_c.shape[1]
    w_in = p['w_in']
    z = u @ w_in
    q = apply_axial_rope(z[..., Q_OFF:K_OFF].reshape(bsz, seq_len, N_HEADS, HEAD_DIM), rope)
    k = apply_axial_rope(z[..., K_OFF:V_OFF].reshape(bsz, seq_len, N_KV_HEADS, HEAD_DIM), rope)
    v = z[..., V_OFF:XR_OFF].reshape(bsz, seq_len, N_KV_HEADS, HEAD_DIM)
    xr = z[..., XR_OFF:GR_OFF]
    gr = z[..., GR_OFF:XF_OFF]
    xf = z[..., XF_OFF:IN_COLS]
    kc = (u_c @ w_in[:, K_OFF:V_OFF]).reshape(bsz, ctx_len, N_KV_HEADS, HEAD_DIM)
    vc = (u_c @ w_in[:, V_OFF:XR_OFF]).reshape(bsz, ctx_len, N_KV_HEADS, HEAD_DIM)
    xrc = u_c @ w_in[:, XR_OFF:GR_OFF]
    h_lat, h_ctx = rglru_bidir(xr, xrc, p['conv_w'], p['conv_b'], p['rg_w_a'], p['rg_b_a'], p['rg_w_i'], p['rg_b_i'], p['rg_lambda'])
    y_attn = window_attention(q, k, v, kc, vc, p['attn_sink']) @ p['w_o_attn']
    y_rg = (jax.nn.gelu(gr) * h_lat.astype(gr.dtype)) @ p['w_o_rg']
    y_four = fourier_mix(xf) @ p['w_o_four']
    y = branch_merge(u, y_attn, y_rg, y_four, p['w_merge'], p['b_merge']) @ p['w_out']
    if last:
        return y, None
    qc = (u_c @ w_in[:, Q_OFF:K_OFF]).reshape(bsz, ctx_len, N_HEADS, HEAD_DIM)
    grc = u_c @ w_in[:, GR_OFF:XF_OFF]
    xfc = u_c @ w_in[:, XF_OFF:IN_COLS]
    yc_attn = context_attention(qc, kc, vc, p['attn_sink']) @ p['w_o_attn']
    yc_rg = (jax.nn.gelu(grc) * h_ctx.astype(grc.dtype)) @ p['w_o_rg']
    yc_four = fourier_mix(xfc) @ p['w_o_four']
    y_c = branch_merge(u_c, yc_attn, yc_rg, yc_four, p['w_merge'], p['b_merge']) @ p['w_out']
    return y, y_c


def trunk_layer(x, ctx, c, c_ctx, rope, p, last):
    sh1, sc1, g1, sh2, sc2, g2 = adaln(c, p['w_mod'], p['b_mod'])
    sh1c, sc1c, g1c, sh2c, sc2c, g2c = adaln(c_ctx, p['w_mod'], p['b_mod'])
    y, y_c = mixer_sublayer(modulate(x, sh1, sc1), modulate(ctx, sh1c, sc1c), rope, p, last)
    x = layer_norm(DEEPNORM_ALPHA * x + g1 * y, p['ln1_g'], p['ln1_b'])
    f = moe_ffn(modulate(x, sh2, sc2), p['w_router'], p['b_router'], p['w_gate_up'], p['b_gate_up'], p['w_down'], p['b_down'])
    x = layer_norm(DEEPNORM_ALPHA * x + g2 * f, p['ln2_g'], p['ln2_b'])
    if last:
        return x, None
    ctx = layer_norm(DEEPNORM_ALPHA * ctx + g1c * y_c, p['ln1_g'], p['ln1_b'])
    f_c = moe_ffn(modulate(ctx, sh2c, sc2c), p['w_router'], p['b_router'], p['w_gate_up'], p['b_gate_up'], p['w_down'], p['b_down'])
    ctx = layer_norm(DEEPNORM_ALPHA * ctx + g2c * f_c, p['ln2_g'], p['ln2_b'])
    return x, ctx


def setup_inputs(seed: int = 0) -> dict:
    key = jax.random.key(seed)
    keys = jax.random.split(key, 32)

    def nrm(i, shape, scale):
        return jax.random.normal(keys[i], shape, jnp.float32) * scale

    a_pow_c = jax.random.uniform(keys[15], (DEPTH, 2, D_RNN), jnp.float32, minval=0.9, maxval=0.999)
    a0 = a_pow_c ** (1.0 / RG_C)
    rg_lambda = jnp.log(a0) - jnp.log1p(-a0)
    return {
        'x': nrm(0, (BATCH, SEQ, D_MODEL), 1.0),
        'c': nrm(1, (BATCH, D_MODEL), 1.0),
        'ctx': nrm(2, (BATCH, CTX_LEN, D_MODEL), 1.0),
        'c_ctx': nrm(3, (D_MODEL,), 1.0),
        'w_mod': nrm(4, (DEPTH, D_MODEL, N_MOD * D_MODEL), 0.5 * D_MODEL ** -0.5),
        'b_mod': nrm(5, (DEPTH, N_MOD * D_MODEL), 0.02),
        'w_in': nrm(6, (DEPTH, D_MODEL, IN_COLS), D_MODEL ** -0.5),
        'attn_sink': nrm(7, (DEPTH, N_HEADS), 0.5),
        'w_o_attn': nrm(8, (DEPTH, D_ATTN, D_MODEL), D_ATTN ** -0.5),
        'conv_w': nrm(9, (DEPTH, CONV_W, D_RNN), CONV_W ** -0.5),
        'conv_b': nrm(10, (DEPTH, D_RNN), 0.02),
        'rg_w_a': nrm(11, (DEPTH, 2, RG_BLOCKS, RG_BW, RG_BW), RG_BW ** -0.5),
        'rg_b_a': nrm(12, (DEPTH, 2, D_RNN), 0.02),
        'rg_w_i': nrm(13, (DEPTH, 2, RG_BLOCKS, RG_BW, RG_BW), RG_BW ** -0.5),
        'rg_b_i': nrm(14, (DEPTH, 2, D_RNN), 0.02),
        'rg_lambda': rg_lambda,
        'w_o_rg': nrm(16, (DEPTH, D_RNN, D_MODEL), D_RNN ** -0.5),
        'w_o_four': nrm(17, (DEPTH, D_FOUR, D_MODEL), D_FOUR ** -0.5),
        'w_merge': nrm(18, (DEPTH, D_MODEL, N_BRANCH * D_MODEL), D_MODEL ** -0.5),
        'b_merge': nrm(19, (DEPTH, N_BRANCH * D_MODEL), 0.02),
        'w_out': nrm(20, (DEPTH, D_MODEL, D_MODEL), DEEPNORM_BETA * D_MODEL ** -0.5),
        'ln1_g': 1.0 + nrm(21, (DEPTH, D_MODEL), 0.02),
        'ln1_b': nrm(22, (DEPTH, D_MODEL), 0.02),
        'w_router': nrm(23, (DEPTH, D_MODEL, N_EXPERTS), D_MODEL ** -0.5),
        'b_router': nrm(24, (DEPTH, N_EXPERTS), 0.01),
        'w_gate_up': nrm(25, (DEPTH, N_EXPERTS, D_MODEL, 2 * D_EXPERT), D_MODEL ** -0.5),
        'b_gate_up': nrm(26, (DEPTH, N_EXPERTS, 2 * D_EXPERT), 0.02),
        'w_down': nrm(27, (DEPTH, N_EXPERTS, D_EXPERT, D_MODEL), DEEPNORM_BETA * D_EXPERT ** -0.5),
        'b_down': nrm(28, (DEPTH, N_EXPERTS, D_MODEL), 0.02),
        'ln2_g': 1.0 + nrm(29, (DEPTH, D_MODEL), 0.02),
        'ln2_b': nrm(30, (DEPTH, D_MODEL), 0.02),
    }


def reference(x, c, ctx, c_ctx, w_mod, b_mod, w_in, attn_sink, w_o_attn, conv_w, conv_b, rg_w_a, rg_b_a, rg_w_i, rg_b_i, rg_lambda, w_o_rg, w_o_four, w_merge, b_merge, w_out, ln1_g, ln1_b, w_router, b_router, w_gate_up, b_gate_up, w_down, b_down, ln2_g, ln2_b):
    ROWS = x.shape[1] // GRID_W
    rope = axial_rope_tables(ROWS)
    for l in range(DEPTH):
        p = dict(w_mod=w_mod[l], b_mod=b_mod[l], w_in=w_in[l], attn_sink=attn_sink[l], w_o_attn=w_o_attn[l],
                 conv_w=conv_w[l], conv_b=conv_b[l], rg_w_a=rg_w_a[l], rg_b_a=rg_b_a[l], rg_w_i=rg_w_i[l],
                 rg_b_i=rg_b_i[l], rg_lambda=rg_lambda[l], w_o_rg=w_o_rg[l], w_o_four=w_o_four[l],
                 w_merge=w_merge[l], b_merge=b_merge[l], w_out=w_out[l], ln1_g=ln1_g[l], ln1_b=ln1_b[l],
                 w_router=w_router[l], b_router=b_router[l], w_gate_up=w_gate_up[l], b_gate_up=b_gate_up[l],
                 w_down=w_down[l], b_down=b_down[l], ln2_g=ln2_g[l], ln2_b=ln2_b[l])
        x, ctx = trunk_layer(x, ctx, c, c_ctx, rope, p, l == DEPTH - 1)
    return x
```

```python
import math
import os
from contextlib import ExitStack

import numpy as np
import concourse.bass as bass
import concourse.mybir as mybir
from concourse.bass_utils import run_bass_kernel_spmd

F32 = mybir.dt.float32
BF16 = mybir.dt.bfloat16
ALU = mybir.AluOpType
AF = mybir.ActivationFunctionType
AX = mybir.AxisListType

D = 1024
L = 8192
CT = 256
NT = L + CT
NE = 32
ALPHA = 4.0 ** 0.25
LN_EPS = 1e-5
QO, QPO, KO, KPO, VO, XRO, GRO, XFO, WIN = 0, 1024, 2048, 2304, 2560, 2816, 3840, 4864, 5888
ARENA_BYTES = 212000
N_DMA_SEMS = 84


class Buf:
    _n = 0

    def __init__(self, name):
        Buf._n += 1
        self.id = Buf._n
        self.name = name
        self.whole = [dict(), dict()]
        self.parts = {}
        self.phys = None


def _merge(dst, src):
    for k, v in src.items():
        if dst.get(k, 0) < v:
            dst[k] = v


class Prog:
    ENGS = ("pe", "act", "dve", "pool", "sp")

    def __init__(self, nc):
        self.nc = nc
        self.ops = {e: [] for e in self.ENGS}
        self.cnt = {e: 0 for e in self.ENGS}
        self.seen = {e: dict() for e in self.ENGS}
        self.latest = {}
        self.n_ops = 0
        self.phys_free = list(range(N_DMA_SEMS))
        self.phys_cnt = [0] * N_DMA_SEMS
        self.phys_bufs = []

    @staticmethod
    def _norm(x):
        return (x, None) if isinstance(x, Buf) else x

    def _collect(self, reads, writes, skip_dw=False):
        deps = {}

        def mw(src):
            if skip_dw:
                for k, v in src.items():
                    if k[0] != "d" and deps.get(k, 0) < v:
                        deps[k] = v
            else:
                _merge(deps, src)
        for x in reads:
            b, k = self._norm(x)
            _merge(deps, b.whole[0])
            if k is None:
                for p in b.parts.values():
                    _merge(deps, p[0])
            elif k in b.parts:
                _merge(deps, b.parts[k][0])
        for x in writes:
            b, k = self._norm(x)
            mw(b.whole[0])
            _merge(deps, b.whole[1])
            if k is None:
                for p in b.parts.values():
                    mw(p[0])
                    _merge(deps, p[1])
            elif k in b.parts:
                mw(b.parts[k][0])
                _merge(deps, b.parts[k][1])
        return deps

    def _record(self, reads, writes, tok, dma_write=False):
        key, val = tok
        if self.latest.get(key, 0) < val:
            self.latest[key] = val
        for x in reads:
            b, k = self._norm(x)
            tgt = b.whole if k is None else b.parts.setdefault(k, [dict(), dict()])
            if tgt[1].get(key, 0) < val:
                tgt[1][key] = val
        for x in writes:
            b, k = self._norm(x)
            if k is None:
                if dma_write:
                    neww = {kk: vv for kk, vv in b.whole[0].items() if kk[0] == "d"}
                    for p in b.parts.values():
                        for kk, vv in p[0].items():
                            if kk[0] == "d" and neww.get(kk, 0) < vv:
                                neww[kk] = vv
                    neww[key] = max(neww.get(key, 0), val)
                    b.whole = [neww, dict()]
                else:
                    b.whole = [{key: val}, dict()]
                b.parts = {}
            else:
                if dma_write and k in b.parts:
                    neww = {kk: vv for kk, vv in b.parts[k][0].items() if kk[0] == "d"}
                    neww[key] = max(neww.get(key, 0), val)
                    b.parts[k] = [neww, dict()]
                else:
                    b.parts[k] = [{key: val}, dict()]

    def _waits(self, eng, deps):
        out = []
        seen = self.seen[eng]
        for key, val in deps.items():
            if eng == "pe" and key == ("e", "pe"):
                continue
            if seen.get(key, 0) >= val:
                continue
            seen[key] = val
            out.append((key, val))
        return out

    def op(self, eng, fn, reads=(), writes=(), signal=True):
        deps = self._collect(reads, writes)
        waits = self._waits(eng, deps)
        n = self.cnt[eng] + 1
        if signal:
            self.cnt[eng] = n
        tok = (("e", eng), n)
        self._record(reads, writes, tok)
        self.ops[eng].append((fn, waits, tok if signal else None))
        self.n_ops += 1

    def dma(self, q, fn, reads=(), writes=(), sembuf=None):
        if sembuf is None:
            sembuf = self._norm(writes[0])[0] if writes else self._norm(reads[0])[0]
        if sembuf.phys is None:
            sembuf.phys = self.phys_free.pop(0)
            self.phys_bufs.append(sembuf)
        deps = self._collect(reads, writes, skip_dw=True)
        waits = self._waits(q, deps)
        s = sembuf.phys
        self.phys_cnt[s] += 16
        key = ("d", s)
        self._record(reads, writes, (key, self.phys_cnt[s]), dma_write=True)
        self.ops[q].append((fn, waits, (key, 16)))
        self.n_ops += 1

    def barrier(self):
        for e in self.ENGS:
            waits = []
            for k, v in self.latest.items():
                if k == ("e", e):
                    continue
                if self.seen[e].get(k, 0) < v:
                    self.seen[e][k] = v
                    waits.append((k, v))
            if waits:
                self.ops[e].append((None, waits, None))
        for b in self.phys_bufs:
            self.phys_free.append(b.phys)
            b.phys = None
        self.phys_bufs = []

    def emit(self, stack):
        nc = self.nc
        sems = {}
        for e in self.ENGS:
            sems[("e", e)] = stack.enter_context(nc.semaphore("se_" + e))
        for i in range(N_DMA_SEMS):
            if self.phys_cnt[i] > 0:
                sems[("d", i)] = stack.enter_context(nc.semaphore("sd_%d" % i))
        block = stack.enter_context(nc.Block())
        prog = self

        def run(ename, eng):
            for fn, waits, inc in prog.ops[ename]:
                for k, v in waits:
                    eng.wait_ge(sems[k], v)
                if fn is None:
                    continue
                ins = fn(eng)
                if inc is not None:
                    k, v = inc
                    ins.then_inc(sems[k], 1 if k[0] == "e" else 16)

        @block.tensor
        def _(e):
            run("pe", e)

        @block.scalar
        def _(e):
            run("act", e)

        @block.vector
        def _(e):
            run("dve", e)

        @block.gpsimd
        def _(e):
            run("pool", e)

        @block.sync
        def _(e):
            run("sp", e)


class K:
    def __init__(self, taps=(), layers=(0, 1), phases=None):
        self.taps = set(taps)
        self.layers = layers
        self.phases = phases
        self.nc = bass.Bass("TRN2", target_bir_lowering=False)
        self.P = Prog(self.nc)
        self.top = 0
        self.bank_rr = 0
        self.rr = 0

    def din(self, name, shape, dt=F32):
        return self.nc.dram_tensor(name, list(shape), dt, kind="ExternalInput").ap()

    def dscr(self, name, shape, dt):
        kind = "ExternalOutput" if name in self.taps else "Internal"
        return self.nc.dram_tensor(name, list(shape), dt, kind=kind).ap()

    def alloc(self, shape, dt, name="t"):
        esz = 4 if dt == F32 else 2
        n = 1
        for s in shape[1:]:
            n *= s
        nbytes = n * esz
        off = self.top
        self.top += (nbytes + 63) // 64 * 64
        assert self.top <= ARENA_BYTES, ("SBUF arena overflow", name, self.top)
        v = self.arena[:, off // 2:(off + nbytes) // 2]
        if dt == F32:
            v = v.bitcast(F32)
        if len(shape) == 3:
            v = v.rearrange("p (a b) -> p a b", a=shape[1])
        elif len(shape) == 4:
            v = v.rearrange("p (a b c) -> p a b c", a=shape[1], b=shape[2])
        if shape[0] < 128:
            v = v[0:shape[0]]
        return v, Buf(name)

    def bank(self):
        i = self.bank_rr
        self.bank_rr = (i + 1) % 8
        return self.ps[:, i * 512:(i + 1) * 512], self.BPS[i]

    def op(self, eng, fn, reads=(), writes=(), signal=True):
        self.P.op(eng, fn, reads, writes, signal)

    def ld(self, q, out, in_, wbuf, rbufs=()):
        self.P.dma(q, lambda e: e.dma_start(out=out, in_=in_), reads=list(rbufs), writes=[wbuf])

    def st(self, q, out, in_, rbuf):
        self.P.dma(q, lambda e: e.dma_start(out=out, in_=in_), reads=[rbuf], writes=[])

    def mm(self, out, lhsT, rhs, start, stop, reads, wbuf, signal=None):
        self.P.op("pe", lambda e: e.matmul(out=out, lhsT=lhsT, rhs=rhs, start=start, stop=stop),
                  reads=reads, writes=[wbuf], signal=(stop if signal is None else signal))

    def act(self, out, in_, func, reads, wbuf, scale=1.0, bias=0.0):
        if func == AF.Copy and not (isinstance(scale, float) and scale == 1.0):
            func = AF.Identity
        self.P.op("act", lambda e: e.activation(out=out, in_=in_, func=func, scale=scale, bias=bias),
                  reads=reads, writes=[wbuf])

    def tt(self, eng, out, in0, in1, op, reads, wbuf):
        self.P.op(eng, lambda e: e.tensor_tensor(out=out, in0=in0, in1=in1, op=op), reads=reads, writes=[wbuf])

    def ts(self, eng, out, in0, s1, s2, op0, op1, reads, wbuf):
        if s2 is None:
            self.P.op(eng, lambda e: e.tensor_scalar(out=out, in0=in0, scalar1=s1, scalar2=None, op0=op0),
                      reads=reads, writes=[wbuf])
        else:
            self.P.op(eng, lambda e: e.tensor_scalar(out=out, in0=in0, scalar1=s1, scalar2=s2, op0=op0, op1=op1),
                      reads=reads, writes=[wbuf])

    def phase_begin(self):
        self.top = self.persist_top

    def phase_end(self):
        self.P.barrier()

    def want(self, name):
        return self.phases is None or name in self.phases

    def build(self):
        nc = self.nc
        st = ExitStack()
        with st:
            self.arena = st.enter_context(nc.sbuf_tensor("arena", [128, ARENA_BYTES // 2], BF16))
            self.ps = st.enter_context(nc.psum_tensor("psum", [128, 4096], F32))
            self.BPS = [Buf("ps%d" % i) for i in range(8)]
            self.declare_io()
            self.setup_persist()
            if self.want("tin"):
                self.phase_tin()
            for l in self.layers:
                last = (l == 1)
                if self.want("mods"):
                    self.phase_mods(l)
                if self.want("proj"):
                    self.phase_proj(l, last)
                if self.want("rg"):
                    self.phase_rg(l, last)
                if self.want("att"):
                    self.phase_att(l, last)
                if self.want("four"):
                    self.phase_four(l, last)
                if self.want("merge"):
                    self.phase_merge(l, last)
                if self.want("ln1"):
                    self.phase_ln(l, last, first=True)
                if self.want("moe"):
                    self.phase_moe(l, last)
                if self.want("ln2"):
                    self.phase_ln(l, last, first=False)
            self.P.barrier()
            self.P.emit(st)
        return nc

    def declare_io(self):
        d = self.din
        self.x_in = d("x", [L, D])
        self.ctx_in = d("ctx", [CT, D])
        self.cvecT = d("cvecT", [128, 8, 2])
        self.w_mod = d("w_mod", [2, D, 6 * D])
        self.b_mod_c = d("b_mod_c", [2, 128, 48])
        self.w_in = d("w_in", [2, D, WIN])
        self.sink64 = d("sink64", [2, 64, 16])
        self.w_o_attn = d("w_o_attn", [2, D, D])
        self.w_o_rg = d("w_o_rg", [2, D, D])
        self.w_o_four = d("w_o_four", [2, D, D])
        self.w_out = d("w_out", [2, D, D])
        self.w_merge = d("w_merge", [2, D, 3 * D])
        self.b_merge_c = d("b_merge_c", [2, 128, 24])
        self.rgcols = d("rgcols", [2, 128, 8, 11])
        self.rg_wbd = d("rg_wbd", [2, 8, 128, 4, 128])
        self.lncols = d("lncols", [2, 128, 4, 8])
        self.w_router = d("w_router", [2, D, NE])
        self.b_router_bc = d("b_router_bc", [2, 128, NE])
        big = self.want("moe")
        self.w_gu = d("w_gu", [2, NE, 8, D, 256] if big else [1, 1, 1, 128, 256])
        self.b_gu_c = d("b_gu_c", [2, 128, NE, 16])
        self.w_dn = d("w_dn", [2, NE, 4, D, 256] if big else [1, 1, 1, 128, 256])
        self.b_dn = d("b_dn", [2, NE, D])
        self.cos2 = d("cos2", [128, NT])
        self.sin2 = d("sin2", [128, NT])
        self.tri = d("tri", [128, 2, 128])
        self.w128 = d("w128", [128, 256])
        self.fab = d("fab", [128, 128, 128])
        self.cs256 = d("cs256", [128, 2, 2, 256])
        self.csn256 = d("csn256", [128, 2, 512])
        self.sel = d("sel", [NE, NE, 128])
        self.out = self.nc.dram_tensor("out", [L, D], F32, kind="ExternalOutput").ap()
        s = self.dscr
        self.XT = s("XT", [D, NT], F32)
        self.X1T = s("X1T", [D, NT], F32)
        self.YT = s("YT", [D, NT], F32)
        self.UT = s("UT", [D, NT], BF16)
        self.QT = s("QT", [D, NT], BF16)
        self.KT = s("KT", [256, NT], BF16)
        self.V = s("V", [NT, 256], BF16)
        self.XRT = s("XRT", [D, NT], F32)
        self.GRT = s("GRT", [D, NT], BF16)
        self.XF = s("XF", [NT, D], BF16)
        self.AT = s("AT", [D, NT], BF16)
        self.RT = s("RT", [D, NT], BF16)
        self.FT = s("FT", [D, NT], BF16)
        self.U2T = s("U2T", [D, NT], BF16)
        self.GT = s("GT", [NE, NT], BF16)

    def setup_persist(self):
        self.ident, self.Bident = self.alloc([128, 128], F32, "ident")
        self.ones_s, self.Bones_s = self.alloc([128, 128], F32, "ones_s")
        self.ones_b, self.Bones_b = self.alloc([128, 64], BF16, "ones_b")
        self.modc, self.Bmodc = self.alloc([128, 2, 48], F32, "modc")
        ident, ones_s, ones_b = self.ident, self.ones_s, self.ones_b
        self.op("pool", lambda e: e.memset(ident, 0.0), writes=[self.Bident])
        self.op("pool", lambda e: e.affine_select(out=ident, in_=ident, compare_op=ALU.not_equal, fill=1.0,
                                                  base=0, pattern=[[-1, 128]], channel_multiplier=1),
                reads=[self.Bident], writes=[self.Bident])
        self.op("pool", lambda e: e.memset(ones_s, 1.0 / D), writes=[self.Bones_s])
        self.op("pool", lambda e: e.memset(ones_b, 1.0), writes=[self.Bones_b])
        self.persist_top = self.top

    def phase_tin(self):
        self.phase_begin()
        xin = [self.alloc([128, D], F32, "xin%d" % i) for i in range(4)]
        xT = [self.alloc([128, 8, 512], F32, "xT%d" % i) for i in range(2)]
        for g in range(17):
            t0 = g * 512
            n = 512 if g < 16 else CT
            xt, Bxt = xT[g % 2]
            for s in range(n // 128):
                xi, Bxi = xin[s]
                src = self.x_in[t0 + s * 128:t0 + (s + 1) * 128, :] if g < 16 else self.ctx_in[s * 128:(s + 1) * 128, :]
                self.ld("sp", xi, src, Bxi)
                for half in range(2):
                    bk, Bbk = self.bank()
                    for i in range(4):
                        c = half * 4 + i
                        self.op("pe", lambda e, bk=bk, xi=xi, i=i, c=c: e.transpose(
                            out=bk[:, i * 128:(i + 1) * 128], in_=xi[:, c * 128:(c + 1) * 128], identity=self.ident),
                            reads=[Bxi, self.Bident], writes=[Bbk], signal=(i == 3))
                    o = xt[:, half * 4:(half + 1) * 4, s * 128:(s + 1) * 128]
                    i3 = bk.rearrange("p (a b) -> p a b", a=4)
                    if half == 0:
                        self.op("act", lambda e, o=o, i3=i3: e.copy(out=o, in_=i3), reads=[Bbk], writes=[(Bxt, (s, half))])
                    else:
                        self.op("dve", lambda e, o=o, i3=i3: e.tensor_copy(out=o, in_=i3), reads=[Bbk], writes=[(Bxt, (s, half))])
            self.st("sp", self.XT[:, t0:t0 + n].rearrange("(c p) t -> p c t", p=128), xt[:, :, 0:n], Bxt)
        self.phase_end()

    def phase_mods(self, l):
        self.phase_begin()
        scv, Bscv = self.alloc([128, 8, 2], F32, "scv")
        bmc, Bbmc = self.alloc([128, 48], F32, "bmc")
        wm = [self.alloc([128, 8, 512], F32, "wm%d" % i) for i in range(2)]
        self.ld("sp", scv, self.cvecT, Bscv)
        self.ld("sp", bmc, self.b_mod_c[l], Bbmc)
        self.act(scv, scv, AF.Silu, [Bscv], Bscv)
        bk, Bbk = self.bank()
        for n in range(12):
            w, Bw = wm[n % 2]
            self.ld("sp", w, self.w_mod[l][:, n * 512:(n + 1) * 512].rearrange("(k p) n -> p k n", p=128), Bw)
            for jj in range(4):
                j = n * 4 + jj
                for k in range(8):
                    self.mm(bk[:, 2 * j:2 * j + 2], w[:, k, jj * 128:(jj + 1) * 128], scv[:, k, :],
                            k == 0, k == 7, [Bw, Bscv], Bbk)
        pv = bk[:, 0:96].rearrange("p (j v) -> p v j", v=2)
        for v in range(2):
            self.tt("dve", self.modc[:, v, :], pv[:, v, :], bmc, ALU.add, [Bbk, Bbmc], (self.Bmodc, v))
        for lo in (8, 32):
            self.ts("dve", self.modc[:, :, lo:lo + 8], self.modc[:, :, lo:lo + 8], 1.0, None, ALU.add, None,
                    [self.Bmodc], self.Bmodc)
        self.phase_end()

    def mcol(self, which, c, v):
        j = which * 8 + c
        return self.modc[:, v, j:j + 1]

    def phase_proj(self, l, last):
        self.phase_begin()
        win, Bwin = self.alloc([128, 8, WIN], BF16, "win")
        for k in range(8):
            self.ld("pool", win[:, k, :], self.w_in[l][k * 128:(k + 1) * 128, :], (Bwin, k))
        xT = [self.alloc([128, 8, 512], F32, "xT%d" % i) for i in range(2)]
        cs = [self.alloc([128, 2, 512], F32, "cs%d" % i) for i in range(2)]
        uT = [self.alloc([128, 8, 512], BF16, "uT%d" % i) for i in range(2)]
        sf = [self.alloc([128, 512], F32, "sf%d" % i) for i in range(6)]
        sb = [self.alloc([128, 512], BF16, "sb%d" % i) for i in range(8)]
        sx = [self.alloc([128, 1024], BF16, "sx%d" % i) for i in range(2)]
        rt = [self.alloc([128, 2, 512], F32, "rt%d" % i) for i in range(2)]
        nsf = nsb = nsx = nrt = 0
        ntile = 17
        for g in range(ntile):
            t0 = g * 512
            n = 512 if g < 16 else CT
            v = 0 if g < 16 else 1
            x, Bx = xT[g % 2]
            c_, Bc_ = cs[g % 2]
            u, Bu = uT[g % 2]
            self.ld("sp", x[:, :, 0:n], self.XT[:, t0:t0 + n].rearrange("(c p) t -> p c t", p=128), Bx)
            self.ld("sp", c_[:, 0, 0:n], self.cos2[:, t0:t0 + n], (Bc_, 0))
            self.ld("sp", c_[:, 1, 0:n], self.sin2[:, t0:t0 + n], (Bc_, 1))
            for c in range(8):
                self.act(u[:, c, 0:n], x[:, c, 0:n], AF.Identity, [Bx, self.Bmodc], (Bu, c),
                         scale=self.mcol(1, c, v), bias=self.mcol(0, c, v))
            self.st("sp", self.UT[:, t0:t0 + n].rearrange("(c p) t -> p c t", p=128), u[:, :, 0:n], Bu)

            def proj_fm(col0):
                bk, Bbk = self.bank()
                for k in range(8):
                    self.mm(bk[:, 0:n], win[:, k, col0:col0 + 128], u[:, k, 0:n], k == 0, k == 7, [Bwin, Bu], Bbk)
                return bk, Bbk
            for (base, pbase, nch, dst) in ((QO, QPO, 8, self.QT), (KO, KPO, 2, self.KT)):
                if last and g == 16 and dst is self.QT:
                    continue
                for c in range(nch):
                    bA, BA = proj_fm(base + c * 128)
                    bB, BB = proj_fm(pbase + c * 128)
                    r, Br = rt[nrt % 2]
                    nrt += 1
                    o, Bo = sb[nsb % 8]
                    nsb += 1
                    self.tt("dve", r[:, 0, 0:n], bA[:, 0:n], c_[:, 0, 0:n], ALU.mult, [BA, Bc_], (Br, 0))
                    self.tt("dve", r[:, 1, 0:n], bB[:, 0:n], c_[:, 1, 0:n], ALU.mult, [BB, Bc_], (Br, 1))
                    self.tt("pool", o[:, 0:n], r[:, 0, 0:n], r[:, 1, 0:n], ALU.add, [Br], Bo)
                    self.st("sp", dst[c * 128:(c + 1) * 128, t0:t0 + n], o[:, 0:n], Bo)
            for c in range(8):
                bk, Bbk = proj_fm(XRO + c * 128)
                o, Bo = sf[nsf % 6]
                nsf += 1
                self.act(o[:, 0:n], bk[:, 0:n], AF.Copy, [Bbk], Bo)
                self.st("sp", self.XRT[c * 128:(c + 1) * 128, t0:t0 + n], o[:, 0:n], Bo)
            if not (last and g == 16):
                for c in range(8):
                    bk, Bbk = proj_fm(GRO + c * 128)
                    o, Bo = sb[nsb % 8]
                    nsb += 1
                    self.act(o[:, 0:n], bk[:, 0:n], AF.Copy, [Bbk], Bo)
                    self.st("sp", self.GRT[c * 128:(c + 1) * 128, t0:t0 + n], o[:, 0:n], Bo)
            for s in range(n // 128):
                bk, Bbk = self.bank()
                for k in range(8):
                    self.mm(bk[:, 0:256], u[:, k, s * 128:(s + 1) * 128], win[:, k, VO:VO + 256], k == 0, k == 7,
                            [Bwin, Bu], Bbk)
                o, Bo = sb[nsb % 8]
                nsb += 1
                self.op("dve", lambda e, o=o, bk=bk: e.tensor_copy(out=o[:, 0:256], in_=bk[:, 0:256]), reads=[Bbk], writes=[Bo])
                self.st("sp", self.V[t0 + s * 128:t0 + (s + 1) * 128, :], o[:, 0:256], Bo)
                if last and g == 16:
                    continue
                o, Bo = sx[nsx % 2]
                nsx += 1
                for h in range(2):
                    bk, Bbk = self.bank()
                    for k in range(8):
                        self.mm(bk, u[:, k, s * 128:(s + 1) * 128], win[:, k, XFO + h * 512:XFO + (h + 1) * 512],
                                k == 0, k == 7, [Bwin, Bu], Bbk)
                    if h == 0:
                        self.act(o[:, 0:512], bk, AF.Copy, [Bbk], (Bo, 0))
                    else:
                        self.op("dve", lambda e, o=o, bk=bk: e.tensor_copy(out=o[:, 512:1024], in_=bk), reads=[Bbk], writes=[(Bo, 1)])
                self.st("sp", self.XF[t0 + s * 128:t0 + (s + 1) * 128, :], o, Bo)
        self.phase_end()

    def phase_rg(self, l, last):
        self.phase_begin()
        XA, BXA = self.alloc([128, NT], F32, "XA")
        xc, Bxc = self.alloc([128, NT], F32, "xc")
        xcb, Bxcb = self.alloc([128, NT], BF16, "xcb")
        Bc, BBc = self.alloc([128, NT], F32, "Bc")
        H, BH = self.alloc([128, NT], F32, "H")
        tmp = [self.alloc([128, 1024], F32, "rgt%d" % i) for i in range(5)]
        ge = [self.alloc([128, 1024], F32, "ge%d" % i) for i in range(3)]
        grt = [self.alloc([128, 1024], BF16, "grt%d" % i) for i in range(2)]
        ot = [self.alloc([128, 1024], BF16, "rot%d" % i) for i in range(2)]
        wbd = [self.alloc([128, 4, 128], BF16, "wbd%d" % i) for i in range(2)]
        cols, Bcols = self.alloc([128, 8, 11], F32, "rgcols")
        cvec, Bcvec = self.alloc([128, 8, 2], F32, "rgc")
        ct, Bct = self.alloc([128, 8, 2], F32, "rgct")
        self.ld("sp", cols, self.rgcols[l], Bcols)
        lam = cols[:, :, 9:11]
        self.act(cvec, lam, AF.Exp, [Bcols], Bcvec, scale=-1.0)
        self.ts("dve", ct, cvec, -1.0 / 3.0, 0.5, ALU.mult, ALU.add, [Bcvec], Bct)
        self.tt("dve", ct, ct, cvec, ALU.mult, [Bct, Bcvec], Bct)
        self.ts("dve", ct, ct, -1.0, 1.0, ALU.mult, ALU.add, [Bct], Bct)
        self.tt("dve", ct, ct, cvec, ALU.mult, [Bct, Bcvec], Bct)
        self.ts("dve", cvec, ct, -8.0, None, ALU.mult, None, [Bct], Bcvec)
        segs = ((0, L), (L, NT))
        groups = [(i * 1024, 1024) for i in range(8)] + [(L, CT)]

        def rev(ap2, a, b):
            base = ap2[:, a:b]
            pstep = base.ap[0][0]
            return bass.AP(base.tensor, base.offset + (b - a - 1), [[pstep, 128], [-1, b - a]])
        for c in range(8):
            w, Bw = wbd[c % 2]
            self.ld("pool", w, self.rg_wbd[l, c], Bw)
            self.ld("sp", XA, self.XRT[c * 128:(c + 1) * 128, :], BXA)
            cw = lambda i: cols[:, c, i:i + 1]
            for (a, b) in segs:
                self.ts("dve", xc[:, a:b], XA[:, a:b], cw(2), cw(4), ALU.mult, ALU.add, [BXA, Bcols], Bxc)
                for (tap, so, do) in ((1, 0, 1), (0, 0, 2), (3, 1, 0)):
                    nn = (b - a) - max(so, do)
                    wtap = cw(tap)
                    self.op("dve", lambda e, a=a, so=so, do=do, nn=nn, wtap=wtap: e.scalar_tensor_tensor(
                        out=xc[:, a + do:a + do + nn], in0=XA[:, a + so:a + so + nn], scalar=wtap,
                        in1=xc[:, a + do:a + do + nn], op0=ALU.mult, op1=ALU.add),
                        reads=[BXA, Bcols, Bxc], writes=[Bxc])
            self.act(xcb, xc, AF.Copy, [Bxc], Bxcb)
            for d in range(2):
                A, BA = XA, BXA
                for (g0, gn) in groups:
                    R, BR = tmp[0]
                    IG, BIG = tmp[1]
                    for h in range(gn // 512 if gn >= 512 else 1):
                        hn = min(512, gn)
                        o0 = g0 + h * 512
                        bk, Bbk = self.bank()
                        self.mm(bk[:, 0:hn], w[:, d, :], xcb[:, o0:o0 + hn], True, True, [Bw, Bxcb], Bbk)
                        self.act(R[:, h * 512:h * 512 + hn], bk[:, 0:hn], AF.Sigmoid, [Bbk, Bcols], (BR, h),
                                 bias=cols[:, c, 5 + d:6 + d])
                        bk, Bbk = self.bank()
                        self.mm(bk[:, 0:hn], w[:, 2 + d, :], xcb[:, o0:o0 + hn], True, True, [Bw, Bxcb], Bbk)
                        self.act(IG[:, h * 512:h * 512 + hn], bk[:, 0:hn], AF.Sigmoid, [Bbk, Bcols], (BIG, h),
                                 bias=cols[:, c, 7 + d:8 + d])
                    M, BM = tmp[2]
                    S, BS = tmp[3]
                    GX, BGX = tmp[4]
                    self.act(A[:, g0:g0 + gn], R[:, 0:gn], AF.Exp, [BR, Bcvec], BA, scale=cvec[:, c, d:d + 1])
                    self.tt("pool", M[:, 0:gn], A[:, g0:g0 + gn], A[:, g0:g0 + gn], ALU.mult, [BA], BM)
                    self.act(S[:, 0:gn], M[:, 0:gn], AF.Sqrt, [BM], BS, scale=-1.0, bias=1.0)
                    self.tt("pool", GX[:, 0:gn], IG[:, 0:gn], xc[:, g0:g0 + gn], ALU.mult, [BIG, Bxc], BGX)
                    self.tt("dve", Bc[:, g0:g0 + gn], S[:, 0:gn], GX[:, 0:gn], ALU.mult, [BS, BGX], BBc)
                dst, Bdst = (H, BH) if d == 0 else (Bc, BBc)
                if d == 0:
                    self.op("dve", lambda e: e.tensor_tensor_scan(out=H[:, L:NT], data0=A[:, L:NT], data1=Bc[:, L:NT],
                                                                 initial=0.0, op0=ALU.mult, op1=ALU.add),
                            reads=[BA, BBc], writes=[BH])
                    prev = H[:, NT - 1:NT]
                    for i in range(4):
                        a, b = i * 2048, (i + 1) * 2048
                        self.op("dve", lambda e, a=a, b=b, prev=prev: e.tensor_tensor_scan(
                            out=H[:, a:b], data0=A[:, a:b], data1=Bc[:, a:b], initial=prev, op0=ALU.mult, op1=ALU.add),
                            reads=[BA, BBc, BH], writes=[BH])
                        prev = H[:, b - 1:b]
                else:
                    self.op("dve", lambda e: e.tensor_tensor_scan(out=rev(Bc, L, NT), data0=rev(A, L, NT), data1=rev(Bc, L, NT),
                                                                 initial=0.0, op0=ALU.mult, op1=ALU.add),
                            reads=[BA, BBc], writes=[BBc])
                    prev = Bc[:, L:L + 1]
                    for i in range(3, -1, -1):
                        a, b = i * 2048, (i + 1) * 2048
                        self.op("dve", lambda e, a=a, b=b, prev=prev: e.tensor_tensor_scan(
                            out=rev(Bc, a, b), data0=rev(A, a, b), data1=rev(Bc, a, b), initial=prev,
                            op0=ALU.mult, op1=ALU.add), reads=[BA, BBc], writes=[BBc])
                        prev = Bc[:, a:a + 1]
            for gi, (g0, gn) in enumerate(groups):
                if last and g0 == L:
                    continue
                gr, Bgr = grt[gi % 2]
                o, Bo = ot[gi % 2]
                self.ld("sp", gr[:, 0:gn], self.GRT[c * 128:(c + 1) * 128, g0:g0 + gn], Bgr)
                t0_, B0 = ge[0]
                t1_, B1 = ge[1]
                t2_, B2 = ge[2]
                self.tt("pool", t0_[:, 0:gn], gr[:, 0:gn], gr[:, 0:gn], ALU.mult, [Bgr], B0)
                self.ts("dve", t0_[:, 0:gn], t0_[:, 0:gn], 0.044715, 1.0, ALU.mult, ALU.add, [B0], B0)
                self.tt("pool", t0_[:, 0:gn], t0_[:, 0:gn], gr[:, 0:gn], ALU.mult, [B0, Bgr], B0)
                self.act(t1_[:, 0:gn], t0_[:, 0:gn], AF.Sigmoid, [B0], B1, scale=2.0 * math.sqrt(2.0 / math.pi))
                self.tt("pool", t1_[:, 0:gn], t1_[:, 0:gn], gr[:, 0:gn], ALU.mult, [B1, Bgr], B1)
                self.tt("pool", t2_[:, 0:gn], H[:, g0:g0 + gn], Bc[:, g0:g0 + gn], ALU.add, [BH, BBc], B2)
                self.tt("dve", o[:, 0:gn], t1_[:, 0:gn], t2_[:, 0:gn], ALU.mult, [B1, B2], Bo)
                self.st("sp", self.RT[c * 128:(c + 1) * 128, g0:g0 + gn], o[:, 0:gn], Bo)
        self.phase_end()

    def phase_att(self, l, last):
        self.phase_begin()
        KTs, BKT = self.alloc([64, 4, NT], BF16, "KTs")
        Vs, BV = self.alloc([128, 66, 256], BF16, "Vs")
        tri, Btri = self.alloc([128, 2, 128], BF16, "tri")
        sk, Bsk = self.alloc([64, 16], F32, "sk")
        Qs = [self.alloc([64, 16, 512], BF16, "Qs%d" % i) for i in range(2)]
        PT = [self.alloc([128, 512], BF16, "PT%d" % i) for i in range(12)]
        OT = [self.alloc([64, 16, 128], BF16, "OT%d" % i) for i in range(2)]
        rc = [self.alloc([64, 512], F32, "rc%d" % i) for i in range(2)]
        self.ld("sp", KTs, self.KT.rearrange("(g d) t -> d g t", d=64), BKT)
        vsrc = self.V.rearrange("(b p) c -> p b c", p=128)
        for i in range(6):
            self.ld("sp", Vs[:, i * 11:(i + 1) * 11, :], vsrc[:, i * 11:(i + 1) * 11, :], BV)
        self.ld("pool", tri, self.tri, Btri)
        self.ld("sp", sk, self.sink64[l], Bsk)
        self.act(sk, sk, AF.Exp, [Bsk], Bsk)
        npt = 0
        nblk = 64 if last else 66
        for n in range(nblk):
            if n % 4 == 0:
                q, Bq = Qs[(n // 4) % 2]
                nq = min(512, (nblk - n) * 128)
                self.ld("sp", q[:, :, 0:nq], self.QT[:, n * 128:n * 128 + nq].rearrange("(h d) t -> d h t", d=64), Bq)
            qo = (n % 4) * 128
            if n < 64:
                kbs = [(kb, m) for (kb, m) in ((n - 1, 0), (n, None), (n + 1, 1)) if 0 <= kb < 64] + [(64, None), (65, None)]
            else:
                kbs = [(64, None), (65, None)]
            o, Bo = OT[n % 2]
            for g in range(4):
                pts = []
                for (kb, m) in kbs:
                    bk, Bbk = self.bank()
                    self.mm(bk.rearrange("p (h q) -> p h q", h=4), KTs[:, g, kb * 128:(kb + 1) * 128],
                            q[:, 4 * g:4 * g + 4, qo:qo + 128], True, True, [BKT, Bq], Bbk)
                    p, Bp = PT[npt % 12]
                    npt += 1
                    self.act(p, bk, AF.Exp, [Bbk], Bp, scale=0.125)
                    if m is not None:
                        p3 = p.rearrange("p (h q) -> p h q", h=4)
                        msk = tri[:, m:m + 1, :].to_broadcast([128, 4, 128])
                        self.tt("pool", p3, p3, msk, ALU.mult, [Bp, Btri], Bp)
                    pts.append((p, Bp, kb))
                bn, Bbn = self.bank()
                bd, Bbd = self.bank()
                for i, (p, Bp, kb) in enumerate(pts):
                    self.mm(bn[0:64, :], Vs[:, kb, g * 64:(g + 1) * 64], p, i == 0, i == len(pts) - 1, [BV, Bp], Bbn)
                for i, (p, Bp, kb) in enumerate(pts):
                    self.mm(bd[0:64, :], self.ones_b, p, i == 0, i == len(pts) - 1, [self.Bones_b, Bp], Bbd)
                r, Br = rc[g % 2]
                r3 = r.rearrange("p (h q) -> p h q", h=4)
                self.tt("dve", r3, bd[0:64, :].rearrange("p (h q) -> p h q", h=4),
                        sk[:, 4 * g:4 * g + 4].unsqueeze(2).to_broadcast([64, 4, 128]), ALU.add, [Bbd, Bsk], Br)
                self.op("dve", lambda e, r=r: e.reciprocal(out=r, in_=r), reads=[Br], writes=[Br])
                self.tt("dve", o[:, 4 * g:4 * g + 4, :], bn[0:64, :].rearrange("p (h q) -> p h q", h=4), r3, ALU.mult,
                        [Bbn, Br], (Bo, g))
            self.st("sp", self.AT[:, n * 128:(n + 1) * 128].rearrange("(h d) t -> d h t", d=64), o, Bo)
        self.phase_end()

    def phase_four(self, l, last):
        self.phase_begin()
        SC = 1.0 / math.sqrt(L * 256.0)
        SCC = 1.0 / math.sqrt(CT * 256.0)
        w128, Bw128 = self.alloc([128, 256], BF16, "w128")
        fab, Bfab = self.alloc([128, 128, 128], BF16, "fab")
        csm, Bcsm = self.alloc([128, 2, 2, 256], BF16, "csm")
        Xr = [self.alloc([128, 64, 128], BF16, "Xr%d" % i) for i in range(2)]
        Bst, BBst = self.alloc([128, 128, 128], BF16, "Bst")
        PQ = [self.alloc([128, 2, L], BF16, "PQ%d" % i) for i in range(2)]
        yo = [self.alloc([128, 512], BF16, "fyo%d" % i) for i in range(4)]
        self.ld("pool", w128, self.w128, Bw128)
        for i in range(4):
            self.ld("pool", fab[:, i * 32:(i + 1) * 32, :], self.fab[:, i * 32:(i + 1) * 32, :], Bfab)
        self.ld("pool", csm, self.cs256, Bcsm)
        nyo = 0
        FSTOP = int(os.environ.get("FOUR_STOP", "9"))

        def chan_stage(grp, pq, t0, n, src_off, scale):
            nonlocal nyo
            for cp in range(2):
                bk, Bbk = self.bank()
                i = 0
                for j in range(2):
                    for pqi in range(2):
                        p_, Bp_ = pq[j]
                        self.mm(bk[:, 0:n], csm[:, j, pqi, cp * 128:(cp + 1) * 128], p_[:, pqi, src_off:src_off + n],
                                i == 0, i == 3, [Bcsm, Bp_], Bbk)
                        i += 1
                o, Bo = yo[nyo % 4]
                nyo += 1
                self.act(o[:, 0:n], bk[:, 0:n], AF.Copy, [Bbk], Bo, scale=scale)
                row = (2 * grp + cp) * 128
                self.st("sp", self.FT[row:row + 128, t0:t0 + n], o[:, 0:n], Bo)
        for cc in range(8):
            xr, Bxr = Xr[cc % 2]
            pq, Bpq = PQ[cc % 2]
            xsrc = self.XF[0:L, cc * 128:(cc + 1) * 128].rearrange("(a b) c -> a b c", b=64)
            for i in range(8):
                self.ld("sp", xr[:, i * 8:(i + 1) * 8, :], xsrc[:, i * 8:(i + 1) * 8, :], Bxr)
            for cb in range(32):
                bk, Bbk = self.bank()
                for ci in range(4):
                    ch = cb * 4 + ci
                    lhsT = xr[:, :, ch]
                    self.mm(bk[0:64, ci * 128:(ci + 1) * 128], lhsT, w128[:, 0:128], True, True, [Bxr, Bw128], Bbk, signal=False)
                    self.mm(bk[64:128, ci * 128:(ci + 1) * 128], lhsT, w128[:, 128:256], True, True, [Bxr, Bw128], Bbk,
                            signal=(ci == 3))
                src = bk.rearrange("p (c k) -> p c k", c=4)
                dstv = Bst[:, :, cb * 4:cb * 4 + 4].rearrange("p k c -> p c k")
                if cb % 2 == 0:
                    self.op("act", lambda e, dstv=dstv, src=src: e.copy(out=dstv, in_=src), reads=[Bbk], writes=[(BBst, cb)])
                else:
                    self.op("dve", lambda e, dstv=dstv, src=src: e.tensor_copy(out=dstv, in_=src), reads=[Bbk], writes=[(BBst, cb)])
            for kg in range(8 if FSTOP >= 2 else 0):
                banks = [self.bank() for _ in range(4)]
                for ki in range(16):
                    k1 = kg * 16 + ki
                    bk, Bbk = banks[ki // 4]
                    self.mm(bk[:, (ki % 4) * 128:(ki % 4) * 128 + 128], Bst[:, k1, :], fab[:, k1, :], True, True,
                            [BBst, Bfab], Bbk, signal=(ki % 4 == 3))
                for bi in range(4 if os.environ.get("FOUR_NOEVAC") is None else 0):
                    bk, Bbk = banks[bi]
                    for pqi in range(2):
                        src = bk.rearrange("p (k r q) -> p k r q", k=4, r=2)[:, :, pqi, :]
                        k10 = kg * 16 + bi * 4
                        dbase = pq[:, pqi, :]
                        pstep = dbase.ap[0][0]
                        dstv = bass.AP(dbase.tensor, dbase.offset + k10, [[pstep, 128], [1, 4], [128, 64]])
                        if bi % 2 == 0:
                            self.op("act", lambda e, dstv=dstv, src=src: e.copy(out=dstv, in_=src), reads=[Bbk],
                                    writes=[(Bpq, (kg, bi, pqi))])
                        else:
                            self.op("dve", lambda e, dstv=dstv, src=src: e.tensor_copy(out=dstv, in_=src), reads=[Bbk],
                                    writes=[(Bpq, (kg, bi, pqi))])
            if cc % 2 == 1 and FSTOP >= 3:
                for g in range(16):
                    chan_stage(cc // 2, [PQ[0], PQ[1]], g * 512, 512, g * 512, SC)
        if not last and FSTOP >= 4:
            csn, Bcsn = self.alloc([128, 2, 512], BF16, "csn")
            self.ld("pool", csn, self.csn256, Bcsn)
            xct, Bxct = self.alloc([128, 2, D], BF16, "xct")
            self.ld("sp", xct, self.XF[L:NT, :].rearrange("(k p) c -> p k c", p=128), Bxct)
            for cc in range(8):
                pq, Bpq = PQ[cc % 2]
                bk, Bbk = self.bank()
                for k in range(2):
                    self.mm(bk, xct[:, k, cc * 128:(cc + 1) * 128], csn[:, k, :], k == 0, k == 1, [Bxct, Bcsn], Bbk)
                self.act(pq[:, :, 0:CT], bk.rearrange("p (r t) -> p r t", r=2), AF.Copy, [Bbk], Bpq)
                if cc % 2 == 1:
                    chan_stage(cc // 2, [PQ[0], PQ[1]], L, CT, 0, SCC)
        self.phase_end()

    def phase_merge(self, l, last):
        self.phase_begin()
        wo = []
        for i, src in enumerate((self.w_o_attn, self.w_o_rg, self.w_o_four, self.w_out)):
            w, Bw = self.alloc([128, 8, D], BF16, "wo%d" % i)
            for k in range(0, 8, 4):
                self.ld("pool", w[:, k:k + 4, :], src[l][k * 128:(k + 4) * 128, :].rearrange("(k p) n -> p k n", p=128), (Bw, k))
            wo.append((w, Bw))
        wmg, Bwmg = self.alloc([128, 8, 3 * D], BF16, "wmg")
        for k in range(8):
            self.ld("pool", wmg[:, k, :], self.w_merge[l][k * 128:(k + 1) * 128, :], (Bwmg, k))
        bmg, Bbmg = self.alloc([128, 24], F32, "bmg")
        self.ld("sp", bmg, self.b_merge_c[l], Bbmg)
        N = 256
        ins = [[self.alloc([128, 8, N], BF16, "mi%d_%d" % (i, j)) for j in range(2)] for i in range(4)]
        mg = [self.alloc([128, 8, N], BF16, "mg%d" % i) for i in range(2)]
        gt = [self.alloc([128, N], BF16, "gt%d" % i) for i in range(6)]
        pt = [self.alloc([128, N], F32, "pt%d" % i) for i in range(6)]
        yo = [self.alloc([128, 8, N], F32, "myo%d" % i) for i in range(2)]
        ntok = L if last else NT
        ngt = npt = 0
        for g in range(ntok // N):
            t0 = g * N
            tiles = []
            for i, src in enumerate((self.UT, self.AT, self.RT, self.FT)):
                t, Bt = ins[i][g % 2]
                self.ld("sp", t, src[:, t0:t0 + N].rearrange("(c p) t -> p c t", p=128), Bt)
                tiles.append((t, Bt))
            u, Bu = tiles[0]
            m, Bm = mg[g % 2]
            for j in range(8):
                terms = []
                for br in range(3):
                    bg, Bbg = self.bank()
                    col = br * D + j * 128
                    for k in range(8):
                        self.mm(bg[:, 0:N], wmg[:, k, col:col + 128], u[:, k, :], k == 0, k == 7, [Bwmg, Bu], Bbg)
                    gg, Bgg = gt[ngt % 6]
                    ngt += 1
                    self.act(gg, bg[:, 0:N], AF.Sigmoid, [Bbg, Bbmg], Bgg, bias=bmg[:, br * 8 + j:br * 8 + j + 1])
                    by, Bby = self.bank()
                    w, Bw = wo[br]
                    t, Bt = tiles[1 + br]
                    for k in range(8):
                        self.mm(by[:, 0:N], w[:, k, j * 128:(j + 1) * 128], t[:, k, :], k == 0, k == 7, [Bw, Bt], Bby)
                    p, Bp = pt[npt % 6]
                    npt += 1
                    self.tt("dve", p, by[:, 0:N], gg, ALU.mult, [Bby, Bgg], Bp)
                    terms.append((p, Bp))
                (p0, B0), (p1, B1), (p2, B2) = terms
                self.tt("pool", p0, p0, p1, ALU.add, [B0, B1], B0)
                self.tt("pool", m[:, j, :], p0, p2, ALU.add, [B0, B2], (Bm, j))
            o, Bo = yo[g % 2]
            w, Bw = wo[3]
            for j in range(8):
                by, Bby = self.bank()
                for k in range(8):
                    self.mm(by[:, 0:N], w[:, k, j * 128:(j + 1) * 128], m[:, k, :], k == 0, k == 7, [Bw, Bm], Bby)
                if j % 2 == 0:
                    self.act(o[:, j, :], by[:, 0:N], AF.Copy, [Bby], (Bo, j))
                else:
                    self.op("dve", lambda e, o=o, by=by, j=j: e.tensor_copy(out=o[:, j, :], in_=by[:, 0:N]), reads=[Bby], writes=[(Bo, j)])
            self.st("sp", self.YT[:, t0:t0 + N].rearrange("(c p) t -> p c t", p=128), o, Bo)
        self.phase_end()

    def phase_ln(self, l, last, first):
        self.phase_begin()
        src_x = self.XT if first else self.X1T
        dst_x = self.X1T if first else self.XT
        gate_w = 2 if first else 5
        gi, bi = (0, 1) if first else (2, 3)
        lnc, Blnc = self.alloc([128, 4, 8], F32, "lnc")
        self.ld("sp", lnc, self.lncols[l], Blnc)
        N = 512
        xs = [self.alloc([128, 8, N], F32, "lx%d" % i) for i in range(2)]
        ys = [self.alloc([128, 8, N], F32, "ly%d" % i) for i in range(2)]
        rs, Brs = self.alloc([128, 8, N], F32, "lr")
        sq, Bsq = self.alloc([128, 8, N], F32, "lsq")
        xo = [self.alloc([128, 8, N], F32, "lxo%d" % i) for i in range(2)]
        st_ = [self.alloc([128, N], F32, "lst%d" % i) for i in range(4)]
        final = last and not first
        if first:
            u2 = [self.alloc([128, 8, N], BF16, "lu%d" % i) for i in range(2)]
            u2f, Bu2f = self.alloc([128, 8, N], F32, "luf")
            wr, Bwr = self.alloc([128, 8, NE], F32, "wr")
            brb, Bbrb = self.alloc([128, NE], F32, "brb")
            self.ld("sp", wr, self.w_router[l].rearrange("(k p) e -> p k e", p=128), Bwr)
            self.ld("sp", brb, self.b_router_bc[l], Bbrb)
            lg = [self.alloc([128, NE], F32, "lg%d" % i) for i in range(2)]
            sm = [self.alloc([128, 16], F32, "sm%d" % i) for i in range(2)]
            gts = [self.alloc([NE, N], BF16, "gts%d" % i) for i in range(2)]
        if final:
            ot = [self.alloc([128, D], F32, "lot%d" % i) for i in range(2)]
        ntok = L if last else NT
        ntile = (ntok + N - 1) // N
        for g in range(ntile):
            t0 = g * N
            n = min(N, ntok - t0)
            v = 0 if t0 < L else 1
            x, Bx = xs[g % 2]
            y, By = ys[g % 2]
            self.ld("sp", x[:, :, 0:n], src_x[:, t0:t0 + n].rearrange("(c p) t -> p c t", p=128), Bx)
            self.ld("sp", y[:, :, 0:n], self.YT[:, t0:t0 + n].rearrange("(c p) t -> p c t", p=128), By)
            for c in range(8):
                self.act(y[:, c, 0:n], y[:, c, 0:n], AF.Copy, [By, self.Bmodc], (By, c), scale=self.mcol(gate_w, c, v))
                self.op("dve", lambda e, c=c, x=x, y=y, n=n: e.scalar_tensor_tensor(
                    out=rs[:, c, 0:n], in0=x[:, c, 0:n], scalar=ALPHA, in1=y[:, c, 0:n], op0=ALU.mult, op1=ALU.add),
                    reads=[Bx, (By, c)], writes=[(Brs, c)])
                self.act(sq[:, c, 0:n], rs[:, c, 0:n], AF.Square, [(Brs, c)], (Bsq, c))
            bm, Bbm = self.bank()
            bq, Bbq = self.bank()
            for c in range(8):
                self.mm(bm[:, 0:n], self.ones_s, rs[:, c, 0:n], c == 0, c == 7, [self.Bones_s, (Brs, c)], Bbm)
            for c in range(8):
                self.mm(bq[:, 0:n], self.ones_s, sq[:, c, 0:n], c == 0, c == 7, [self.Bones_s, (Bsq, c)], Bbq)
            (mean, Bmean), (m2, Bm2), (var, Bvar), (rstd, Brstd) = st_
            self.act(mean[:, 0:n], bm[:, 0:n], AF.Copy, [Bbm], Bmean)
            self.tt("dve", m2[:, 0:n], bm[:, 0:n], mean[:, 0:n], ALU.mult, [Bbm, Bmean], Bm2)
            self.tt("dve", var[:, 0:n], bq[:, 0:n], m2[:, 0:n], ALU.subtract, [Bbq, Bm2], Bvar)
            self.ts("dve", var[:, 0:n], var[:, 0:n], LN_EPS, None, ALU.add, None, [Bvar], Bvar)
            self.act(var[:, 0:n], var[:, 0:n], AF.Sqrt, [Bvar], Bvar)
            self.op("dve", lambda e, n=n: e.reciprocal(out=rstd[:, 0:n], in_=var[:, 0:n]), reads=[Bvar], writes=[Brstd])
            o, Bo = xo[g % 2]
            for c in range(8):
                self.tt("pool", rs[:, c, 0:n], rs[:, c, 0:n], mean[:, 0:n], ALU.subtract, [(Brs, c), Bmean], (Brs, c))
                self.tt("dve", rs[:, c, 0:n], rs[:, c, 0:n], rstd[:, 0:n], ALU.mult, [(Brs, c), Brstd], (Brs, c))
                self.act(o[:, c, 0:n], rs[:, c, 0:n], AF.Identity, [(Brs, c), Blnc], (Bo, c),
                         scale=lnc[:, gi, c:c + 1], bias=lnc[:, bi, c:c + 1])
            if not final:
                self.st("sp", dst_x[:, t0:t0 + n].rearrange("(c p) t -> p c t", p=128), o[:, :, 0:n], Bo)
            if first:
                u, Bu = u2[g % 2]
                for c in range(8):
                    self.act(u[:, c, 0:n], o[:, c, 0:n], AF.Identity, [(Bo, c), self.Bmodc], (Bu, c),
                             scale=self.mcol(4, c, v), bias=self.mcol(3, c, v))
                    self.act(u2f[:, c, 0:n], o[:, c, 0:n], AF.Identity, [(Bo, c), self.Bmodc], (Bu2f, c),
                             scale=self.mcol(4, c, v), bias=self.mcol(3, c, v))
                self.st("sp", self.U2T[:, t0:t0 + n].rearrange("(c p) t -> p c t", p=128), u[:, :, 0:n], Bu)
                gtile, Bgt = gts[g % 2]
                for s in range(n // 128):
                    bk, Bbk = self.bank()
                    for c in range(8):
                        self.mm(bk[:, 0:NE], u2f[:, c, s * 128:(s + 1) * 128], wr[:, c, :], c == 0, c == 7,
                                [(Bu2f, c), Bwr], Bbk)
                    lgt, Blg = lg[s % 2]
                    smt, Bsm = sm[s % 2]
                    self.tt("dve", lgt, bk[:, 0:NE], brb, ALU.add, [Bbk, Bbrb], Blg)
                    self.op("dve", lambda e, smt=smt, lgt=lgt: e.max(out=smt[:, 0:8], in_=lgt), reads=[Blg], writes=[Bsm])
                    self.ts("dve", smt[:, 8:9], smt[:, 0:1], -1.0, None, ALU.mult, None, [Bsm], Bsm)
                    bk2, Bbk2 = self.bank()
                    self.ts("dve", bk2[:, 0:NE], lgt, smt[:, 3:4], None, ALU.is_ge, None, [Blg, Bsm], Bbk2)
                    self.act(lgt, lgt, AF.Exp, [Blg, Bsm], Blg, bias=smt[:, 8:9])
                    self.tt("dve", lgt, lgt, bk2[:, 0:NE], ALU.mult, [Blg, Bbk2], Blg)
                    self.op("dve", lambda e, smt=smt, lgt=lgt: e.tensor_reduce(out=smt[:, 9:10], in_=lgt, axis=AX.X, op=ALU.add),
                            reads=[Blg], writes=[Bsm])
                    self.op("dve", lambda e, smt=smt: e.reciprocal(out=smt[:, 10:11], in_=smt[:, 9:10]), reads=[Bsm], writes=[Bsm])
                    self.ts("dve", lgt, lgt, smt[:, 10:11], None, ALU.mult, None, [Blg, Bsm], Blg)
                    bk3, Bbk3 = self.bank()
                    self.op("pe", lambda e, bk3=bk3, lgt=lgt: e.transpose(out=bk3[0:NE, 0:128], in_=lgt, identity=self.ident),
                            reads=[Blg, self.Bident], writes=[Bbk3])
                    self.act(gtile[:, s * 128:(s + 1) * 128], bk3[0:NE, 0:128], AF.Copy, [Bbk3], (Bgt, s))
                self.st("sp", self.GT[:, t0:t0 + n], gtile[:, 0:n], Bgt)
            if final:
                for s in range(n // 128):
                    ott, Bott = ot[s % 2]
                    for half in range(2):
                        bk, Bbk = self.bank()
                        for i in range(4):
                            c = half * 4 + i
                            self.op("pe", lambda e, bk=bk, o=o, i=i, c=c, s=s: e.transpose(
                                out=bk[:, i * 128:(i + 1) * 128], in_=o[:, c, s * 128:(s + 1) * 128], identity=self.ident),
                                reads=[(Bo, c), self.Bident], writes=[Bbk], signal=(i == 3))
                        if half == 0:
                            self.act(ott[:, 0:512], bk, AF.Copy, [Bbk], (Bott, 0))
                        else:
                            self.op("dve", lambda e, ott=ott, bk=bk: e.tensor_copy(out=ott[:, 512:1024], in_=bk), reads=[Bbk], writes=[(Bott, 1)])
                    self.st("sp", self.out[t0 + s * 128:t0 + (s + 1) * 128, :], ott, Bott)
        self.phase_end()

    def phase_moe(self, l, last):
        self.phase_begin()
        T = 2048
        bgu, Bbgu = self.alloc([128, NE, 16], F32, "bgu")
        bdn, Bbdn = self.alloc([NE, D], BF16, "bdn")
        sel, Bsel = self.alloc([NE, NE, 128], BF16, "sel")
        self.ld("sp", bgu, self.b_gu_c[l], Bbgu)
        self.ld("pool", bdn, self.b_dn[l], Bbdn)
        self.ld("pool", sel, self.sel, Bsel)
        uT, BuT = self.alloc([128, 8, T], BF16, "mu")
        fT, BfT = self.alloc([128, 8, T], F32, "mf")
        aT, BaT = self.alloc([128, 8, T], BF16, "ma")
        gbc, Bgbc = self.alloc([128, T], BF16, "gbc")
        gT, BgT = self.alloc([NE, T], BF16, "mgT")
        wg = [self.alloc([128, 8, 256], BF16, "wg%d" % i) for i in range(4)]
        wd = [self.alloc([128, 8, 256], BF16, "wd%d" % i) for i in range(4)]
        tf = [self.alloc([128, 512], F32, "mt%d" % i) for i in range(9)]
        ntf = 0
        ntok = L if last else NT
        sts = [(t0, min(T, ntok - t0)) for t0 in range(0, ntok, T)]
        units = []
        for si in range(len(sts)):
            for e in range(NE):
                for c in range(8):
                    units.append(("g", si, e, c))
                for jj in range(4):
                    units.append(("d", si, e, jj))
        cnt = {"g": 0, "d": 0}
        slots = {}
        issued = 0

        def issue(upto):
            nonlocal issued
            while issued < min(upto, len(units)):
                kind, si, e, i = units[issued]
                if kind == "g":
                    w, Bw = wg[cnt["g"] % 4]
                    cnt["g"] += 1
                    self.ld("pool", w, self.w_gu[l, e, i].rearrange("(k p) n -> p k n", p=128), Bw)
                else:
                    w, Bw = wd[cnt["d"] % 4]
                    cnt["d"] += 1
                    self.ld("pool", w, self.w_dn[l, e, i].rearrange("(k p) n -> p k n", p=128), Bw)
                slots[issued] = (w, Bw)
                issued += 1
        ui = 0
        LOOK = 4
        for si, (s0, sn) in enumerate(sts):
            tiles = [(o, min(512, sn - o)) for o in range(0, sn, 512)]
            self.ld("sp", uT[:, :, 0:sn], self.U2T[:, s0:s0 + sn].rearrange("(c p) t -> p c t", p=128), BuT)
            self.ld("sp", gT[:, 0:sn], self.GT[:, s0:s0 + sn], BgT)
            for (o, n) in tiles:
                for j in range(8):
                    bk, Bbk = self.bank()
                    self.mm(bk[:, 0:n], bdn[:, j * 128:(j + 1) * 128], gT[:, o:o + n], True, True, [Bbdn, BgT], Bbk)
                    self.act(fT[:, j, o:o + n], bk[:, 0:n], AF.Copy, [Bbk], (BfT, (j, o)))
            for e in range(NE):
                for (o, n) in tiles:
                    bk, Bbk = self.bank()
                    self.mm(bk[:, 0:n], sel[:, e, :], gT[:, o:o + n], True, True, [Bsel, BgT], Bbk)
                    self.act(gbc[:, o:o + n], bk[:, 0:n], AF.Copy, [Bbk], (Bgbc, o))
                for c in range(8):
                    issue(ui + LOOK)
                    w, Bw = slots.pop(ui)
                    ui += 1
                    for (o, n) in tiles:
                        bg, Bbg = self.bank()
                        bl, Bbl = self.bank()
                        for k in range(8):
                            self.mm(bg[:, 0:n], w[:, k, 0:128], uT[:, k, o:o + n], k == 0, k == 7, [Bw, BuT], Bbg)
                        for k in range(8):
                            self.mm(bl[:, 0:n], w[:, k, 128:256], uT[:, k, o:o + n], k == 0, k == 7, [Bw, BuT], Bbl)
                        (glu, Bglu), (sig, Bsig), (lin, Blin) = tf[ntf % 9], tf[(ntf + 1) % 9], tf[(ntf + 2) % 9]
                        ntf += 3
                        self.ts("dve", glu[:, 0:n], bg[:, 0:n], bgu[:, e, c:c + 1], 7.0, ALU.add, ALU.min, [Bbg, Bbgu], Bglu)
                        self.act(sig[:, 0:n], glu[:, 0:n], AF.Sigmoid, [Bglu], Bsig, scale=1.702)
                        self.ts("dve", lin[:, 0:n], bl[:, 0:n], bgu[:, e, 8 + c:9 + c], 7.0, ALU.add, ALU.min, [Bbl, Bbgu], Blin)
                        self.ts("dve", lin[:, 0:n], lin[:, 0:n], -7.0, 1.0, ALU.max, ALU.add, [Blin], Blin)
                        self.tt("pool", glu[:, 0:n], glu[:, 0:n], sig[:, 0:n], ALU.mult, [Bglu, Bsig], Bglu)
                        self.tt("pool", glu[:, 0:n], glu[:, 0:n], lin[:, 0:n], ALU.mult, [Bglu, Blin], Bglu)
                        self.tt("dve", aT[:, c, o:o + n], glu[:, 0:n], gbc[:, o:o + n], ALU.mult, [Bglu, (Bgbc, o)], (BaT, (c, o)))
                for jj in range(4):
                    issue(ui + LOOK)
                    w, Bw = slots.pop(ui)
                    ui += 1
                    for jl in range(2):
                        j = jj * 2 + jl
                        for (o, n) in tiles:
                            bk, Bbk = self.bank()
                            for c in range(8):
                                self.mm(bk[:, 0:n], w[:, c, jl * 128:(jl + 1) * 128], aT[:, c, o:o + n], c == 0, c == 7,
                                        [Bw, (BaT, (c, o))], Bbk)
                            self.tt("dve", fT[:, j, o:o + n], bk[:, 0:n], fT[:, j, o:o + n], ALU.add,
                                    [Bbk, (BfT, (j, o))], (BfT, (j, o)))
            self.st("sp", self.YT[:, s0:s0 + sn].rearrange("(c p) t -> p c t", p=128), fT[:, :, 0:sn], BfT)
        self.phase_end()


def _cols(v, nchunk):
    return np.ascontiguousarray(np.asarray(v, np.float32).reshape(nchunk, 128).T)


def _consts():
    c = {}
    n_freq = 16
    freqs = (np.float32(10000.0) ** (-np.arange(n_freq, dtype=np.float32) / np.float32(n_freq))).astype(np.float32)
    pos = np.arange(L)
    row = (pos // 64).astype(np.float32)
    col = (pos % 64).astype(np.float32)
    ang_r = (row[:, None] * freqs[None, :]).astype(np.float32)
    ang_c = (col[:, None] * freqs[None, :]).astype(np.float32)
    cos = np.ones((64, NT), np.float32)
    sin = np.zeros((64, NT), np.float32)
    for d in range(64):
        ang = ang_r if d < 32 else ang_c
        j = d % 16
        sgn = -1.0 if (d % 32) < 16 else 1.0
        cos[d, :L] = np.cos(ang[:, j])
        sin[d, :L] = sgn * np.sin(ang[:, j])
    c["cos2"] = np.ascontiguousarray(np.concatenate([cos, cos], 0))
    c["sin2"] = np.ascontiguousarray(np.concatenate([sin, sin], 0))
    k = np.arange(128)
    tri = np.zeros((128, 2, 128), np.float32)
    tri[:, 0, :] = (k[:, None] >= k[None, :])
    tri[:, 1, :] = (k[:, None] <= k[None, :])
    c["tri"] = tri
    a = 2.0 * np.pi * np.outer(k, k) / 128.0
    c["w128"] = np.concatenate([np.cos(a), -np.sin(a)], 1).astype(np.float32)
    l2 = np.arange(64)
    k1 = np.arange(128)
    k2 = np.arange(64)
    kk = k1[:, None] + 128 * k2[None, :]
    th = 2.0 * np.pi * (l2[:, None, None] * kk[None, :, :] % L) / L
    fr, fi = np.cos(th), -np.sin(th)
    fab = np.zeros((128, 128, 128), np.float32)
    fab[0:64, :, 0:64] = fr
    fab[0:64, :, 64:128] = fi
    fab[64:128, :, 0:64] = -fi
    fab[64:128, :, 64:128] = fr
    c["fab"] = fab
    n = np.arange(256)
    a = 2.0 * np.pi * (np.outer(n, n) % 256) / 256.0
    C, S = np.cos(a), np.sin(a)
    cs = np.zeros((128, 2, 2, 256), np.float32)
    csn = np.zeros((128, 2, 512), np.float32)
    for kc in range(2):
        cs[:, kc, 0, :] = C[kc * 128:(kc + 1) * 128]
        cs[:, kc, 1, :] = S[kc * 128:(kc + 1) * 128]
        csn[:, kc, 0:256] = C[kc * 128:(kc + 1) * 128]
        csn[:, kc, 256:512] = -S[kc * 128:(kc + 1) * 128]
    c["cs256"] = cs
    c["csn256"] = csn
    sel = np.zeros((NE, NE, 128), np.float32)
    for e in range(NE):
        sel[e, e, :] = 1.0
    c["sel"] = sel
    return c


def _prep_shared(inp):
    f = lambda a: np.asarray(a, np.float32)
    sh = {}
    sh["w_mod"] = f(inp["w_mod"])
    sh["b_mod_c"] = np.stack([_cols(inp["b_mod"][l], 48) for l in range(2)])
    w_in = f(inp["w_in"])
    perm = np.array([(d + 16) if (d % 32) < 16 else (d - 16) for d in range(64)])
    qperm = np.concatenate([h * 64 + perm for h in range(16)])
    kperm = np.concatenate([h * 64 + perm for h in range(4)])
    q, k = w_in[:, :, 0:1024], w_in[:, :, 1024:1280]
    sh["w_in"] = np.ascontiguousarray(np.concatenate(
        [q, q[:, :, qperm], k, k[:, :, kperm], w_in[:, :, 1280:]], axis=2))
    sh["sink64"] = np.ascontiguousarray(np.broadcast_to(f(inp["attn_sink"])[:, None, :], (2, 64, 16)))
    for nm in ("w_o_attn", "w_o_rg", "w_o_four", "w_out", "w_merge"):
        sh[nm] = f(inp[nm])
    sh["b_merge_c"] = np.stack([_cols(inp["b_merge"][l], 24) for l in range(2)])
    rgc = np.zeros((2, 128, 8, 11), np.float32)
    for l in range(2):
        for t in range(4):
            rgc[l, :, :, t] = _cols(inp["conv_w"][l, t], 8)
        rgc[l, :, :, 4] = _cols(inp["conv_b"][l], 8)
        for d in range(2):
            rgc[l, :, :, 5 + d] = _cols(inp["rg_b_a"][l, d], 8)
            rgc[l, :, :, 7 + d] = _cols(inp["rg_b_i"][l, d], 8)
            rgc[l, :, :, 9 + d] = _cols(inp["rg_lambda"][l, d], 8)
    sh["rgcols"] = rgc
    wbd = np.zeros((2, 8, 128, 4, 128), np.float32)
    for l in range(2):
        for gi, nm in enumerate(("rg_w_a", "rg_w_i")):
            w = f(inp[nm])
            for d in range(2):
                for c in range(8):
                    for hb in range(2):
                        wbd[l, c, hb * 64:(hb + 1) * 64, gi * 2 + d, hb * 64:(hb + 1) * 64] = w[l, d, 2 * c + hb]
    sh["rg_wbd"] = wbd
    lnc = np.zeros((2, 128, 4, 8), np.float32)
    for l in range(2):
        for i, nm in enumerate(("ln1_g", "ln1_b", "ln2_g", "ln2_b")):
            lnc[l, :, i, :] = _cols(inp[nm][l], 8)
    sh["lncols"] = lnc
    sh["w_router"] = f(inp["w_router"])
    sh["b_router_bc"] = np.ascontiguousarray(np.broadcast_to(f(inp["b_router"])[:, None, :], (2, 128, NE)))
    wgu = f(inp["w_gate_up"]).reshape(2, NE, D, 2, 8, 128)
    sh["w_gu"] = np.ascontiguousarray(wgu.transpose(0, 1, 4, 2, 3, 5)).reshape(2, NE, 8, D, 256)
    bgu = f(inp["b_gate_up"]).reshape(2, NE, 16, 128)
    sh["b_gu_c"] = np.ascontiguousarray(bgu.transpose(0, 3, 1, 2))
    wdn = f(inp["w_down"]).reshape(2, NE, D, 4, 256)
    sh["w_dn"] = np.ascontiguousarray(wdn.transpose(0, 1, 3, 2, 4))
    sh["b_dn"] = f(inp["b_down"])
    sh.update(_consts())
    return sh


def _core_inputs(inp, b, sh):
    m = dict(sh)
    m["x"] = np.ascontiguousarray(np.asarray(inp["x"][b], np.float32))
    m["ctx"] = np.ascontiguousarray(np.asarray(inp["ctx"][b], np.float32))
    cv = np.stack([np.asarray(inp["c"][b], np.float32), np.asarray(inp["c_ctx"], np.float32)], 0)
    m["cvecT"] = np.ascontiguousarray(cv.reshape(2, 8, 128).transpose(2, 1, 0))
    return m


def kernel(**inputs):
    sh = _prep_shared(inputs)
    nc = K().build()
    in_maps = [_core_inputs(inputs, b, sh) for b in range(8)]
    res = run_bass_kernel_spmd(nc, in_maps, core_ids=list(range(8)))
    return np.stack([np.asarray(r["out"], np.float32) for r in res.results], 0)
```

```python
import math
import os
from contextlib import ExitStack

import numpy as np
import concourse.bass as bass
import concourse.mybir as mybir
from concourse.bass_utils import run_bass_kernel_spmd

F32 = mybir.dt.float32
BF16 = mybir.dt.bfloat16
ALU = mybir.AluOpType
AF = mybir.ActivationFunctionType
AX = mybir.AxisListType

D = 1024
L = 8192
CT = 256
NT = L + CT
NE = 32
ALPHA = 4.0 ** 0.25
LN_EPS = 1e-5
QO, QPO, KO, KPO, VO, XRO, GRO, XFO, WIN = 0, 1024, 2048, 2304, 2560, 2816, 3840, 4864, 5888
ARENA_BYTES = 212000
N_DMA_SEMS = 84


class Buf:
    _n = 0

    def __init__(self, name):
        Buf._n += 1
        self.id = Buf._n
        self.name = name
        self.whole = [dict(), dict()]
        self.parts = {}
        self.phys = None


def _merge(dst, src):
    for k, v in src.items():
        if dst.get(k, 0) < v:
            dst[k] = v


class Prog:
    ENGS = ("pe", "act", "dve", "pool", "sp")

    def __init__(self, nc):
        self.nc = nc
        self.ops = {e: [] for e in self.ENGS}
        self.cnt = {e: 0 for e in self.ENGS}
        self.seen = {e: dict() for e in self.ENGS}
        self.latest = {}
        self.n_ops = 0
        self.phys_free = list(range(N_DMA_SEMS))
        self.phys_cnt = [0] * N_DMA_SEMS
        self.phys_bufs = []

    @staticmethod
    def _norm(x):
        return (x, None) if isinstance(x, Buf) else x

    def _collect(self, reads, writes, skip_dw=False):
        deps = {}

        def mw(src):
            if skip_dw:
                for k, v in src.items():
                    if k[0] != "d" and deps.get(k, 0) < v:
                        deps[k] = v
            else:
                _merge(deps, src)
        for x in reads:
            b, k = self._norm(x)
            _merge(deps, b.whole[0])
            if k is None:
                for p in b.parts.values():
                    _merge(deps, p[0])
            elif k in b.parts:
                _merge(deps, b.parts[k][0])
        for x in writes:
            b, k = self._norm(x)
            mw(b.whole[0])
            _merge(deps, b.whole[1])
            if k is None:
                for p in b.parts.values():
                    mw(p[0])
                    _merge(deps, p[1])
            elif k in b.parts:
                mw(b.parts[k][0])
                _merge(deps, b.parts[k][1])
        return deps

    def _record(self, reads, writes, tok, dma_write=False):
        key, val = tok
        if self.latest.get(key, 0) < val:
            self.latest[key] = val
        for x in reads:
            b, k = self._norm(x)
            tgt = b.whole if k is None else b.parts.setdefault(k, [dict(), dict()])
            if tgt[1].get(key, 0) < val:
                tgt[1][key] = val
        for x in writes:
            b, k = self._norm(x)
            if k is None:
                if dma_write:
                    neww = {kk: vv for kk, vv in b.whole[0].items() if kk[0] == "d"}
                    for p in b.parts.values():
                        for kk, vv in p[0].items():
                            if kk[0] == "d" and neww.get(kk, 0) < vv:
                                neww[kk] = vv
                    neww[key] = max(neww.get(key, 0), val)
                    b.whole = [neww, dict()]
                else:
                    b.whole = [{key: val}, dict()]
                b.parts = {}
            else:
                if dma_write and k in b.parts:
                    neww = {kk: vv for kk, vv in b.parts[k][0].items() if kk[0] == "d"}
                    neww[key] = max(neww.get(key, 0), val)
                    b.parts[k] = [neww, dict()]
                else:
                    b.parts[k] = [{key: val}, dict()]

    def _waits(self, eng, deps):
        out = []
        seen = self.seen[eng]
        for key, val in deps.items():
            if eng == "pe" and key == ("e", "pe"):
                continue
            if seen.get(key, 0) >= val:
                continue
            seen[key] = val
            out.append((key, val))
        return out

    def op(self, eng, fn, reads=(), writes=(), signal=True):
        deps = self._collect(reads, writes)
        waits = self._waits(eng, deps)
        n = self.cnt[eng] + 1
        if signal:
            self.cnt[eng] = n
        tok = (("e", eng), n)
        self._record(reads, writes, tok)
        self.ops[eng].append((fn, waits, tok if signal else None))
        self.n_ops += 1

    def dma(self, q, fn, reads=(), writes=(), sembuf=None):
        if sembuf is None:
            sembuf = self._norm(writes[0])[0] if writes else self._norm(reads[0])[0]
        if sembuf.phys is None:
            sembuf.phys = self.phys_free.pop(0)
            self.phys_bufs.append(sembuf)
        deps = self._collect(reads, writes, skip_dw=True)
        waits = self._waits(q, deps)
        s = sembuf.phys
        self.phys_cnt[s] += 16
        key = ("d", s)
        self._record(reads, writes, (key, self.phys_cnt[s]), dma_write=True)
        self.ops[q].append((fn, waits, (key, 16)))
        self.n_ops += 1

    def barrier(self):
        for e in self.ENGS:
            waits = []
            for k, v in self.latest.items():
                if k == ("e", e):
                    continue
                if self.seen[e].get(k, 0) < v:
                    self.seen[e][k] = v
                    waits.append((k, v))
            if waits:
                self.ops[e].append((None, waits, None))
        for b in self.phys_bufs:
            self.phys_free.append(b.phys)
            b.phys = None
        self.phys_bufs = []

    def emit(self, stack):
        nc = self.nc
        sems = {}
        for e in self.ENGS:
            sems[("e", e)] = stack.enter_context(nc.semaphore("se_" + e))
        for i in range(N_DMA_SEMS):
            if self.phys_cnt[i] > 0:
                sems[("d", i)] = stack.enter_context(nc.semaphore("sd_%d" % i))
        block = stack.enter_context(nc.Block())
        prog = self

        def run(ename, eng):
            for fn, waits, inc in prog.ops[ename]:
                for k, v in waits:
                    eng.wait_ge(sems[k], v)
                if fn is None:
                    continue
                ins = fn(eng)
                if inc is not None:
                    k, v = inc
                    ins.then_inc(sems[k], 1 if k[0] == "e" else 16)

        @block.tensor
        def _(e):
            run("pe", e)

        @block.scalar
        def _(e):
            run("act", e)

        @block.vector
        def _(e):
            run("dve", e)

        @block.gpsimd
        def _(e):
            run("pool", e)

        @block.sync
        def _(e):
            run("sp", e)


class K:
    def __init__(self, taps=(), layers=(0, 1), phases=None):
        self.taps = set(taps)
        self.layers = layers
        self.phases = phases
        self.nc = bass.Bass("TRN2", target_bir_lowering=False)
        self.P = Prog(self.nc)
        self.top = 0
        self.bank_rr = 0
        self.rr = 0

    def din(self, name, shape, dt=F32):
        return self.nc.dram_tensor(name, list(shape), dt, kind="ExternalInput").ap()

    def dscr(self, name, shape, dt):
        kind = "ExternalOutput" if name in self.taps else "Internal"
        return self.nc.dram_tensor(name, list(shape), dt, kind=kind).ap()

    def alloc(self, shape, dt, name="t"):
        esz = 4 if dt == F32 else 2
        n = 1
        for s in shape[1:]:
            n *= s
        nbytes = n * esz
        off = self.top
        self.top += (nbytes + 63) // 64 * 64
        assert self.top <= ARENA_BYTES, ("SBUF arena overflow", name, self.top)
        v = self.arena[:, off // 2:(off + nbytes) // 2]
        if dt == F32:
            v = v.bitcast(F32)
        if len(shape) == 3:
            v = v.rearrange("p (a b) -> p a b", a=shape[1])
        elif len(shape) == 4:
            v = v.rearrange("p (a b c) -> p a b c", a=shape[1], b=shape[2])
        if shape[0] < 128:
            v = v[0:shape[0]]
        return v, Buf(name)

    def bank(self):
        i = self.bank_rr
        self.bank_rr = (i + 1) % 8
        return self.ps[:, i * 512:(i + 1) * 512], self.BPS[i]

    def op(self, eng, fn, reads=(), writes=(), signal=True):
        self.P.op(eng, fn, reads, writes, signal)

    def ld(self, q, out, in_, wbuf, rbufs=()):
        self.P.dma(q, lambda e: e.dma_start(out=out, in_=in_), reads=list(rbufs), writes=[wbuf])

    def st(self, q, out, in_, rbuf):
        self.P.dma(q, lambda e: e.dma_start(out=out, in_=in_), reads=[rbuf], writes=[])

    def mm(self, out, lhsT, rhs, start, stop, reads, wbuf, signal=None):
        self.P.op("pe", lambda e: e.matmul(out=out, lhsT=lhsT, rhs=rhs, start=start, stop=stop),
                  reads=reads, writes=[wbuf], signal=(stop if signal is None else signal))

    def act(self, out, in_, func, reads, wbuf, scale=1.0, bias=0.0):
        if func == AF.Copy and not (isinstance(scale, float) and scale == 1.0):
            func = AF.Identity
        self.P.op("act", lambda e: e.activation(out=out, in_=in_, func=func, scale=scale, bias=bias),
                  reads=reads, writes=[wbuf])

    def tt(self, eng, out, in0, in1, op, reads, wbuf):
        self.P.op(eng, lambda e: e.tensor_tensor(out=out, in0=in0, in1=in1, op=op), reads=reads, writes=[wbuf])

    def ts(self, eng, out, in0, s1, s2, op0, op1, reads, wbuf):
        if s2 is None:
            self.P.op(eng, lambda e: e.tensor_scalar(out=out, in0=in0, scalar1=s1, scalar2=None, op0=op0),
                      reads=reads, writes=[wbuf])
        else:
            self.P.op(eng, lambda e: e.tensor_scalar(out=out, in0=in0, scalar1=s1, scalar2=s2, op0=op0, op1=op1),
                      reads=reads, writes=[wbuf])

    def phase_begin(self):
        self.top = self.persist_top

    def phase_end(self):
        self.P.barrier()

    def want(self, name):
        return self.phases is None or name in self.phases

    def build(self):
        nc = self.nc
        st = ExitStack()
        with st:
            self.arena = st.enter_context(nc.sbuf_tensor("arena", [128, ARENA_BYTES // 2], BF16))
            self.ps = st.enter_context(nc.psum_tensor("psum", [128, 4096], F32))
            self.BPS = [Buf("ps%d" % i) for i in range(8)]
            self.declare_io()
            self.setup_persist()
            if self.want("tin"):
                self.phase_tin()
            for l in self.layers:
                last = (l == 1)
                if self.want("mods"):
                    self.phase_mods(l)
                if self.want("proj"):
                    self.phase_proj(l, last)
                if self.want("rg"):
                    self.phase_rg(l, last)
                if self.want("att"):
                    self.phase_att(l, last)
                if self.want("four"):
                    self.phase_four(l, last)
                if self.want("merge"):
                    self.phase_merge(l, last)
                if self.want("ln1"):
                    self.phase_ln(l, last, first=True)
                if self.want("moe"):
                    self.phase_moe(l, last)
                if self.want("ln2"):
                    self.phase_ln(l, last, first=False)
            self.P.barrier()
            self.P.emit(st)
        return nc

    def declare_io(self):
        d = self.din
        self.x_in = d("x", [L, D])
        self.ctx_in = d("ctx", [CT, D])
        self.cvecT = d("cvecT", [128, 8, 2])
        self.w_mod = d("w_mod", [2, D, 6 * D])
        self.b_mod_c = d("b_mod_c", [2, 128, 48])
        self.w_in = d("w_in", [2, D, WIN])
        self.sink64 = d("sink64", [2, 64, 16])
        self.w_o_attn = d("w_o_attn", [2, D, D])
        self.w_o_rg = d("w_o_rg", [2, D, D])
        self.w_o_four = d("w_o_four", [2, D, D])
        self.w_out = d("w_out", [2, D, D])
        self.w_merge = d("w_merge", [2, D, 3 * D])
        self.b_merge_c = d("b_merge_c", [2, 128, 24])
        self.rgcols = d("rgcols", [2, 128, 8, 11])
        self.rg_wbd = d("rg_wbd", [2, 8, 128, 4, 128])
        self.lncols = d("lncols", [2, 128, 4, 8])
        self.w_router = d("w_router", [2, D, NE])
        self.b_router_bc = d("b_router_bc", [2, 128, NE])
        big = self.want("moe")
        self.w_gu = d("w_gu", [2, NE, 8, D, 256] if big else [1, 1, 1, 128, 256])
        self.b_gu_c = d("b_gu_c", [2, 128, NE, 16])
        self.w_dn = d("w_dn", [2, NE, 4, D, 256] if big else [1, 1, 1, 128, 256])
        self.b_dn = d("b_dn", [2, NE, D])
        self.cos2 = d("cos2", [128, NT])
        self.sin2 = d("sin2", [128, NT])
        self.tri = d("tri", [128, 2, 128])
        self.w128 = d("w128", [128, 256])
        self.fab = d("fab", [128, 128, 128])
        self.cs256 = d("cs256", [128, 2, 2, 256])
        self.csn256 = d("csn256", [128, 2, 512])
        self.sel = d("sel", [NE, NE, 128])
        self.out = self.nc.dram_tensor("out", [L, D], F32, kind="ExternalOutput").ap()
        s = self.dscr
        self.XT = s("XT", [D, NT], F32)
        self.X1T = s("X1T", [D, NT], F32)
        self.YT = s("YT", [D, NT], F32)
        self.UT = s("UT", [D, NT], BF16)
        self.QT = s("QT", [D, NT], BF16)
        self.KT = s("KT", [256, NT], BF16)
        self.V = s("V", [NT, 256], BF16)
        self.XRT = s("XRT", [D, NT], F32)
        self.GRT = s("GRT", [D, NT], BF16)
        self.XF = s("XF", [NT, D], BF16)
        self.AT = s("AT", [D, NT], BF16)
        self.RT = s("RT", [D, NT], BF16)
        self.FT = s("FT", [D, NT], BF16)
        self.U2T = s("U2T", [D, NT], BF16)
        self.GT = s("GT", [NE, NT], BF16)
        if "MODC" in self.taps:
            self.MODC = s("MODC", [128, 2, 48], F32)

    def setup_persist(self):
        self.ident, self.Bident = self.alloc([128, 128], F32, "ident")
        self.ones_s, self.Bones_s = self.alloc([128, 128], F32, "ones_s")
        self.ones_b, self.Bones_b = self.alloc([128, 64], BF16, "ones_b")
        self.modc, self.Bmodc = self.alloc([128, 2, 48], F32, "modc")
        ident, ones_s, ones_b = self.ident, self.ones_s, self.ones_b
        self.op("pool", lambda e: e.memset(ident, 0.0), writes=[self.Bident])
        self.op("pool", lambda e: e.affine_select(out=ident, in_=ident, compare_op=ALU.not_equal, fill=1.0,
                                                  base=0, pattern=[[-1, 128]], channel_multiplier=1),
                reads=[self.Bident], writes=[self.Bident])
        self.op("pool", lambda e: e.memset(ones_s, 1.0 / D), writes=[self.Bones_s])
        self.op("pool", lambda e: e.memset(ones_b, 1.0), writes=[self.Bones_b])
        self.persist_top = self.top

    def phase_tin(self):
        self.phase_begin()
        xin = [self.alloc([128, D], F32, "xin%d" % i) for i in range(4)]
        xT = [self.alloc([128, 8, 512], F32, "xT%d" % i) for i in range(2)]
        for g in range(17):
            t0 = g * 512
            n = 512 if g < 16 else CT
            xt, Bxt = xT[g % 2]
            for s in range(n // 128):
                xi, Bxi = xin[s]
                src = self.x_in[t0 + s * 128:t0 + (s + 1) * 128, :] if g < 16 else self.ctx_in[s * 128:(s + 1) * 128, :]
                self.ld("sp", xi, src, Bxi)
                for half in range(2):
                    bk, Bbk = self.bank()
                    for i in range(4):
                        c = half * 4 + i
                        self.op("pe", lambda e, bk=bk, xi=xi, i=i, c=c: e.transpose(
                            out=bk[:, i * 128:(i + 1) * 128], in_=xi[:, c * 128:(c + 1) * 128], identity=self.ident),
                            reads=[Bxi, self.Bident], writes=[Bbk], signal=(i == 3))
                    o = xt[:, half * 4:(half + 1) * 4, s * 128:(s + 1) * 128]
                    i3 = bk.rearrange("p (a b) -> p a b", a=4)
                    if half == 0:
                        self.op("act", lambda e, o=o, i3=i3: e.copy(out=o, in_=i3), reads=[Bbk], writes=[(Bxt, (s, half))])
                    else:
                        self.op("dve", lambda e, o=o, i3=i3: e.tensor_copy(out=o, in_=i3), reads=[Bbk], writes=[(Bxt, (s, half))])
            self.st("sp", self.XT[:, t0:t0 + n].rearrange("(c p) t -> p c t", p=128), xt[:, :, 0:n], Bxt)
        self.phase_end()

    def phase_mods(self, l):
        self.phase_begin()
        scv, Bscv = self.alloc([128, 8, 2], F32, "scv")
        bmc, Bbmc = self.alloc([128, 48], F32, "bmc")
        wm = [self.alloc([128, 8, 512], F32, "wm%d" % i) for i in range(2)]
        self.ld("sp", scv, self.cvecT, Bscv)
        self.ld("sp", bmc, self.b_mod_c[l], Bbmc)
        self.act(scv, scv, AF.Silu, [Bscv], Bscv)
        bk, Bbk = self.bank()
        for n in range(12):
            w, Bw = wm[n % 2]
            self.ld("sp", w, self.w_mod[l][:, n * 512:(n + 1) * 512].rearrange("(k p) n -> p k n", p=128), Bw)
            for jj in range(4):
                j = n * 4 + jj
                for k in range(8):
                    self.mm(bk[:, 2 * j:2 * j + 2], w[:, k, jj * 128:(jj + 1) * 128], scv[:, k, :],
                            k == 0, k == 7, [Bw, Bscv], Bbk)
        pv = bk[:, 0:96].rearrange("p (j v) -> p v j", v=2)
        for v in range(2):
            self.tt("dve", self.modc[:, v, :], pv[:, v, :], bmc, ALU.add, [Bbk, Bbmc], (self.Bmodc, v))
        for lo in (8, 32):
            self.ts("dve", self.modc[:, :, lo:lo + 8], self.modc[:, :, lo:lo + 8], 1.0, None, ALU.add, None,
                    [self.Bmodc], self.Bmodc)
        if "MODC" in self.taps:
            self.st("sp", self.MODC, self.modc, self.Bmodc)
        self.phase_end()

    def mcol(self, which, c, v):
        j = which * 8 + c
        return self.modc[:, v, j:j + 1]

    def phase_proj(self, l, last):
        self.phase_begin()
        win, Bwin = self.alloc([128, 8, WIN], BF16, "win")
        for k in range(8):
            self.ld("pool", win[:, k, :], self.w_in[l][k * 128:(k + 1) * 128, :], (Bwin, k))
        xT = [self.alloc([128, 8, 512], F32, "xT%d" % i) for i in range(2)]
        cs = [self.alloc([128, 2, 512], F32, "cs%d" % i) for i in range(2)]
        uT = [self.alloc([128, 8, 512], BF16, "uT%d" % i) for i in range(2)]
        sf = [self.alloc([128, 512], F32, "sf%d" % i) for i in range(6)]
        sb = [self.alloc([128, 512], BF16, "sb%d" % i) for i in range(8)]
        sx = [self.alloc([128, 1024], BF16, "sx%d" % i) for i in range(2)]
        rt = [self.alloc([128, 2, 512], F32, "rt%d" % i) for i in range(2)]
        nsf = nsb = nsx = nrt = 0
        ntile = 17
        for g in range(ntile):
            t0 = g * 512
            n = 512 if g < 16 else CT
            v = 0 if g < 16 else 1
            x, Bx = xT[g % 2]
            c_, Bc_ = cs[g % 2]
            u, Bu = uT[g % 2]
            self.ld("sp", x[:, :, 0:n], self.XT[:, t0:t0 + n].rearrange("(c p) t -> p c t", p=128), Bx)
            self.ld("sp", c_[:, 0, 0:n], self.cos2[:, t0:t0 + n], (Bc_, 0))
            self.ld("sp", c_[:, 1, 0:n], self.sin2[:, t0:t0 + n], (Bc_, 1))
            for c in range(8):
                self.act(u[:, c, 0:n], x[:, c, 0:n], AF.Identity, [Bx, self.Bmodc], (Bu, c),
                         scale=self.mcol(1, c, v), bias=self.mcol(0, c, v))
            self.st("sp", self.UT[:, t0:t0 + n].rearrange("(c p) t -> p c t", p=128), u[:, :, 0:n], Bu)

            def proj_fm(col0):
                bk, Bbk = self.bank()
                for k in range(8):
                    self.mm(bk[:, 0:n], win[:, k, col0:col0 + 128], u[:, k, 0:n], k == 0, k == 7, [Bwin, Bu], Bbk)
                return bk, Bbk
            for (base, pbase, nch, dst) in ((QO, QPO, 8, self.QT), (KO, KPO, 2, self.KT)):
                if last and g == 16 and dst is self.QT:
                    continue
                for c in range(nch):
                    bA, BA = proj_fm(base + c * 128)
                    bB, BB = proj_fm(pbase + c * 128)
                    r, Br = rt[nrt % 2]
                    nrt += 1
                    o, Bo = sb[nsb % 8]
                    nsb += 1
                    self.tt("dve", r[:, 0, 0:n], bA[:, 0:n], c_[:, 0, 0:n], ALU.mult, [BA, Bc_], (Br, 0))
                    self.tt("dve", r[:, 1, 0:n], bB[:, 0:n], c_[:, 1, 0:n], ALU.mult, [BB, Bc_], (Br, 1))
                    self.tt("pool", o[:, 0:n], r[:, 0, 0:n], r[:, 1, 0:n], ALU.add, [Br], Bo)
                    self.st("sp", dst[c * 128:(c + 1) * 128, t0:t0 + n], o[:, 0:n], Bo)
            for c in range(8):
                bk, Bbk = proj_fm(XRO + c * 128)
                o, Bo = sf[nsf % 6]
                nsf += 1
                self.act(o[:, 0:n], bk[:, 0:n], AF.Copy, [Bbk], Bo)
                self.st("sp", self.XRT[c * 128:(c + 1) * 128, t0:t0 + n], o[:, 0:n], Bo)
            if not (last and g == 16):
                for c in range(8):
                    bk, Bbk = proj_fm(GRO + c * 128)
                    o, Bo = sb[nsb % 8]
                    nsb += 1
                    self.act(o[:, 0:n], bk[:, 0:n], AF.Copy, [Bbk], Bo)
                    self.st("sp", self.GRT[c * 128:(c + 1) * 128, t0:t0 + n], o[:, 0:n], Bo)
            for s in range(n // 128):
                bk, Bbk = self.bank()
                for k in range(8):
                    self.mm(bk[:, 0:256], u[:, k, s * 128:(s + 1) * 128], win[:, k, VO:VO + 256], k == 0, k == 7,
                            [Bwin, Bu], Bbk)
                o, Bo = sb[nsb % 8]
                nsb += 1
                self.op("dve", lambda e, o=o, bk=bk: e.tensor_copy(out=o[:, 0:256], in_=bk[:, 0:256]), reads=[Bbk], writes=[Bo])
                self.st("sp", self.V[t0 + s * 128:t0 + (s + 1) * 128, :], o[:, 0:256], Bo)
                if last and g == 16:
                    continue
                o, Bo = sx[nsx % 2]
                nsx += 1
                for h in range(2):
                    bk, Bbk = self.bank()
                    for k in range(8):
                        self.mm(bk, u[:, k, s * 128:(s + 1) * 128], win[:, k, XFO + h * 512:XFO + (h + 1) * 512],
                                k == 0, k == 7, [Bwin, Bu], Bbk)
                    if h == 0:
                        self.act(o[:, 0:512], bk, AF.Copy, [Bbk], (Bo, 0))
                    else:
                        self.op("dve", lambda e, o=o, bk=bk: e.tensor_copy(out=o[:, 512:1024], in_=bk), reads=[Bbk], writes=[(Bo, 1)])
                self.st("sp", self.XF[t0 + s * 128:t0 + (s + 1) * 128, :], o, Bo)
        self.phase_end()

    def phase_rg(self, l, last):
        self.phase_begin()
        XA, BXA = self.alloc([128, NT], F32, "XA")
        xc, Bxc = self.alloc([128, NT], F32, "xc")
        xcb, Bxcb = self.alloc([128, NT], BF16, "xcb")
        Bc, BBc = self.alloc([128, NT], F32, "Bc")
        H, BH = self.alloc([128, NT], F32, "H")
        tmp = [self.alloc([128, 1024], F32, "rgt%d" % i) for i in range(5)]
        ge = [self.alloc([128, 1024], F32, "ge%d" % i) for i in range(3)]
        grt = [self.alloc([128, 1024], BF16, "grt%d" % i) for i in range(2)]
        ot = [self.alloc([128, 1024], BF16, "rot%d" % i) for i in range(2)]
        wbd = [self.alloc([128, 4, 128], BF16, "wbd%d" % i) for i in range(2)]
        cols, Bcols = self.alloc([128, 8, 11], F32, "rgcols")
        cvec, Bcvec = self.alloc([128, 8, 2], F32, "rgc")
        ct, Bct = self.alloc([128, 8, 2], F32, "rgct")
        self.ld("sp", cols, self.rgcols[l], Bcols)
        lam = cols[:, :, 9:11]
        self.act(cvec, lam, AF.Exp, [Bcols], Bcvec, scale=-1.0)
        self.ts("dve", ct, cvec, -1.0 / 3.0, 0.5, ALU.mult, ALU.add, [Bcvec], Bct)
        self.tt("dve", ct, ct, cvec, ALU.mult, [Bct, Bcvec], Bct)
        self.ts("dve", ct, ct, -1.0, 1.0, ALU.mult, ALU.add, [Bct], Bct)
        self.tt("dve", ct, ct, cvec, ALU.mult, [Bct, Bcvec], Bct)
        self.ts("dve", cvec, ct, -8.0, None, ALU.mult, None, [Bct], Bcvec)
        segs = ((0, L), (L, NT))
        groups = [(i * 1024, 1024) for i in range(8)] + [(L, CT)]

        def rev(ap2, a, b):
            base = ap2[:, a:b]
            pstep = base.ap[0][0]
            return bass.AP(base.tensor, base.offset + (b - a - 1), [[pstep, 128], [-1, b - a]])
        for c in range(8):
            w, Bw = wbd[c % 2]
            self.ld("pool", w, self.rg_wbd[l, c], Bw)
            self.ld("sp", XA, self.XRT[c * 128:(c + 1) * 128, :], BXA)
            cw = lambda i: cols[:, c, i:i + 1]
            for (a, b) in segs:
                self.ts("dve", xc[:, a:b], XA[:, a:b], cw(2), cw(4), ALU.mult, ALU.add, [BXA, Bcols], Bxc)
                for (tap, so, do) in ((1, 0, 1), (0, 0, 2), (3, 1, 0)):
                    nn = (b - a) - max(so, do)
                    wtap = cw(tap)
                    self.op("dve", lambda e, a=a, so=so, do=do, nn=nn, wtap=wtap: e.scalar_tensor_tensor(
                        out=xc[:, a + do:a + do + nn], in0=XA[:, a + so:a + so + nn], scalar=wtap,
                        in1=xc[:, a + do:a + do + nn], op0=ALU.mult, op1=ALU.add),
                        reads=[BXA, Bcols, Bxc], writes=[Bxc])
            self.act(xcb, xc, AF.Copy, [Bxc], Bxcb)
            for d in range(2):
                A, BA = XA, BXA
                for (g0, gn) in groups:
                    R, BR = tmp[0]
                    IG, BIG = tmp[1]
                    for h in range(gn // 512 if gn >= 512 else 1):
                        hn = min(512, gn)
                        o0 = g0 + h * 512
                        bk, Bbk = self.bank()
                        self.mm(bk[:, 0:hn], w[:, d, :], xcb[:, o0:o0 + hn], True, True, [Bw, Bxcb], Bbk)
                        self.act(R[:, h * 512:h * 512 + hn], bk[:, 0:hn], AF.Sigmoid, [Bbk, Bcols], (BR, h),
                                 bias=cols[:, c, 5 + d:6 + d])
                        bk, Bbk = self.bank()
                        self.mm(bk[:, 0:hn], w[:, 2 + d, :], xcb[:, o0:o0 + hn], True, True, [Bw, Bxcb], Bbk)
                        self.act(IG[:, h * 512:h * 512 + hn], bk[:, 0:hn], AF.Sigmoid, [Bbk, Bcols], (BIG, h),
                                 bias=cols[:, c, 7 + d:8 + d])
                    M, BM = tmp[2]
                    S, BS = tmp[3]
                    GX, BGX = tmp[4]
                    self.act(A[:, g0:g0 + gn], R[:, 0:gn], AF.Exp, [BR, Bcvec], BA, scale=cvec[:, c, d:d + 1])
                    self.tt("pool", M[:, 0:gn], A[:, g0:g0 + gn], A[:, g0:g0 + gn], ALU.mult, [BA], BM)
                    self.act(S[:, 0:gn], M[:, 0:gn], AF.Sqrt, [BM], BS, scale=-1.0, bias=1.0)
                    self.tt("pool", GX[:, 0:gn], IG[:, 0:gn], xc[:, g0:g0 + gn], ALU.mult, [BIG, Bxc], BGX)
                    self.tt("dve", Bc[:, g0:g0 + gn], S[:, 0:gn], GX[:, 0:gn], ALU.mult, [BS, BGX], BBc)
                dst, Bdst = (H, BH) if d == 0 else (Bc, BBc)
                if d == 0:
                    self.op("dve", lambda e: e.tensor_tensor_scan(out=H[:, L:NT], data0=A[:, L:NT], data1=Bc[:, L:NT],
                                                                 initial=0.0, op0=ALU.mult, op1=ALU.add),
                            reads=[BA, BBc], writes=[BH])
                    prev = H[:, NT - 1:NT]
                    for i in range(4):
                        a, b = i * 2048, (i + 1) * 2048
                        self.op("dve", lambda e, a=a, b=b, prev=prev: e.tensor_tensor_scan(
                            out=H[:, a:b], data0=A[:, a:b], data1=Bc[:, a:b], initial=prev, op0=ALU.mult, op1=ALU.add),
                            reads=[BA, BBc, BH], writes=[BH])
                        prev = H[:, b - 1:b]
                else:
                    self.op("dve", lambda e: e.tensor_tensor_scan(out=rev(Bc, L, NT), data0=rev(A, L, NT), data1=rev(Bc, L, NT),
                                                                 initial=0.0, op0=ALU.mult, op1=ALU.add),
                            reads=[BA, BBc], writes=[BBc])
                    prev = Bc[:, L:L + 1]
                    for i in range(3, -1, -1):
                        a, b = i * 2048, (i + 1) * 2048
                        self.op("dve", lambda e, a=a, b=b, prev=prev: e.tensor_tensor_scan(
                            out=rev(Bc, a, b), data0=rev(A, a, b), data1=rev(Bc, a, b), initial=prev,
                            op0=ALU.mult, op1=ALU.add), reads=[BA, BBc], writes=[BBc])
                        prev = Bc[:, a:a + 1]
            for gi, (g0, gn) in enumerate(groups):
                if last and g0 == L:
                    continue
                gr, Bgr = grt[gi % 2]
                o, Bo = ot[gi % 2]
                self.ld("sp", gr[:, 0:gn], self.GRT[c * 128:(c + 1) * 128, g0:g0 + gn], Bgr)
                t0_, B0 = ge[0]
                t1_, B1 = ge[1]
                t2_, B2 = ge[2]
                self.tt("pool", t0_[:, 0:gn], gr[:, 0:gn], gr[:, 0:gn], ALU.mult, [Bgr], B0)
                self.ts("dve", t0_[:, 0:gn], t0_[:, 0:gn], 0.044715, 1.0, ALU.mult, ALU.add, [B0], B0)
                self.tt("pool", t0_[:, 0:gn], t0_[:, 0:gn], gr[:, 0:gn], ALU.mult, [B0, Bgr], B0)
                self.act(t1_[:, 0:gn], t0_[:, 0:gn], AF.Sigmoid, [B0], B1, scale=2.0 * math.sqrt(2.0 / math.pi))
                self.tt("pool", t1_[:, 0:gn], t1_[:, 0:gn], gr[:, 0:gn], ALU.mult, [B1, Bgr], B1)
                self.tt("pool", t2_[:, 0:gn], H[:, g0:g0 + gn], Bc[:, g0:g0 + gn], ALU.add, [BH, BBc], B2)
                self.tt("dve", o[:, 0:gn], t1_[:, 0:gn], t2_[:, 0:gn], ALU.mult, [B1, B2], Bo)
                self.st("sp", self.RT[c * 128:(c + 1) * 128, g0:g0 + gn], o[:, 0:gn], Bo)
        self.phase_end()

    def phase_att(self, l, last):
        self.phase_begin()
        KTs, BKT = self.alloc([64, 4, NT], BF16, "KTs")
        Vs, BV = self.alloc([128, 66, 256], BF16, "Vs")
        tri, Btri = self.alloc([128, 2, 128], BF16, "tri")
        sk, Bsk = self.alloc([64, 16], F32, "sk")
        Qs = [self.alloc([64, 16, 512], BF16, "Qs%d" % i) for i in range(2)]
        PT = [self.alloc([128, 512], BF16, "PT%d" % i) for i in range(12)]
        OT = [self.alloc([64, 16, 128], BF16, "OT%d" % i) for i in range(2)]
        rc = [self.alloc([64, 512], F32, "rc%d" % i) for i in range(2)]
        self.ld("sp", KTs, self.KT.rearrange("(g d) t -> d g t", d=64), BKT)
        vsrc = self.V.rearrange("(b p) c -> p b c", p=128)
        for i in range(6):
            self.ld("sp", Vs[:, i * 11:(i + 1) * 11, :], vsrc[:, i * 11:(i + 1) * 11, :], BV)
        self.ld("pool", tri, self.tri, Btri)
        self.ld("sp", sk, self.sink64[l], Bsk)
        self.act(sk, sk, AF.Exp, [Bsk], Bsk)
        npt = 0
        nblk = 64 if last else 66
        for n in range(nblk):
            if n % 4 == 0:
                q, Bq = Qs[(n // 4) % 2]
                nq = min(512, (nblk - n) * 128)
                self.ld("sp", q[:, :, 0:nq], self.QT[:, n * 128:n * 128 + nq].rearrange("(h d) t -> d h t", d=64), Bq)
            qo = (n % 4) * 128
            if n < 64:
                kbs = [(kb, m) for (kb, m) in ((n - 1, 0), (n, None), (n + 1, 1)) if 0 <= kb < 64] + [(64, None), (65, None)]
            else:
                kbs = [(64, None), (65, None)]
            o, Bo = OT[n % 2]
            for g in range(4):
                pts = []
                for (kb, m) in kbs:
                    bk, Bbk = self.bank()
                    self.mm(bk.rearrange("p (h q) -> p h q", h=4), KTs[:, g, kb * 128:(kb + 1) * 128],
                            q[:, 4 * g:4 * g + 4, qo:qo + 128], True, True, [BKT, Bq], Bbk)
                    p, Bp = PT[npt % 12]
                    npt += 1
                    self.act(p, bk, AF.Exp, [Bbk], Bp, scale=0.125)
                    if m is not None:
                        p3 = p.rearrange("p (h q) -> p h q", h=4)
                        msk = tri[:, m:m + 1, :].to_broadcast([128, 4, 128])
                        self.tt("pool", p3, p3, msk, ALU.mult, [Bp, Btri], Bp)
                    pts.append((p, Bp, kb))
                bn, Bbn = self.bank()
                bd, Bbd = self.bank()
                for i, (p, Bp, kb) in enumerate(pts):
                    self.mm(bn[0:64, :], Vs[:, kb, g * 64:(g + 1) * 64], p, i == 0, i == len(pts) - 1, [BV, Bp], Bbn)
                for i, (p, Bp, kb) in enumerate(pts):
                    self.mm(bd[0:64, :], self.ones_b, p, i == 0, i == len(pts) - 1, [self.Bones_b, Bp], Bbd)
                r, Br = rc[g % 2]
                r3 = r.rearrange("p (h q) -> p h q", h=4)
                self.tt("dve", r3, bd[0:64, :].rearrange("p (h q) -> p h q", h=4),
                        sk[:, 4 * g:4 * g + 4].unsqueeze(2).to_broadcast([64, 4, 128]), ALU.add, [Bbd, Bsk], Br)
                self.op("dve", lambda e, r=r: e.reciprocal(out=r, in_=r), reads=[Br], writes=[Br])
                self.tt("dve", o[:, 4 * g:4 * g + 4, :], bn[0:64, :].rearrange("p (h q) -> p h q", h=4), r3, ALU.mult,
                        [Bbn, Br], (Bo, g))
            self.st("sp", self.AT[:, n * 128:(n + 1) * 128].rearrange("(h d) t -> d h t", d=64), o, Bo)
        self.phase_end()

    def phase_four(self, l, last):
        self.phase_begin()
        SC = 1.0 / math.sqrt(L * 256.0)
        SCC = 1.0 / math.sqrt(CT * 256.0)
        w128, Bw128 = self.alloc([128, 256], BF16, "w128")
        fab, Bfab = self.alloc([128, 128, 128], BF16, "fab")
        csm, Bcsm = self.alloc([128, 2, 2, 256], BF16, "csm")
        Xr = [self.alloc([128, 64, 128], BF16, "Xr%d" % i) for i in range(2)]
        Bst, BBst = self.alloc([128, 128, 128], BF16, "Bst")
        PQ = [self.alloc([128, 2, L], BF16, "PQ%d" % i) for i in range(2)]
        yo = [self.alloc([128, 512], BF16, "fyo%d" % i) for i in range(4)]
        self.ld("pool", w128, self.w128, Bw128)
        for i in range(4):
            self.ld("pool", fab[:, i * 32:(i + 1) * 32, :], self.fab[:, i * 32:(i + 1) * 32, :], Bfab)
        self.ld("pool", csm, self.cs256, Bcsm)
        nyo = 0
        FSTOP = int(os.environ.get("FOUR_STOP", "9"))

        def chan_stage(grp, pq, t0, n, src_off, scale):
            nonlocal nyo
            for cp in range(2):
                bk, Bbk = self.bank()
                i = 0
                for j in range(2):
                    for pqi in range(2):
                        p_, Bp_ = pq[j]
                        self.mm(bk[:, 0:n], csm[:, j, pqi, cp * 128:(cp + 1) * 128], p_[:, pqi, src_off:src_off + n],
                                i == 0, i == 3, [Bcsm, Bp_], Bbk)
                        i += 1
                o, Bo = yo[nyo % 4]
                nyo += 1
                self.act(o[:, 0:n], bk[:, 0:n], AF.Copy, [Bbk], Bo, scale=scale)
                row = (2 * grp + cp) * 128
                self.st("sp", self.FT[row:row + 128, t0:t0 + n], o[:, 0:n], Bo)
        for cc in range(8):
            xr, Bxr = Xr[cc % 2]
            pq, Bpq = PQ[cc % 2]
            xsrc = self.XF[0:L, cc * 128:(cc + 1) * 128].rearrange("(a b) c -> a b c", b=64)
            for i in range(8):
                self.ld("sp", xr[:, i * 8:(i + 1) * 8, :], xsrc[:, i * 8:(i + 1) * 8, :], Bxr)
            for cb in range(32):
                bk, Bbk = self.bank()
                for ci in range(4):
                    ch = cb * 4 + ci
                    lhsT = xr[:, :, ch]
                    self.mm(bk[0:64, ci * 128:(ci + 1) * 128], lhsT, w128[:, 0:128], True, True, [Bxr, Bw128], Bbk, signal=False)
                    self.mm(bk[64:128, ci * 128:(ci + 1) * 128], lhsT, w128[:, 128:256], True, True, [Bxr, Bw128], Bbk,
                            signal=(ci == 3))
                src = bk.rearrange("p (c k) -> p c k", c=4)
                dstv = Bst[:, :, cb * 4:cb * 4 + 4].rearrange("p k c -> p c k")
                if cb % 2 == 0:
                    self.op("act", lambda e, dstv=dstv, src=src: e.copy(out=dstv, in_=src), reads=[Bbk], writes=[(BBst, cb)])
                else:
                    self.op("dve", lambda e, dstv=dstv, src=src: e.tensor_copy(out=dstv, in_=src), reads=[Bbk], writes=[(BBst, cb)])
            for kg in range(8 if FSTOP >= 2 else 0):
                banks = [self.bank() for _ in range(4)]
                for ki in range(16):
                    k1 = kg * 16 + ki
                    bk, Bbk = banks[ki // 4]
                    self.mm(bk[:, (ki % 4) * 128:(ki % 4) * 128 + 128], Bst[:, k1, :], fab[:, k1, :], True, True,
                            [BBst, Bfab], Bbk, signal=(ki % 4 == 3))
                for bi in range(4 if os.environ.get("FOUR_NOEVAC") is None else 0):
                    bk, Bbk = banks[bi]
                    for pqi in range(2):
                        src = bk.rearrange("p (k r q) -> p k r q", k=4, r=2)[:, :, pqi, :]
                        k10 = kg * 16 + bi * 4
                        dbase = pq[:, pqi, :]
                        pstep = dbase.ap[0][0]
                        dstv = bass.AP(dbase.tensor, dbase.offset + k10, [[pstep, 128], [1, 4], [128, 64]])
                        if bi % 2 == 0:
                            self.op("act", lambda e, dstv=dstv, src=src: e.copy(out=dstv, in_=src), reads=[Bbk],
                                    writes=[(Bpq, (kg, bi, pqi))])
                        else:
                            self.op("dve", lambda e, dstv=dstv, src=src: e.tensor_copy(out=dstv, in_=src), reads=[Bbk],
                                    writes=[(Bpq, (kg, bi, pqi))])
            if cc % 2 == 1 and FSTOP >= 3:
                for g in range(16):
                    chan_stage(cc // 2, [PQ[0], PQ[1]], g * 512, 512, g * 512, SC)
        if not last and FSTOP >= 4:
            csn, Bcsn = self.alloc([128, 2, 512], BF16, "csn")
            self.ld("pool", csn, self.csn256, Bcsn)
            xct, Bxct = self.alloc([128, 2, D], BF16, "xct")
            self.ld("sp", xct, self.XF[L:NT, :].rearrange("(k p) c -> p k c", p=128), Bxct)
            for cc in range(8):
                pq, Bpq = PQ[cc % 2]
                bk, Bbk = self.bank()
                for k in range(2):
                    self.mm(bk, xct[:, k, cc * 128:(cc + 1) * 128], csn[:, k, :], k == 0, k == 1, [Bxct, Bcsn], Bbk)
                self.act(pq[:, :, 0:CT], bk.rearrange("p (r t) -> p r t", r=2), AF.Copy, [Bbk], Bpq)
                if cc % 2 == 1:
                    chan_stage(cc // 2, [PQ[0], PQ[1]], L, CT, 0, SCC)
        self.phase_end()

    def phase_merge(self, l, last):
        self.phase_begin()
        wo = []
        for i, src in enumerate((self.w_o_attn, self.w_o_rg, self.w_o_four, self.w_out)):
            w, Bw = self.alloc([128, 8, D], BF16, "wo%d" % i)
            for k in range(0, 8, 4):
                self.ld("pool", w[:, k:k + 4, :], src[l][k * 128:(k + 4) * 128, :].rearrange("(k p) n -> p k n", p=128), (Bw, k))
            wo.append((w, Bw))
        wmg, Bwmg = self.alloc([128, 8, 3 * D], BF16, "wmg")
        for k in range(8):
            self.ld("pool", wmg[:, k, :], self.w_merge[l][k * 128:(k + 1) * 128, :], (Bwmg, k))
        bmg, Bbmg = self.alloc([128, 24], F32, "bmg")
        self.ld("sp", bmg, self.b_merge_c[l], Bbmg)
        N = 256
        ins = [[self.alloc([128, 8, N], BF16, "mi%d_%d" % (i, j)) for j in range(2)] for i in range(4)]
        mg = [self.alloc([128, 8, N], BF16, "mg%d" % i) for i in range(2)]
        gt = [self.alloc([128, N], BF16, "gt%d" % i) for i in range(6)]
        pt = [self.alloc([128, N], F32, "pt%d" % i) for i in range(6)]
        yo = [self.alloc([128, 8, N], F32, "myo%d" % i) for i in range(2)]
        ntok = L if last else NT
        ngt = npt = 0
        for g in range(ntok // N):
            t0 = g * N
            tiles = []
            for i, src in enumerate((self.UT, self.AT, self.RT, self.FT)):
                t, Bt = ins[i][g % 2]
                self.ld("sp", t, src[:, t0:t0 + N].rearrange("(c p) t -> p c t", p=128), Bt)
                tiles.append((t, Bt))
            u, Bu = tiles[0]
            m, Bm = mg[g % 2]
            for j in range(8):
                terms = []
                for br in range(3):
                    bg, Bbg = self.bank()
                    col = br * D + j * 128
                    for k in range(8):
                        self.mm(bg[:, 0:N], wmg[:, k, col:col + 128], u[:, k, :], k == 0, k == 7, [Bwmg, Bu], Bbg)
                    gg, Bgg = gt[ngt % 6]
                    ngt += 1
                    self.act(gg, bg[:, 0:N], AF.Sigmoid, [Bbg, Bbmg], Bgg, bias=bmg[:, br * 8 + j:br * 8 + j + 1])
                    by, Bby = self.bank()
                    w, Bw = wo[br]
                    t, Bt = tiles[1 + br]
                    for k in range(8):
                        self.mm(by[:, 0:N], w[:, k, j * 128:(j + 1) * 128], t[:, k, :], k == 0, k == 7, [Bw, Bt], Bby)
                    p, Bp = pt[npt % 6]
                    npt += 1
                    self.tt("dve", p, by[:, 0:N], gg, ALU.mult, [Bby, Bgg], Bp)
                    terms.append((p, Bp))
                (p0, B0), (p1, B1), (p2, B2) = terms
                self.tt("pool", p0, p0, p1, ALU.add, [B0, B1], B0)
                self.tt("pool", m[:, j, :], p0, p2, ALU.add, [B0, B2], (Bm, j))
            o, Bo = yo[g % 2]
            w, Bw = wo[3]
            for j in range(8):
                by, Bby = self.bank()
                for k in range(8):
                    self.mm(by[:, 0:N], w[:, k, j * 128:(j + 1) * 128], m[:, k, :], k == 0, k == 7, [Bw, Bm], Bby)
                if j % 2 == 0:
                    self.act(o[:, j, :], by[:, 0:N], AF.Copy, [Bby], (Bo, j))
                else:
                    self.op("dve", lambda e, o=o, by=by, j=j: e.tensor_copy(out=o[:, j, :], in_=by[:, 0:N]), reads=[Bby], writes=[(Bo, j)])
            self.st("sp", self.YT[:, t0:t0 + N].rearrange("(c p) t -> p c t", p=128), o, Bo)
        self.phase_end()

    def phase_ln(self, l, last, first):
        self.phase_begin()
        src_x = self.XT if first else self.X1T
        dst_x = self.X1T if first else self.XT
        gate_w = 2 if first else 5
        gi, bi = (0, 1) if first else (2, 3)
        lnc, Blnc = self.alloc([128, 4, 8], F32, "lnc")
        self.ld("sp", lnc, self.lncols[l], Blnc)
        N = 512
        xs = [self.alloc([128, 8, N], F32, "lx%d" % i) for i in range(2)]
        ys = [self.alloc([128, 8, N], F32, "ly%d" % i) for i in range(2)]
        rs, Brs = self.alloc([128, 8, N], F32, "lr")
        sq, Bsq = self.alloc([128, 8, N], F32, "lsq")
        xo = [self.alloc([128, 8, N], F32, "lxo%d" % i) for i in range(2)]
        st_ = [self.alloc([128, N], F32, "lst%d" % i) for i in range(4)]
        final = last and not first
        if first:
            u2 = [self.alloc([128, 8, N], BF16, "lu%d" % i) for i in range(2)]
            u2f, Bu2f = self.alloc([128, 8, N], F32, "luf")
            wr, Bwr = self.alloc([128, 8, NE], F32, "wr")
            brb, Bbrb = self.alloc([128, NE], F32, "brb")
            self.ld("sp", wr, self.w_router[l].rearrange("(k p) e -> p k e", p=128), Bwr)
            self.ld("sp", brb, self.b_router_bc[l], Bbrb)
            lg = [self.alloc([128, NE], F32, "lg%d" % i) for i in range(2)]
            sm = [self.alloc([128, 16], F32, "sm%d" % i) for i in range(2)]
            gts = [self.alloc([NE, N], BF16, "gts%d" % i) for i in range(2)]
        if final:
            ot = [self.alloc([128, D], F32, "lot%d" % i) for i in range(2)]
        ntok = L if last else NT
        ntile = (ntok + N - 1) // N
        for g in range(ntile):
            t0 = g * N
            n = min(N, ntok - t0)
            v = 0 if t0 < L else 1
            x, Bx = xs[g % 2]
            y, By = ys[g % 2]
            self.ld("sp", x[:, :, 0:n], src_x[:, t0:t0 + n].rearrange("(c p) t -> p c t", p=128), Bx)
            self.ld("sp", y[:, :, 0:n], self.YT[:, t0:t0 + n].rearrange("(c p) t -> p c t", p=128), By)
            for c in range(8):
                self.act(y[:, c, 0:n], y[:, c, 0:n], AF.Copy, [By, self.Bmodc], (By, c), scale=self.mcol(gate_w, c, v))
                self.op("dve", lambda e, c=c, x=x, y=y, n=n: e.scalar_tensor_tensor(
                    out=rs[:, c, 0:n], in0=x[:, c, 0:n], scalar=ALPHA, in1=y[:, c, 0:n], op0=ALU.mult, op1=ALU.add),
                    reads=[Bx, (By, c)], writes=[(Brs, c)])
                self.act(sq[:, c, 0:n], rs[:, c, 0:n], AF.Square, [(Brs, c)], (Bsq, c))
            bm, Bbm = self.bank()
            bq, Bbq = self.bank()
            for c in range(8):
                self.mm(bm[:, 0:n], self.ones_s, rs[:, c, 0:n], c == 0, c == 7, [self.Bones_s, (Brs, c)], Bbm)
            for c in range(8):
                self.mm(bq[:, 0:n], self.ones_s, sq[:, c, 0:n], c == 0, c == 7, [self.Bones_s, (Bsq, c)], Bbq)
            (mean, Bmean), (m2, Bm2), (var, Bvar), (rstd, Brstd) = st_
            self.act(mean[:, 0:n], bm[:, 0:n], AF.Copy, [Bbm], Bmean)
            self.tt("dve", m2[:, 0:n], bm[:, 0:n], mean[:, 0:n], ALU.mult, [Bbm, Bmean], Bm2)
            self.tt("dve", var[:, 0:n], bq[:, 0:n], m2[:, 0:n], ALU.subtract, [Bbq, Bm2], Bvar)
            self.ts("dve", var[:, 0:n], var[:, 0:n], LN_EPS, None, ALU.add, None, [Bvar], Bvar)
            self.act(var[:, 0:n], var[:, 0:n], AF.Sqrt, [Bvar], Bvar)
            self.op("dve", lambda e, n=n: e.reciprocal(out=rstd[:, 0:n], in_=var[:, 0:n]), reads=[Bvar], writes=[Brstd])
            o, Bo = xo[g % 2]
            for c in range(8):
                self.tt("pool", rs[:, c, 0:n], rs[:, c, 0:n], mean[:, 0:n], ALU.subtract, [(Brs, c), Bmean], (Brs, c))
                self.tt("dve", rs[:, c, 0:n], rs[:, c, 0:n], rstd[:, 0:n], ALU.mult, [(Brs, c), Brstd], (Brs, c))
                self.act(o[:, c, 0:n], rs[:, c, 0:n], AF.Identity, [(Brs, c), Blnc], (Bo, c),
                         scale=lnc[:, gi, c:c + 1], bias=lnc[:, bi, c:c + 1])
            if not final:
                self.st("sp", dst_x[:, t0:t0 + n].rearrange("(c p) t -> p c t", p=128), o[:, :, 0:n], Bo)
            if first:
                u, Bu = u2[g % 2]
                for c in range(8):
                    self.act(u[:, c, 0:n], o[:, c, 0:n], AF.Identity, [(Bo, c), self.Bmodc], (Bu, c),
                             scale=self.mcol(4, c, v), bias=self.mcol(3, c, v))
                    self.act(u2f[:, c, 0:n], o[:, c, 0:n], AF.Identity, [(Bo, c), self.Bmodc], (Bu2f, c),
                             scale=self.mcol(4, c, v), bias=self.mcol(3, c, v))
                self.st("sp", self.U2T[:, t0:t0 + n].rearrange("(c p) t -> p c t", p=128), u[:, :, 0:n], Bu)
                gtile, Bgt = gts[g % 2]
                for s in range(n // 128):
                    bk, Bbk = self.bank()
                    for c in range(8):
                        self.mm(bk[:, 0:NE], u2f[:, c, s * 128:(s + 1) * 128], wr[:, c, :], c == 0, c == 7,
                                [(Bu2f, c), Bwr], Bbk)
                    lgt, Blg = lg[s % 2]
                    smt, Bsm = sm[s % 2]
                    self.tt("dve", lgt, bk[:, 0:NE], brb, ALU.add, [Bbk, Bbrb], Blg)
                    self.op("dve", lambda e, smt=smt, lgt=lgt: e.max(out=smt[:, 0:8], in_=lgt), reads=[Blg], writes=[Bsm])
                    self.ts("dve", smt[:, 8:9], smt[:, 0:1], -1.0, None, ALU.mult, None, [Bsm], Bsm)
                    bk2, Bbk2 = self.bank()
                    self.ts("dve", bk2[:, 0:NE], lgt, smt[:, 3:4], None, ALU.is_ge, None, [Blg, Bsm], Bbk2)
                    self.act(lgt, lgt, AF.Exp, [Blg, Bsm], Blg, bias=smt[:, 8:9])
                    self.tt("dve", lgt, lgt, bk2[:, 0:NE], ALU.mult, [Blg, Bbk2], Blg)
                    self.op("dve", lambda e, smt=smt, lgt=lgt: e.tensor_reduce(out=smt[:, 9:10], in_=lgt, axis=AX.X, op=ALU.add),
                            reads=[Blg], writes=[Bsm])
                    self.op("dve", lambda e, smt=smt: e.reciprocal(out=smt[:, 10:11], in_=smt[:, 9:10]), reads=[Bsm], writes=[Bsm])
                    self.ts("dve", lgt, lgt, smt[:, 10:11], None, ALU.mult, None, [Blg, Bsm], Blg)
                    bk3, Bbk3 = self.bank()
                    self.op("pe", lambda e, bk3=bk3, lgt=lgt: e.transpose(out=bk3[0:NE, 0:128], in_=lgt, identity=self.ident),
                            reads=[Blg, self.Bident], writes=[Bbk3])
                    self.act(gtile[:, s * 128:(s + 1) * 128], bk3[0:NE, 0:128], AF.Copy, [Bbk3], (Bgt, s))
                self.st("sp", self.GT[:, t0:t0 + n], gtile[:, 0:n], Bgt)
            if final:
                for s in range(n // 128):
                    ott, Bott = ot[s % 2]
                    for half in range(2):
                        bk, Bbk = self.bank()
                        for i in range(4):
                            c = half * 4 + i
                            self.op("pe", lambda e, bk=bk, o=o, i=i, c=c, s=s: e.transpose(
                                out=bk[:, i * 128:(i + 1) * 128], in_=o[:, c, s * 128:(s + 1) * 128], identity=self.ident),
                                reads=[(Bo, c), self.Bident], writes=[Bbk], signal=(i == 3))
                        if half == 0:
                            self.act(ott[:, 0:512], bk, AF.Copy, [Bbk], (Bott, 0))
                        else:
                            self.op("dve", lambda e, ott=ott, bk=bk: e.tensor_copy(out=ott[:, 512:1024], in_=bk), reads=[Bbk], writes=[(Bott, 1)])
                    self.st("sp", self.out[t0 + s * 128:t0 + (s + 1) * 128, :], ott, Bott)
        self.phase_end()

    def phase_moe(self, l, last):
        self.phase_begin()
        T = 2048
        bgu, Bbgu = self.alloc([128, NE, 16], F32, "bgu")
        bdn, Bbdn = self.alloc([NE, D], BF16, "bdn")
        sel, Bsel = self.alloc([NE, NE, 128], BF16, "sel")
        self.ld("sp", bgu, self.b_gu_c[l], Bbgu)
        bgu1, Bbgu1 = self.alloc([128, NE, 8], F32, "bgu1")
        self.ts("dve", bgu1, bgu[:, :, 8:16], 1.0, None, ALU.add, None, [Bbgu], Bbgu1)
        self.ld("pool", bdn, self.b_dn[l], Bbdn)
        self.ld("pool", sel, self.sel, Bsel)
        uT, BuT = self.alloc([128, 8, T], BF16, "mu")
        fT, BfT = self.alloc([128, 8, T], F32, "mf")
        aT, BaT = self.alloc([128, 8, T], BF16, "ma")
        gbc, Bgbc = self.alloc([128, T], BF16, "gbc")
        gT, BgT = self.alloc([NE, T], BF16, "mgT")
        wg = [self.alloc([128, 8, 256], BF16, "wg%d" % i) for i in range(4)]
        wd = [self.alloc([128, 8, 256], BF16, "wd%d" % i) for i in range(4)]
        tf = [self.alloc([128, 512], F32, "mt%d" % i) for i in range(9)]
        ntf = 0
        ntok = L if last else NT
        sts = [(t0, min(T, ntok - t0)) for t0 in range(0, ntok, T)]
        units = []
        for si in range(len(sts)):
            for e in range(NE):
                for c in range(8):
                    units.append(("g", si, e, c))
                for jj in range(4):
                    units.append(("d", si, e, jj))
        cnt = {"g": 0, "d": 0}
        slots = {}
        issued = 0

        def issue(upto):
            nonlocal issued
            while issued < min(upto, len(units)):
                kind, si, e, i = units[issued]
                if kind == "g":
                    w, Bw = wg[cnt["g"] % 4]
                    cnt["g"] += 1
                    self.ld("pool", w, self.w_gu[l, e, i].rearrange("(k p) n -> p k n", p=128), Bw)
                else:
                    w, Bw = wd[cnt["d"] % 4]
                    cnt["d"] += 1
                    self.ld("pool", w, self.w_dn[l, e, i].rearrange("(k p) n -> p k n", p=128), Bw)
                slots[issued] = (w, Bw)
                issued += 1
        ui = 0
        LOOK = 4
        for si, (s0, sn) in enumerate(sts):
            tiles = [(o, min(512, sn - o)) for o in range(0, sn, 512)]
            self.ld("sp", uT[:, :, 0:sn], self.U2T[:, s0:s0 + sn].rearrange("(c p) t -> p c t", p=128), BuT)
            self.ld("sp", gT[:, 0:sn], self.GT[:, s0:s0 + sn], BgT)
            for (o, n) in tiles:
                for j in range(8):
                    bk, Bbk = self.bank()
                    self.mm(bk[:, 0:n], bdn[:, j * 128:(j + 1) * 128], gT[:, o:o + n], True, True, [Bbdn, BgT], Bbk)
                    self.act(fT[:, j, o:o + n], bk[:, 0:n], AF.Copy, [Bbk], (BfT, (j, o)))
            for e in range(NE):
                for (o, n) in tiles:
                    bk, Bbk = self.bank()
                    self.mm(bk[:, 0:n], sel[:, e, :], gT[:, o:o + n], True, True, [Bsel, BgT], Bbk)
                    self.act(gbc[:, o:o + n], bk[:, 0:n], AF.Copy, [Bbk], (Bgbc, o), scale=1.0 / 1.702)
                for c in range(8):
                    issue(ui + LOOK)
                    w, Bw = slots.pop(ui)
                    ui += 1
                    for (o, n) in tiles:
                        bg, Bbg = self.bank()
                        bl, Bbl = self.bank()
                        for k in range(8):
                            self.mm(bg[:, 0:n], w[:, k, 0:128], uT[:, k, o:o + n], k == 0, k == 7, [Bw, BuT], Bbg)
                        for k in range(8):
                            self.mm(bl[:, 0:n], w[:, k, 128:256], uT[:, k, o:o + n], k == 0, k == 7, [Bw, BuT], Bbl)
                        (glu, Bglu), (sig, Bsig), (lin, Blin) = tf[ntf % 9], tf[(ntf + 1) % 9], tf[(ntf + 2) % 9]
                        ntf += 3
                        self.ts("dve", glu[:, 0:n], bg[:, 0:n], bgu[:, e, c:c + 1], 7.0, ALU.add, ALU.min, [Bbg, Bbgu], Bglu)
                        self.act(sig[:, 0:n], glu[:, 0:n], AF.Silu, [Bglu], Bsig, scale=1.702)
                        self.ts("dve", lin[:, 0:n], bl[:, 0:n], bgu1[:, e, c:c + 1], 8.0, ALU.add, ALU.min, [Bbl, Bbgu1], Blin)
                        self.op("dve", lambda e_, lin=lin, sig=sig, n=n: e_.scalar_tensor_tensor(
                            out=lin[:, 0:n], in0=lin[:, 0:n], scalar=-6.0, in1=sig[:, 0:n], op0=ALU.max, op1=ALU.mult),
                            reads=[Blin, Bsig], writes=[Blin])
                        self.tt("pool", aT[:, c, o:o + n], lin[:, 0:n], gbc[:, o:o + n], ALU.mult, [Blin, (Bgbc, o)], (BaT, (c, o)))
                for jj in range(4):
                    issue(ui + LOOK)
                    w, Bw = slots.pop(ui)
                    ui += 1
                    for jl in range(2):
                        j = jj * 2 + jl
                        for (o, n) in tiles:
                            bk, Bbk = self.bank()
                            for c in range(8):
                                self.mm(bk[:, 0:n], w[:, c, jl * 128:(jl + 1) * 128], aT[:, c, o:o + n], c == 0, c == 7,
                                        [Bw, (BaT, (c, o))], Bbk)
                            self.tt("dve", fT[:, j, o:o + n], bk[:, 0:n], fT[:, j, o:o + n], ALU.add,
                                    [Bbk, (BfT, (j, o))], (BfT, (j, o)))
            self.st("sp", self.YT[:, s0:s0 + sn].rearrange("(c p) t -> p c t", p=128), fT[:, :, 0:sn], BfT)
        self.phase_end()


def _cols(v, nchunk):
    return np.ascontiguousarray(np.asarray(v, np.float32).reshape(nchunk, 128).T)


def _consts():
    c = {}
    n_freq = 16
    freqs = (np.float32(10000.0) ** (-np.arange(n_freq, dtype=np.float32) / np.float32(n_freq))).astype(np.float32)
    pos = np.arange(L)
    row = (pos // 64).astype(np.float32)
    col = (pos % 64).astype(np.float32)
    ang_r = (row[:, None] * freqs[None, :]).astype(np.float32)
    ang_c = (col[:, None] * freqs[None, :]).astype(np.float32)
    cos = np.ones((64, NT), np.float32)
    sin = np.zeros((64, NT), np.float32)
    for d in range(64):
        ang = ang_r if d < 32 else ang_c
        j = d % 16
        sgn = -1.0 if (d % 32) < 16 else 1.0
        cos[d, :L] = np.cos(ang[:, j])
        sin[d, :L] = sgn * np.sin(ang[:, j])
    c["cos2"] = np.ascontiguousarray(np.concatenate([cos, cos], 0))
    c["sin2"] = np.ascontiguousarray(np.concatenate([sin, sin], 0))
    k = np.arange(128)
    tri = np.zeros((128, 2, 128), np.float32)
    tri[:, 0, :] = (k[:, None] >= k[None, :])
    tri[:, 1, :] = (k[:, None] <= k[None, :])
    c["tri"] = tri
    a = 2.0 * np.pi * np.outer(k, k) / 128.0
    c["w128"] = np.concatenate([np.cos(a), -np.sin(a)], 1).astype(np.float32)
    l2 = np.arange(64)
    k1 = np.arange(128)
    k2 = np.arange(64)
    kk = k1[:, None] + 128 * k2[None, :]
    th = 2.0 * np.pi * (l2[:, None, None] * kk[None, :, :] % L) / L
    fr, fi = np.cos(th), -np.sin(th)
    fab = np.zeros((128, 128, 128), np.float32)
    fab[0:64, :, 0:64] = fr
    fab[0:64, :, 64:128] = fi
    fab[64:128, :, 0:64] = -fi
    fab[64:128, :, 64:128] = fr
    c["fab"] = fab
    n = np.arange(256)
    a = 2.0 * np.pi * (np.outer(n, n) % 256) / 256.0
    C, S = np.cos(a), np.sin(a)
    cs = np.zeros((128, 2, 2, 256), np.float32)
    csn = np.zeros((128, 2, 512), np.float32)
    for kc in range(2):
        cs[:, kc, 0, :] = C[kc * 128:(kc + 1) * 128]
        cs[:, kc, 1, :] = S[kc * 128:(kc + 1) * 128]
        csn[:, kc, 0:256] = C[kc * 128:(kc + 1) * 128]
        csn[:, kc, 256:512] = -S[kc * 128:(kc + 1) * 128]
    c["cs256"] = cs
    c["csn256"] = csn
    sel = np.zeros((NE, NE, 128), np.float32)
    for e in range(NE):
        sel[e, e, :] = 1.0
    c["sel"] = sel
    return c


def _prep_shared(inp):
    f = lambda a: np.asarray(a, np.float32)
    sh = {}
    sh["w_mod"] = f(inp["w_mod"])
    sh["b_mod_c"] = np.stack([_cols(inp["b_mod"][l], 48) for l in range(2)])
    w_in = f(inp["w_in"])
    perm = np.array([(d + 16) if (d % 32) < 16 else (d - 16) for d in range(64)])
    qperm = np.concatenate([h * 64 + perm for h in range(16)])
    kperm = np.concatenate([h * 64 + perm for h in range(4)])
    q, k = w_in[:, :, 0:1024], w_in[:, :, 1024:1280]
    sh["w_in"] = np.ascontiguousarray(np.concatenate(
        [q, q[:, :, qperm], k, k[:, :, kperm], w_in[:, :, 1280:]], axis=2))
    sh["sink64"] = np.ascontiguousarray(np.broadcast_to(f(inp["attn_sink"])[:, None, :], (2, 64, 16)))
    for nm in ("w_o_attn", "w_o_rg", "w_o_four", "w_out", "w_merge"):
        sh[nm] = f(inp[nm])
    sh["b_merge_c"] = np.stack([_cols(inp["b_merge"][l], 24) for l in range(2)])
    rgc = np.zeros((2, 128, 8, 11), np.float32)
    for l in range(2):
        for t in range(4):
            rgc[l, :, :, t] = _cols(inp["conv_w"][l, t], 8)
        rgc[l, :, :, 4] = _cols(inp["conv_b"][l], 8)
        for d in range(2):
            rgc[l, :, :, 5 + d] = _cols(inp["rg_b_a"][l, d], 8)
            rgc[l, :, :, 7 + d] = _cols(inp["rg_b_i"][l, d], 8)
            rgc[l, :, :, 9 + d] = _cols(inp["rg_lambda"][l, d], 8)
    sh["rgcols"] = rgc
    wbd = np.zeros((2, 8, 128, 4, 128), np.float32)
    for l in range(2):
        for gi, nm in enumerate(("rg_w_a", "rg_w_i")):
            w = f(inp[nm])
            for d in range(2):
                for c in range(8):
                    for hb in range(2):
                        wbd[l, c, hb * 64:(hb + 1) * 64, gi * 2 + d, hb * 64:(hb + 1) * 64] = w[l, d, 2 * c + hb]
    sh["rg_wbd"] = wbd
    lnc = np.zeros((2, 128, 4, 8), np.float32)
    for l in range(2):
        for i, nm in enumerate(("ln1_g", "ln1_b", "ln2_g", "ln2_b")):
            lnc[l, :, i, :] = _cols(inp[nm][l], 8)
    sh["lncols"] = lnc
    sh["w_router"] = f(inp["w_router"])
    sh["b_router_bc"] = np.ascontiguousarray(np.broadcast_to(f(inp["b_router"])[:, None, :], (2, 128, NE)))
    wgu = f(inp["w_gate_up"]).reshape(2, NE, D, 2, 8, 128)
    sh["w_gu"] = np.ascontiguousarray(wgu.transpose(0, 1, 4, 2, 3, 5)).reshape(2, NE, 8, D, 256)
    bgu = f(inp["b_gate_up"]).reshape(2, NE, 16, 128)
    sh["b_gu_c"] = np.ascontiguousarray(bgu.transpose(0, 3, 1, 2))
    wdn = f(inp["w_down"]).reshape(2, NE, D, 4, 256)
    sh["w_dn"] = np.ascontiguousarray(wdn.transpose(0, 1, 3, 2, 4))
    sh["b_dn"] = f(inp["b_down"])
    sh.update(_consts())
    return sh


def _core_inputs(inp, b, sh):
    m = dict(sh)
    m["x"] = np.ascontiguousarray(np.asarray(inp["x"][b], np.float32))
    m["ctx"] = np.ascontiguousarray(np.asarray(inp["ctx"][b], np.float32))
    cv = np.stack([np.asarray(inp["c"][b], np.float32), np.asarray(inp["c_ctx"], np.float32)], 0)
    m["cvecT"] = np.ascontiguousarray(cv.reshape(2, 8, 128).transpose(2, 1, 0))
    return m


def kernel(**inputs):
    sh = _prep_shared(inputs)
    nc = K().build()
    in_maps = [_core_inputs(inputs, b, sh) for b in range(8)]
    res = run_bass_kernel_spmd(nc, in_maps, core_ids=list(range(8)))
    return np.stack([np.asarray(r["out"], np.float32) for r in res.results], 0)
```

```python
import math
import os
from contextlib import ExitStack

import numpy as np
import concourse.bass as bass
import concourse.mybir as mybir
from concourse.bass_utils import run_bass_kernel_spmd

F32 = mybir.dt.float32
BF16 = mybir.dt.bfloat16
ALU = mybir.AluOpType
AF = mybir.ActivationFunctionType
AX = mybir.AxisListType

D = 1024
L = 8192
CT = 256
NT = L + CT
NE = 32
ALPHA = 4.0 ** 0.25
LN_EPS = 1e-5
QO, QPO, KO, KPO, VO, XRO, GRO, XFO, WIN = 0, 1024, 2048, 2304, 2560, 2816, 3840, 4864, 5888
ARENA_BYTES = 212000
N_DMA_SEMS = 84


class Buf:
    _n = 0

    def __init__(self, name):
        Buf._n += 1
        self.id = Buf._n
        self.name = name
        self.whole = [dict(), dict()]
        self.parts = {}
        self.phys = None


def _merge(dst, src):
    for k, v in src.items():
        if dst.get(k, 0) < v:
            dst[k] = v


class Prog:
    ENGS = ("pe", "act", "dve", "pool", "sp")

    def __init__(self, nc):
        self.nc = nc
        self.ops = {e: [] for e in self.ENGS}
        self.cnt = {e: 0 for e in self.ENGS}
        self.seen = {e: dict() for e in self.ENGS}
        self.latest = {}
        self.n_ops = 0
        self.phys_free = list(range(N_DMA_SEMS))
        self.phys_cnt = [0] * N_DMA_SEMS
        self.phys_bufs = []

    @staticmethod
    def _norm(x):
        return (x, None) if isinstance(x, Buf) else x

    def _collect(self, reads, writes, skip_dw=False):
        deps = {}

        def mw(src):
            if skip_dw:
                for k, v in src.items():
                    if k[0] != "d" and deps.get(k, 0) < v:
                        deps[k] = v
            else:
                _merge(deps, src)
        for x in reads:
            b, k = self._norm(x)
            _merge(deps, b.whole[0])
            if k is None:
                for p in b.parts.values():
                    _merge(deps, p[0])
            elif k in b.parts:
                _merge(deps, b.parts[k][0])
        for x in writes:
            b, k = self._norm(x)
            mw(b.whole[0])
            _merge(deps, b.whole[1])
            if k is None:
                for p in b.parts.values():
                    mw(p[0])
                    _merge(deps, p[1])
            elif k in b.parts:
                mw(b.parts[k][0])
                _merge(deps, b.parts[k][1])
        return deps

    def _record(self, reads, writes, tok, dma_write=False):
        key, val = tok
        if self.latest.get(key, 0) < val:
            self.latest[key] = val
        for x in reads:
            b, k = self._norm(x)
            tgt = b.whole if k is None else b.parts.setdefault(k, [dict(), dict()])
            if tgt[1].get(key, 0) < val:
                tgt[1][key] = val
        for x in writes:
            b, k = self._norm(x)
            if k is None:
                if dma_write:
                    neww = {kk: vv for kk, vv in b.whole[0].items() if kk[0] == "d"}
                    for p in b.parts.values():
                        for kk, vv in p[0].items():
                            if kk[0] == "d" and neww.get(kk, 0) < vv:
                                neww[kk] = vv
                    neww[key] = max(neww.get(key, 0), val)
                    b.whole = [neww, dict()]
                else:
                    b.whole = [{key: val}, dict()]
                b.parts = {}
            else:
                if dma_write and k in b.parts:
                    neww = {kk: vv for kk, vv in b.parts[k][0].items() if kk[0] == "d"}
                    neww[key] = max(neww.get(key, 0), val)
                    b.parts[k] = [neww, dict()]
                else:
                    b.parts[k] = [{key: val}, dict()]

    def _waits(self, eng, deps):
        out = []
        seen = self.seen[eng]
        for key, val in deps.items():
            if eng == "pe" and key == ("e", "pe"):
                continue
            if seen.get(key, 0) >= val:
                continue
            seen[key] = val
            out.append((key, val))
        return out

    def op(self, eng, fn, reads=(), writes=(), signal=True):
        deps = self._collect(reads, writes)
        waits = self._waits(eng, deps)
        n = self.cnt[eng] + 1
        if signal:
            self.cnt[eng] = n
        tok = (("e", eng), n)
        self._record(reads, writes, tok)
        self.ops[eng].append((fn, waits, tok if signal else None))
        self.n_ops += 1

    def dma(self, q, fn, reads=(), writes=(), sembuf=None):
        if sembuf is None:
            sembuf = self._norm(writes[0])[0] if writes else self._norm(reads[0])[0]
        if sembuf.phys is None:
            sembuf.phys = self.phys_free.pop(0)
            self.phys_bufs.append(sembuf)
        deps = self._collect(reads, writes, skip_dw=True)
        waits = self._waits(q, deps)
        s = sembuf.phys
        self.phys_cnt[s] += 16
        key = ("d", s)
        self._record(reads, writes, (key, self.phys_cnt[s]), dma_write=True)
        self.ops[q].append((fn, waits, (key, 16)))
        self.n_ops += 1

    def barrier(self):
        for e in self.ENGS:
            waits = []
            for k, v in self.latest.items():
                if k == ("e", e):
                    continue
                if self.seen[e].get(k, 0) < v:
                    self.seen[e][k] = v
                    waits.append((k, v))
            if waits:
                self.ops[e].append((None, waits, None))
        for b in self.phys_bufs:
            self.phys_free.append(b.phys)
            b.phys = None
        self.phys_bufs = []

    def emit(self, stack):
        nc = self.nc
        sems = {}
        for e in self.ENGS:
            sems[("e", e)] = stack.enter_context(nc.semaphore("se_" + e))
        for i in range(N_DMA_SEMS):
            if self.phys_cnt[i] > 0:
                sems[("d", i)] = stack.enter_context(nc.semaphore("sd_%d" % i))
        block = stack.enter_context(nc.Block())
        prog = self

        def run(ename, eng):
            for fn, waits, inc in prog.ops[ename]:
                for k, v in waits:
                    eng.wait_ge(sems[k], v)
                if fn is None:
                    continue
                ins = fn(eng)
                if inc is not None:
                    k, v = inc
                    ins.then_inc(sems[k], 1 if k[0] == "e" else 16)

        @block.tensor
        def _(e):
            run("pe", e)

        @block.scalar
        def _(e):
            run("act", e)

        @block.vector
        def _(e):
            run("dve", e)

        @block.gpsimd
        def _(e):
            run("pool", e)

        @block.sync
        def _(e):
            run("sp", e)


class K:
    def __init__(self, taps=(), layers=(0, 1), phases=None):
        self.taps = set(taps)
        self.layers = layers
        self.phases = phases
        self.nc = bass.Bass("TRN2", target_bir_lowering=False)
        self.P = Prog(self.nc)
        self.top = 0
        self.bank_rr = 0
        self.rr = 0

    def din(self, name, shape, dt=F32):
        return self.nc.dram_tensor(name, list(shape), dt, kind="ExternalInput").ap()

    def dscr(self, name, shape, dt):
        kind = "ExternalOutput" if name in self.taps else "Internal"
        return self.nc.dram_tensor(name, list(shape), dt, kind=kind).ap()

    def alloc(self, shape, dt, name="t"):
        esz = 4 if dt == F32 else 2
        n = 1
        for s in shape[1:]:
            n *= s
        nbytes = n * esz
        off = self.top
        self.top += (nbytes + 63) // 64 * 64
        assert self.top <= ARENA_BYTES, ("SBUF arena overflow", name, self.top)
        v = self.arena[:, off // 2:(off + nbytes) // 2]
        if dt == F32:
            v = v.bitcast(F32)
        if len(shape) == 3:
            v = v.rearrange("p (a b) -> p a b", a=shape[1])
        elif len(shape) == 4:
            v = v.rearrange("p (a b c) -> p a b c", a=shape[1], b=shape[2])
        if shape[0] < 128:
            v = v[0:shape[0]]
        return v, Buf(name)

    def bank(self):
        i = self.bank_rr
        self.bank_rr = (i + 1) % 8
        return self.ps[:, i * 512:(i + 1) * 512], self.BPS[i]

    def op(self, eng, fn, reads=(), writes=(), signal=True):
        self.P.op(eng, fn, reads, writes, signal)

    def ld(self, q, out, in_, wbuf, rbufs=()):
        self.P.dma(q, lambda e: e.dma_start(out=out, in_=in_), reads=list(rbufs), writes=[wbuf])

    def st(self, q, out, in_, rbuf):
        self.P.dma(q, lambda e: e.dma_start(out=out, in_=in_), reads=[rbuf], writes=[])

    def mm(self, out, lhsT, rhs, start, stop, reads, wbuf, signal=None):
        self.P.op("pe", lambda e: e.matmul(out=out, lhsT=lhsT, rhs=rhs, start=start, stop=stop),
                  reads=reads, writes=[wbuf], signal=(stop if signal is None else signal))

    def act(self, out, in_, func, reads, wbuf, scale=1.0, bias=0.0):
        if func == AF.Copy and not (isinstance(scale, float) and scale == 1.0):
            func = AF.Identity
        self.P.op("act", lambda e: e.activation(out=out, in_=in_, func=func, scale=scale, bias=bias),
                  reads=reads, writes=[wbuf])

    def tt(self, eng, out, in0, in1, op, reads, wbuf):
        self.P.op(eng, lambda e: e.tensor_tensor(out=out, in0=in0, in1=in1, op=op), reads=reads, writes=[wbuf])

    def ts(self, eng, out, in0, s1, s2, op0, op1, reads, wbuf):
        if s2 is None:
            self.P.op(eng, lambda e: e.tensor_scalar(out=out, in0=in0, scalar1=s1, scalar2=None, op0=op0),
                      reads=reads, writes=[wbuf])
        else:
            self.P.op(eng, lambda e: e.tensor_scalar(out=out, in0=in0, scalar1=s1, scalar2=s2, op0=op0, op1=op1),
                      reads=reads, writes=[wbuf])

    def phase_begin(self):
        self.top = self.persist_top

    def phase_end(self):
        self.P.barrier()

    def want(self, name):
        return self.phases is None or name in self.phases

    def build(self):
        nc = self.nc
        st = ExitStack()
        with st:
            self.arena = st.enter_context(nc.sbuf_tensor("arena", [128, ARENA_BYTES // 2], BF16))
            self.ps = st.enter_context(nc.psum_tensor("psum", [128, 4096], F32))
            self.BPS = [Buf("ps%d" % i) for i in range(8)]
            self.declare_io()
            self.setup_persist()
            if self.want("tin"):
                self.phase_tin()
            for l in self.layers:
                last = (l == 1)
                if self.want("mods"):
                    self.phase_mods(l)
                if self.want("proj"):
                    self.phase_proj(l, last)
                if self.want("rg"):
                    self.phase_rg(l, last)
                if self.want("att"):
                    self.phase_att(l, last)
                if self.want("four"):
                    self.phase_four(l, last)
                if self.want("merge"):
                    self.phase_merge(l, last)
                if self.want("ln1"):
                    self.phase_ln(l, last, first=True)
                if self.want("moe"):
                    self.phase_moe(l, last)
                if self.want("ln2"):
                    self.phase_ln(l, last, first=False)
            self.P.barrier()
            self.P.emit(st)
        return nc

    def declare_io(self):
        d = self.din
        self.x_in = d("x", [L, D])
        self.ctx_in = d("ctx", [CT, D])
        self.cvecT = d("cvecT", [128, 8, 2])
        self.w_mod = d("w_mod", [2, D, 6 * D])
        self.b_mod_c = d("b_mod_c", [2, 128, 48])
        self.w_in = d("w_in", [2, D, WIN])
        self.sink64 = d("sink64", [2, 64, 16])
        self.w_o_attn = d("w_o_attn", [2, D, D])
        self.w_o_rg = d("w_o_rg", [2, D, D])
        self.w_o_four = d("w_o_four", [2, D, D])
        self.w_out = d("w_out", [2, D, D])
        self.w_merge = d("w_merge", [2, D, 3 * D])
        self.b_merge_c = d("b_merge_c", [2, 128, 24])
        self.rgcols = d("rgcols", [2, 128, 8, 11])
        self.rg_wbd = d("rg_wbd", [2, 8, 128, 4, 128])
        self.lncols = d("lncols", [2, 128, 4, 8])
        self.w_router = d("w_router", [2, D, NE])
        self.b_router_bc = d("b_router_bc", [2, 128, NE])
        big = self.want("moe")
        self.w_gu = d("w_gu", [2, NE, 8, D, 256] if big else [1, 1, 1, 128, 256])
        self.b_gu_c = d("b_gu_c", [2, 128, NE, 16])
        self.w_dn = d("w_dn", [2, NE, 4, D, 256] if big else [1, 1, 1, 128, 256])
        self.b_dn = d("b_dn", [2, NE, D])
        self.cos2 = d("cos2", [128, NT])
        self.sin2 = d("sin2", [128, NT])
        self.tri = d("tri", [128, 2, 128])
        self.w128 = d("w128", [128, 256])
        self.fab = d("fab", [128, 128, 128])
        self.cs256 = d("cs256", [128, 2, 2, 256])
        self.csn256 = d("csn256", [128, 2, 512])
        self.sel = d("sel", [NE, NE, 128])
        self.out = self.nc.dram_tensor("out", [L, D], F32, kind="ExternalOutput").ap()
        s = self.dscr
        self.XT = s("XT", [D, NT], F32)
        self.X1T = s("X1T", [D, NT], F32)
        self.YT = s("YT", [D, NT], F32)
        self.UT = s("UT", [D, NT], BF16)
        self.QT = s("QT", [D, NT], BF16)
        self.KT = s("KT", [256, NT], BF16)
        self.V = s("V", [NT, 256], BF16)
        self.XRT = s("XRT", [D, NT], F32)
        self.GRT = s("GRT", [D, NT], BF16)
        self.XF = s("XF", [NT, D], BF16)
        self.AT = s("AT", [D, NT], BF16)
        self.RT = s("RT", [D, NT], BF16)
        self.FT = s("FT", [D, NT], BF16)
        self.U2T = s("U2T", [D, NT], BF16)
        self.GT = s("GT", [NE, NT], BF16)
        if "MODC" in self.taps:
            self.MODC = s("MODC", [128, 2, 48], F32)

    def setup_persist(self):
        self.ident, self.Bident = self.alloc([128, 128], F32, "ident")
        self.ones_s, self.Bones_s = self.alloc([128, 128], F32, "ones_s")
        self.ones_b, self.Bones_b = self.alloc([128, 64], BF16, "ones_b")
        self.modc, self.Bmodc = self.alloc([128, 2, 48], F32, "modc")
        ident, ones_s, ones_b = self.ident, self.ones_s, self.ones_b
        self.op("pool", lambda e: e.memset(ident, 0.0), writes=[self.Bident])
        self.op("pool", lambda e: e.affine_select(out=ident, in_=ident, compare_op=ALU.not_equal, fill=1.0,
                                                  base=0, pattern=[[-1, 128]], channel_multiplier=1),
                reads=[self.Bident], writes=[self.Bident])
        self.op("pool", lambda e: e.memset(ones_s, 1.0 / D), writes=[self.Bones_s])
        self.op("pool", lambda e: e.memset(ones_b, 1.0), writes=[self.Bones_b])
        self.persist_top = self.top

    def phase_tin(self):
        self.phase_begin()
        xin = [self.alloc([128, D], F32, "xin%d" % i) for i in range(4)]
        xT = [self.alloc([128, 8, 512], F32, "xT%d" % i) for i in range(2)]
        for g in range(17):
            t0 = g * 512
            n = 512 if g < 16 else CT
            xt, Bxt = xT[g % 2]
            for s in range(n // 128):
                xi, Bxi = xin[s]
                src = self.x_in[t0 + s * 128:t0 + (s + 1) * 128, :] if g < 16 else self.ctx_in[s * 128:(s + 1) * 128, :]
                self.ld("sp", xi, src, Bxi)
                for half in range(2):
                    bk, Bbk = self.bank()
                    for i in range(4):
                        c = half * 4 + i
                        self.op("pe", lambda e, bk=bk, xi=xi, i=i, c=c: e.transpose(
                            out=bk[:, i * 128:(i + 1) * 128], in_=xi[:, c * 128:(c + 1) * 128], identity=self.ident),
                            reads=[Bxi, self.Bident], writes=[Bbk], signal=(i == 3))
                    o = xt[:, half * 4:(half + 1) * 4, s * 128:(s + 1) * 128]
                    i3 = bk.rearrange("p (a b) -> p a b", a=4)
                    if half == 0:
                        self.op("act", lambda e, o=o, i3=i3: e.copy(out=o, in_=i3), reads=[Bbk], writes=[(Bxt, (s, half))])
                    else:
                        self.op("dve", lambda e, o=o, i3=i3: e.tensor_copy(out=o, in_=i3), reads=[Bbk], writes=[(Bxt, (s, half))])
            self.st("sp", self.XT[:, t0:t0 + n].rearrange("(c p) t -> p c t", p=128), xt[:, :, 0:n], Bxt)
        self.phase_end()

    def phase_mods(self, l):
        self.phase_begin()
        scv, Bscv = self.alloc([128, 8, 2], F32, "scv")
        bmc, Bbmc = self.alloc([128, 48], F32, "bmc")
        wm = [self.alloc([128, 8, 512], F32, "wm%d" % i) for i in range(2)]
        self.ld("sp", scv, self.cvecT, Bscv)
        self.ld("sp", bmc, self.b_mod_c[l], Bbmc)
        self.act(scv, scv, AF.Silu, [Bscv], Bscv)
        bk, Bbk = self.bank()
        for n in range(12):
            w, Bw = wm[n % 2]
            self.ld("sp", w, self.w_mod[l][:, n * 512:(n + 1) * 512].rearrange("(k p) n -> p k n", p=128), Bw)
            for jj in range(4):
                j = n * 4 + jj
                for k in range(8):
                    self.mm(bk[:, 2 * j:2 * j + 2], w[:, k, jj * 128:(jj + 1) * 128], scv[:, k, :],
                            k == 0, k == 7, [Bw, Bscv], Bbk)
        pv = bk[:, 0:96].rearrange("p (j v) -> p v j", v=2)
        for v in range(2):
            self.tt("dve", self.modc[:, v, :], pv[:, v, :], bmc, ALU.add, [Bbk, Bbmc], (self.Bmodc, v))
        for lo in (8, 32):
            self.ts("dve", self.modc[:, :, lo:lo + 8], self.modc[:, :, lo:lo + 8], 1.0, None, ALU.add, None,
                    [self.Bmodc], self.Bmodc)
        if "MODC" in self.taps:
            self.st("sp", self.MODC, self.modc, self.Bmodc)
        self.phase_end()

    def mcol(self, which, c, v):
        j = which * 8 + c
        return self.modc[:, v, j:j + 1]

    def phase_proj(self, l, last):
        self.phase_begin()
        win, Bwin = self.alloc([128, 8, WIN], BF16, "win")
        for k in range(8):
            self.ld("pool", win[:, k, :], self.w_in[l][k * 128:(k + 1) * 128, :], (Bwin, k))
        xT = [self.alloc([128, 8, 512], F32, "xT%d" % i) for i in range(2)]
        cs = [self.alloc([128, 2, 512], F32, "cs%d" % i) for i in range(2)]
        uT = [self.alloc([128, 8, 512], BF16, "uT%d" % i) for i in range(2)]
        sf = [self.alloc([128, 512], F32, "sf%d" % i) for i in range(6)]
        sb = [self.alloc([128, 512], BF16, "sb%d" % i) for i in range(8)]
        sx = [self.alloc([128, 1024], BF16, "sx%d" % i) for i in range(2)]
        rt = [self.alloc([128, 2, 512], F32, "rt%d" % i) for i in range(2)]
        nsf = nsb = nsx = nrt = 0
        ntile = 17
        for g in range(ntile):
            t0 = g * 512
            n = 512 if g < 16 else CT
            v = 0 if g < 16 else 1
            x, Bx = xT[g % 2]
            c_, Bc_ = cs[g % 2]
            u, Bu = uT[g % 2]

            def proj_loads(gg):
                tt0 = gg * 512
                nn = 512 if gg < 16 else CT
                xx, Bxx = xT[gg % 2]
                cc_, Bcc_ = cs[gg % 2]
                self.ld("sp", xx[:, :, 0:nn], self.XT[:, tt0:tt0 + nn].rearrange("(c p) t -> p c t", p=128), Bxx)
                self.ld("sp", cc_[:, 0, 0:nn], self.cos2[:, tt0:tt0 + nn], (Bcc_, 0))
                self.ld("sp", cc_[:, 1, 0:nn], self.sin2[:, tt0:tt0 + nn], (Bcc_, 1))
            if g == 0:
                proj_loads(0)
            if g + 1 < ntile:
                proj_loads(g + 1)
            for c in range(8):
                self.act(u[:, c, 0:n], x[:, c, 0:n], AF.Identity, [Bx, self.Bmodc], (Bu, c),
                         scale=self.mcol(1, c, v), bias=self.mcol(0, c, v))
            self.st("sp", self.UT[:, t0:t0 + n].rearrange("(c p) t -> p c t", p=128), u[:, :, 0:n], Bu)

            def proj_fm(col0):
                bk, Bbk = self.bank()
                for k in range(8):
                    self.mm(bk[:, 0:n], win[:, k, col0:col0 + 128], u[:, k, 0:n], k == 0, k == 7, [Bwin, Bu], Bbk)
                return bk, Bbk
            for (base, pbase, nch, dst) in ((QO, QPO, 8, self.QT), (KO, KPO, 2, self.KT)):
                if last and g == 16 and dst is self.QT:
                    continue
                for c in range(nch):
                    bA, BA = proj_fm(base + c * 128)
                    bB, BB = proj_fm(pbase + c * 128)
                    r, Br = rt[nrt % 2]
                    nrt += 1
                    o, Bo = sb[nsb % 8]
                    nsb += 1
                    self.tt("dve", r[:, 0, 0:n], bA[:, 0:n], c_[:, 0, 0:n], ALU.mult, [BA, Bc_], (Br, 0))
                    self.tt("dve", r[:, 1, 0:n], bB[:, 0:n], c_[:, 1, 0:n], ALU.mult, [BB, Bc_], (Br, 1))
                    self.tt("pool", o[:, 0:n], r[:, 0, 0:n], r[:, 1, 0:n], ALU.add, [Br], Bo)
                    self.st("sp", dst[c * 128:(c + 1) * 128, t0:t0 + n], o[:, 0:n], Bo)
            for c in range(8):
                bk, Bbk = proj_fm(XRO + c * 128)
                o, Bo = sf[nsf % 6]
                nsf += 1
                self.act(o[:, 0:n], bk[:, 0:n], AF.Copy, [Bbk], Bo)
                self.st("sp", self.XRT[c * 128:(c + 1) * 128, t0:t0 + n], o[:, 0:n], Bo)
            if not (last and g == 16):
                for c in range(8):
                    bk, Bbk = proj_fm(GRO + c * 128)
                    o, Bo = sb[nsb % 8]
                    nsb += 1
                    self.act(o[:, 0:n], bk[:, 0:n], AF.Copy, [Bbk], Bo)
                    self.st("sp", self.GRT[c * 128:(c + 1) * 128, t0:t0 + n], o[:, 0:n], Bo)
            for s in range(n // 128):
                bk, Bbk = self.bank()
                for k in range(8):
                    self.mm(bk[:, 0:256], u[:, k, s * 128:(s + 1) * 128], win[:, k, VO:VO + 256], k == 0, k == 7,
                            [Bwin, Bu], Bbk)
                o, Bo = sb[nsb % 8]
                nsb += 1
                self.op("dve", lambda e, o=o, bk=bk: e.tensor_copy(out=o[:, 0:256], in_=bk[:, 0:256]), reads=[Bbk], writes=[Bo])
                self.st("sp", self.V[t0 + s * 128:t0 + (s + 1) * 128, :], o[:, 0:256], Bo)
                if last and g == 16:
                    continue
                o, Bo = sx[nsx % 2]
                nsx += 1
                for h in range(2):
                    bk, Bbk = self.bank()
                    for k in range(8):
                        self.mm(bk, u[:, k, s * 128:(s + 1) * 128], win[:, k, XFO + h * 512:XFO + (h + 1) * 512],
                                k == 0, k == 7, [Bwin, Bu], Bbk)
                    if h == 0:
                        self.act(o[:, 0:512], bk, AF.Copy, [Bbk], (Bo, 0))
                    else:
                        self.op("dve", lambda e, o=o, bk=bk: e.tensor_copy(out=o[:, 512:1024], in_=bk), reads=[Bbk], writes=[(Bo, 1)])
                self.st("sp", self.XF[t0 + s * 128:t0 + (s + 1) * 128, :], o, Bo)
        self.phase_end()

    def phase_rg(self, l, last):
        self.phase_begin()
        XA, BXA = self.alloc([128, NT], F32, "XA")
        xc, Bxc = self.alloc([128, NT], F32, "xc")
        xcb, Bxcb = self.alloc([128, NT], BF16, "xcb")
        Bc, BBc = self.alloc([128, NT], F32, "Bc")
        H, BH = self.alloc([128, NT], F32, "H")
        tmp = [self.alloc([128, 1024], F32, "rgt%d" % i) for i in range(5)]
        ge = [self.alloc([128, 1024], F32, "ge%d" % i) for i in range(3)]
        grt = [self.alloc([128, 1024], BF16, "grt%d" % i) for i in range(2)]
        ot = [self.alloc([128, 1024], BF16, "rot%d" % i) for i in range(2)]
        wbd = [self.alloc([128, 4, 128], BF16, "wbd%d" % i) for i in range(2)]
        cols, Bcols = self.alloc([128, 8, 11], F32, "rgcols")
        cvec, Bcvec = self.alloc([128, 8, 2], F32, "rgc")
        ct, Bct = self.alloc([128, 8, 2], F32, "rgct")
        self.ld("sp", cols, self.rgcols[l], Bcols)
        lam = cols[:, :, 9:11]
        self.act(cvec, lam, AF.Exp, [Bcols], Bcvec, scale=-1.0)
        self.ts("dve", ct, cvec, -1.0 / 3.0, 0.5, ALU.mult, ALU.add, [Bcvec], Bct)
        self.tt("dve", ct, ct, cvec, ALU.mult, [Bct, Bcvec], Bct)
        self.ts("dve", ct, ct, -1.0, 1.0, ALU.mult, ALU.add, [Bct], Bct)
        self.tt("dve", ct, ct, cvec, ALU.mult, [Bct, Bcvec], Bct)
        self.ts("dve", cvec, ct, -8.0, None, ALU.mult, None, [Bct], Bcvec)
        segs = ((0, L), (L, NT))
        groups = [(i * 1024, 1024) for i in range(8)] + [(L, CT)]

        def rev(ap2, a, b):
            base = ap2[:, a:b]
            pstep = base.ap[0][0]
            return bass.AP(base.tensor, base.offset + (b - a - 1), [[pstep, 128], [-1, b - a]])
        for c in range(8):
            w, Bw = wbd[c % 2]
            self.ld("pool", w, self.rg_wbd[l, c], Bw)
            self.ld("sp", XA, self.XRT[c * 128:(c + 1) * 128, :], BXA)
            cw = lambda i: cols[:, c, i:i + 1]
            for (a, b) in segs:
                self.ts("dve", xc[:, a:b], XA[:, a:b], cw(2), cw(4), ALU.mult, ALU.add, [BXA, Bcols], Bxc)
                for (tap, so, do) in ((1, 0, 1), (0, 0, 2), (3, 1, 0)):
                    nn = (b - a) - max(so, do)
                    wtap = cw(tap)
                    self.op("dve", lambda e, a=a, so=so, do=do, nn=nn, wtap=wtap: e.scalar_tensor_tensor(
                        out=xc[:, a + do:a + do + nn], in0=XA[:, a + so:a + so + nn], scalar=wtap,
                        in1=xc[:, a + do:a + do + nn], op0=ALU.mult, op1=ALU.add),
                        reads=[BXA, Bcols, Bxc], writes=[Bxc])
            self.act(xcb, xc, AF.Copy, [Bxc], Bxcb)
            for d in range(2):
                A, BA = XA, BXA
                for (g0, gn) in groups:
                    R, BR = tmp[0]
                    IG, BIG = tmp[1]
                    for h in range(gn // 512 if gn >= 512 else 1):
                        hn = min(512, gn)
                        o0 = g0 + h * 512
                        bk, Bbk = self.bank()
                        self.mm(bk[:, 0:hn], w[:, d, :], xcb[:, o0:o0 + hn], True, True, [Bw, Bxcb], Bbk)
                        self.act(R[:, h * 512:h * 512 + hn], bk[:, 0:hn], AF.Sigmoid, [Bbk, Bcols], (BR, h),
                                 bias=cols[:, c, 5 + d:6 + d])
                        bk, Bbk = self.bank()
                        self.mm(bk[:, 0:hn], w[:, 2 + d, :], xcb[:, o0:o0 + hn], True, True, [Bw, Bxcb], Bbk)
                        self.act(IG[:, h * 512:h * 512 + hn], bk[:, 0:hn], AF.Sigmoid, [Bbk, Bcols], (BIG, h),
                                 bias=cols[:, c, 7 + d:8 + d])
                    M, BM = tmp[2]
                    S, BS = tmp[3]
                    GX, BGX = tmp[4]
                    self.act(A[:, g0:g0 + gn], R[:, 0:gn], AF.Exp, [BR, Bcvec], BA, scale=cvec[:, c, d:d + 1])
                    self.tt("pool", M[:, 0:gn], A[:, g0:g0 + gn], A[:, g0:g0 + gn], ALU.mult, [BA], BM)
                    self.act(S[:, 0:gn], M[:, 0:gn], AF.Sqrt, [BM], BS, scale=-1.0, bias=1.0)
                    self.tt("pool", GX[:, 0:gn], IG[:, 0:gn], xc[:, g0:g0 + gn], ALU.mult, [BIG, Bxc], BGX)
                    self.tt("dve", Bc[:, g0:g0 + gn], S[:, 0:gn], GX[:, 0:gn], ALU.mult, [BS, BGX], BBc)
                dst, Bdst = (H, BH) if d == 0 else (Bc, BBc)
                if d == 0:
                    self.op("dve", lambda e: e.tensor_tensor_scan(out=H[:, L:NT], data0=A[:, L:NT], data1=Bc[:, L:NT],
                                                                 initial=0.0, op0=ALU.mult, op1=ALU.add),
                            reads=[BA, BBc], writes=[BH])
                    prev = H[:, NT - 1:NT]
                    for i in range(4):
                        a, b = i * 2048, (i + 1) * 2048
                        self.op("dve", lambda e, a=a, b=b, prev=prev: e.tensor_tensor_scan(
                            out=H[:, a:b], data0=A[:, a:b], data1=Bc[:, a:b], initial=prev, op0=ALU.mult, op1=ALU.add),
                            reads=[BA, BBc, BH], writes=[BH])
                        prev = H[:, b - 1:b]
                else:
                    self.op("dve", lambda e: e.tensor_tensor_scan(out=rev(Bc, L, NT), data0=rev(A, L, NT), data1=rev(Bc, L, NT),
                                                                 initial=0.0, op0=ALU.mult, op1=ALU.add),
                            reads=[BA, BBc], writes=[BBc])
                    prev = Bc[:, L:L + 1]
                    for i in range(3, -1, -1):
                        a, b = i * 2048, (i + 1) * 2048
                        self.op("dve", lambda e, a=a, b=b, prev=prev: e.tensor_tensor_scan(
                            out=rev(Bc, a, b), data0=rev(A, a, b), data1=rev(Bc, a, b), initial=prev,
                            op0=ALU.mult, op1=ALU.add), reads=[BA, BBc], writes=[BBc])
                        prev = Bc[:, a:a + 1]
            for gi, (g0, gn) in enumerate(groups):
                if last and g0 == L:
                    continue
                gr, Bgr = grt[gi % 2]
                o, Bo = ot[gi % 2]
                self.ld("sp", gr[:, 0:gn], self.GRT[c * 128:(c + 1) * 128, g0:g0 + gn], Bgr)
                t0_, B0 = ge[0]
                t1_, B1 = ge[1]
                t2_, B2 = ge[2]
                self.tt("pool", t0_[:, 0:gn], gr[:, 0:gn], gr[:, 0:gn], ALU.mult, [Bgr], B0)
                self.ts("dve", t0_[:, 0:gn], t0_[:, 0:gn], 0.044715, 1.0, ALU.mult, ALU.add, [B0], B0)
                self.tt("pool", t0_[:, 0:gn], t0_[:, 0:gn], gr[:, 0:gn], ALU.mult, [B0, Bgr], B0)
                self.act(t1_[:, 0:gn], t0_[:, 0:gn], AF.Sigmoid, [B0], B1, scale=2.0 * math.sqrt(2.0 / math.pi))
                self.tt("pool", t1_[:, 0:gn], t1_[:, 0:gn], gr[:, 0:gn], ALU.mult, [B1, Bgr], B1)
                self.tt("pool", t2_[:, 0:gn], H[:, g0:g0 + gn], Bc[:, g0:g0 + gn], ALU.add, [BH, BBc], B2)
                self.tt("dve", o[:, 0:gn], t1_[:, 0:gn], t2_[:, 0:gn], ALU.mult, [B1, B2], Bo)
                self.st("sp", self.RT[c * 128:(c + 1) * 128, g0:g0 + gn], o[:, 0:gn], Bo)
        self.phase_end()

    def phase_att(self, l, last):
        self.phase_begin()
        KTs, BKT = self.alloc([64, 4, NT], BF16, "KTs")
        Vs, BV = self.alloc([128, 66, 256], BF16, "Vs")
        tri, Btri = self.alloc([128, 2, 128], BF16, "tri")
        sk, Bsk = self.alloc([64, 16], F32, "sk")
        Qs = [self.alloc([64, 16, 512], BF16, "Qs%d" % i) for i in range(2)]
        PT = [self.alloc([128, 512], BF16, "PT%d" % i) for i in range(12)]
        OT = [self.alloc([64, 16, 128], BF16, "OT%d" % i) for i in range(2)]
        rc = [self.alloc([64, 512], F32, "rc%d" % i) for i in range(2)]
        self.ld("sp", KTs, self.KT.rearrange("(g d) t -> d g t", d=64), BKT)
        vsrc = self.V.rearrange("(b p) c -> p b c", p=128)
        for i in range(6):
            self.ld("sp", Vs[:, i * 11:(i + 1) * 11, :], vsrc[:, i * 11:(i + 1) * 11, :], BV)
        self.ld("pool", tri, self.tri, Btri)
        self.ld("sp", sk, self.sink64[l], Bsk)
        self.act(sk, sk, AF.Exp, [Bsk], Bsk)
        npt = 0
        nblk = 64 if last else 66
        for n in range(nblk):
            if n % 4 == 0:
                def q_loads(nn_):
                    qq, Bqq = Qs[(nn_ // 4) % 2]
                    nq = min(512, (nblk - nn_) * 128)
                    self.ld("sp", qq[:, :, 0:nq], self.QT[:, nn_ * 128:nn_ * 128 + nq].rearrange("(h d) t -> d h t", d=64), Bqq)
                if n == 0:
                    q_loads(0)
                if n + 4 < nblk:
                    q_loads(n + 4)
                q, Bq = Qs[(n // 4) % 2]
            qo = (n % 4) * 128
            if n < 64:
                kbs = [(kb, m) for (kb, m) in ((n - 1, 0), (n, None), (n + 1, 1)) if 0 <= kb < 64] + [(64, None), (65, None)]
            else:
                kbs = [(64, None), (65, None)]
            o, Bo = OT[n % 2]
            for g in range(4):
                pts = []
                for (kb, m) in kbs:
                    bk, Bbk = self.bank()
                    self.mm(bk.rearrange("p (h q) -> p h q", h=4), KTs[:, g, kb * 128:(kb + 1) * 128],
                            q[:, 4 * g:4 * g + 4, qo:qo + 128], True, True, [BKT, Bq], Bbk)
                    p, Bp = PT[npt % 12]
                    npt += 1
                    self.act(p, bk, AF.Exp, [Bbk], Bp, scale=0.125)
                    if m is not None:
                        p3 = p.rearrange("p (h q) -> p h q", h=4)
                        msk = tri[:, m:m + 1, :].to_broadcast([128, 4, 128])
                        self.tt("pool", p3, p3, msk, ALU.mult, [Bp, Btri], Bp)
                    pts.append((p, Bp, kb))
                bn, Bbn = self.bank()
                bd, Bbd = self.bank()
                for i, (p, Bp, kb) in enumerate(pts):
                    self.mm(bn[0:64, :], Vs[:, kb, g * 64:(g + 1) * 64], p, i == 0, i == len(pts) - 1, [BV, Bp], Bbn)
                for i, (p, Bp, kb) in enumerate(pts):
                    self.mm(bd[0:64, :], self.ones_b, p, i == 0, i == len(pts) - 1, [self.Bones_b, Bp], Bbd)
                r, Br = rc[g % 2]
                r3 = r.rearrange("p (h q) -> p h q", h=4)
                self.tt("dve", r3, bd[0:64, :].rearrange("p (h q) -> p h q", h=4),
                        sk[:, 4 * g:4 * g + 4].unsqueeze(2).to_broadcast([64, 4, 128]), ALU.add, [Bbd, Bsk], Br)
                self.op("dve", lambda e, r=r: e.reciprocal(out=r, in_=r), reads=[Br], writes=[Br])
                self.tt("dve", o[:, 4 * g:4 * g + 4, :], bn[0:64, :].rearrange("p (h q) -> p h q", h=4), r3, ALU.mult,
                        [Bbn, Br], (Bo, g))
            self.st("sp", self.AT[:, n * 128:(n + 1) * 128].rearrange("(h d) t -> d h t", d=64), o, Bo)
        self.phase_end()

    def phase_four(self, l, last):
        self.phase_begin()
        SC = 1.0 / math.sqrt(L * 256.0)
        SCC = 1.0 / math.sqrt(CT * 256.0)
        w128, Bw128 = self.alloc([128, 256], BF16, "w128")
        fab, Bfab = self.alloc([128, 128, 128], BF16, "fab")
        csm, Bcsm = self.alloc([128, 2, 2, 256], BF16, "csm")
        Xr = [self.alloc([128, 64, 128], BF16, "Xr%d" % i) for i in range(2)]
        Bst, BBst = self.alloc([128, 128, 128], BF16, "Bst")
        PQ = [self.alloc([128, 2, L], BF16, "PQ%d" % i) for i in range(2)]
        yo = [self.alloc([128, 512], BF16, "fyo%d" % i) for i in range(4)]
        self.ld("pool", w128, self.w128, Bw128)
        for i in range(4):
            self.ld("pool", fab[:, i * 32:(i + 1) * 32, :], self.fab[:, i * 32:(i + 1) * 32, :], Bfab)
        self.ld("pool", csm, self.cs256, Bcsm)
        nyo = 0
        FSTOP = int(os.environ.get("FOUR_STOP", "9"))

        def chan_stage(grp, pq, t0, n, src_off, scale):
            nonlocal nyo
            for cp in range(2):
                bk, Bbk = self.bank()
                i = 0
                for j in range(2):
                    for pqi in range(2):
                        p_, Bp_ = pq[j]
                        self.mm(bk[:, 0:n], csm[:, j, pqi, cp * 128:(cp + 1) * 128], p_[:, pqi, src_off:src_off + n],
                                i == 0, i == 3, [Bcsm, Bp_], Bbk)
                        i += 1
                o, Bo = yo[nyo % 4]
                nyo += 1
                self.act(o[:, 0:n], bk[:, 0:n], AF.Copy, [Bbk], Bo, scale=scale)
                row = (2 * grp + cp) * 128
                self.st("sp", self.FT[row:row + 128, t0:t0 + n], o[:, 0:n], Bo)
        for cc in range(8):
            xr, Bxr = Xr[cc % 2]
            pq, Bpq = PQ[cc % 2]
            xsrc = self.XF[0:L, cc * 128:(cc + 1) * 128].rearrange("(a b) c -> a b c", b=64)
            for i in range(8):
                self.ld("sp", xr[:, i * 8:(i + 1) * 8, :], xsrc[:, i * 8:(i + 1) * 8, :], Bxr)
            for cb in range(32):
                bk, Bbk = self.bank()
                for ci in range(4):
                    ch = cb * 4 + ci
                    lhsT = xr[:, :, ch]
                    self.mm(bk[0:64, ci * 128:(ci + 1) * 128], lhsT, w128[:, 0:128], True, True, [Bxr, Bw128], Bbk, signal=False)
                    self.mm(bk[64:128, ci * 128:(ci + 1) * 128], lhsT, w128[:, 128:256], True, True, [Bxr, Bw128], Bbk,
                            signal=(ci == 3))
                src = bk.rearrange("p (c k) -> p c k", c=4)
                dstv = Bst[:, :, cb * 4:cb * 4 + 4].rearrange("p k c -> p c k")
                if cb % 2 == 0:
                    self.op("act", lambda e, dstv=dstv, src=src: e.copy(out=dstv, in_=src), reads=[Bbk], writes=[(BBst, cb)])
                else:
                    self.op("dve", lambda e, dstv=dstv, src=src: e.tensor_copy(out=dstv, in_=src), reads=[Bbk], writes=[(BBst, cb)])
            for kg in range(8 if FSTOP >= 2 else 0):
                banks = [self.bank() for _ in range(4)]
                for ki in range(16):
                    k1 = kg * 16 + ki
                    bk, Bbk = banks[ki // 4]
                    self.mm(bk[:, (ki % 4) * 128:(ki % 4) * 128 + 128], Bst[:, k1, :], fab[:, k1, :], True, True,
                            [BBst, Bfab], Bbk, signal=(ki % 4 == 3))
                for bi in range(4 if os.environ.get("FOUR_NOEVAC") is None else 0):
                    bk, Bbk = banks[bi]
                    for pqi in range(2):
                        src = bk.rearrange("p (k r q) -> p k r q", k=4, r=2)[:, :, pqi, :]
                        k10 = kg * 16 + bi * 4
                        dbase = pq[:, pqi, :]
                        pstep = dbase.ap[0][0]
                        dstv = bass.AP(dbase.tensor, dbase.offset + k10, [[pstep, 128], [1, 4], [128, 64]])
                        if bi % 2 == 0:
                            self.op("act", lambda e, dstv=dstv, src=src: e.copy(out=dstv, in_=src), reads=[Bbk],
                                    writes=[(Bpq, (kg, bi, pqi))])
                        else:
                            self.op("dve", lambda e, dstv=dstv, src=src: e.tensor_copy(out=dstv, in_=src), reads=[Bbk],
                                    writes=[(Bpq, (kg, bi, pqi))])
            if cc % 2 == 1 and FSTOP >= 3:
                for g in range(16):
                    chan_stage(cc // 2, [PQ[0], PQ[1]], g * 512, 512, g * 512, SC)
        if not last and FSTOP >= 4:
            csn, Bcsn = self.alloc([128, 2, 512], BF16, "csn")
            self.ld("pool", csn, self.csn256, Bcsn)
            xct, Bxct = self.alloc([128, 2, D], BF16, "xct")
            self.ld("sp", xct, self.XF[L:NT, :].rearrange("(k p) c -> p k c", p=128), Bxct)
            for cc in range(8):
                pq, Bpq = PQ[cc % 2]
                bk, Bbk = self.bank()
                for k in range(2):
                    self.mm(bk, xct[:, k, cc * 128:(cc + 1) * 128], csn[:, k, :], k == 0, k == 1, [Bxct, Bcsn], Bbk)
                self.act(pq[:, :, 0:CT], bk.rearrange("p (r t) -> p r t", r=2), AF.Copy, [Bbk], Bpq)
                if cc % 2 == 1:
                    chan_stage(cc // 2, [PQ[0], PQ[1]], L, CT, 0, SCC)
        self.phase_end()

    def phase_merge(self, l, last):
        self.phase_begin()
        wo = []
        for i, src in enumerate((self.w_o_attn, self.w_o_rg, self.w_o_four, self.w_out)):
            w, Bw = self.alloc([128, 8, D], BF16, "wo%d" % i)
            for k in range(0, 8, 4):
                self.ld("pool", w[:, k:k + 4, :], src[l][k * 128:(k + 4) * 128, :].rearrange("(k p) n -> p k n", p=128), (Bw, k))
            wo.append((w, Bw))
        wmg, Bwmg = self.alloc([128, 8, 3 * D], BF16, "wmg")
        for k in range(8):
            self.ld("pool", wmg[:, k, :], self.w_merge[l][k * 128:(k + 1) * 128, :], (Bwmg, k))
        bmg, Bbmg = self.alloc([128, 24], F32, "bmg")
        self.ld("sp", bmg, self.b_merge_c[l], Bbmg)
        N = 256
        ins = [[self.alloc([128, 8, N], BF16, "mi%d_%d" % (i, j)) for j in range(2)] for i in range(4)]
        mg = [self.alloc([128, 8, N], BF16, "mg%d" % i) for i in range(2)]
        gt = [self.alloc([128, N], BF16, "gt%d" % i) for i in range(6)]
        pt = [self.alloc([128, N], F32, "pt%d" % i) for i in range(6)]
        yo = [self.alloc([128, 8, N], F32, "myo%d" % i) for i in range(2)]
        ntok = L if last else NT
        ngt = npt = 0
        for g in range(ntok // N):
            t0 = g * N
            def merge_loads(gg):
                for i, src in enumerate((self.UT, self.AT, self.RT, self.FT)):
                    t, Bt = ins[i][gg % 2]
                    self.ld("sp", t, src[:, gg * N:gg * N + N].rearrange("(c p) t -> p c t", p=128), Bt)
            if g == 0:
                merge_loads(0)
            if g + 1 < ntok // N:
                merge_loads(g + 1)
            tiles = [ins[i][g % 2] for i in range(4)]
            u, Bu = tiles[0]
            m, Bm = mg[g % 2]
            for j in range(8):
                terms = []
                for br in range(3):
                    bg, Bbg = self.bank()
                    col = br * D + j * 128
                    for k in range(8):
                        self.mm(bg[:, 0:N], wmg[:, k, col:col + 128], u[:, k, :], k == 0, k == 7, [Bwmg, Bu], Bbg)
                    gg, Bgg = gt[ngt % 6]
                    ngt += 1
                    self.act(gg, bg[:, 0:N], AF.Sigmoid, [Bbg, Bbmg], Bgg, bias=bmg[:, br * 8 + j:br * 8 + j + 1])
                    by, Bby = self.bank()
                    w, Bw = wo[br]
                    t, Bt = tiles[1 + br]
                    for k in range(8):
                        self.mm(by[:, 0:N], w[:, k, j * 128:(j + 1) * 128], t[:, k, :], k == 0, k == 7, [Bw, Bt], Bby)
                    p, Bp = pt[npt % 6]
                    npt += 1
                    self.tt("dve", p, by[:, 0:N], gg, ALU.mult, [Bby, Bgg], Bp)
                    terms.append((p, Bp))
                (p0, B0), (p1, B1), (p2, B2) = terms
                self.tt("pool", p0, p0, p1, ALU.add, [B0, B1], B0)
                self.tt("pool", m[:, j, :], p0, p2, ALU.add, [B0, B2], (Bm, j))
            o, Bo = yo[g % 2]
            w, Bw = wo[3]
            for j in range(8):
                by, Bby = self.bank()
                for k in range(8):
                    self.mm(by[:, 0:N], w[:, k, j * 128:(j + 1) * 128], m[:, k, :], k == 0, k == 7, [Bw, Bm], Bby)
                if j % 2 == 0:
                    self.act(o[:, j, :], by[:, 0:N], AF.Copy, [Bby], (Bo, j))
                else:
                    self.op("dve", lambda e, o=o, by=by, j=j: e.tensor_copy(out=o[:, j, :], in_=by[:, 0:N]), reads=[Bby], writes=[(Bo, j)])
            self.st("sp", self.YT[:, t0:t0 + N].rearrange("(c p) t -> p c t", p=128), o, Bo)
        self.phase_end()

    def phase_ln(self, l, last, first):
        self.phase_begin()
        src_x = self.XT if first else self.X1T
        dst_x = self.X1T if first else self.XT
        gate_w = 2 if first else 5
        gi, bi = (0, 1) if first else (2, 3)
        lnc, Blnc = self.alloc([128, 4, 8], F32, "lnc")
        self.ld("sp", lnc, self.lncols[l], Blnc)
        N = 512
        xs = [self.alloc([128, 8, N], F32, "lx%d" % i) for i in range(2)]
        ys = [self.alloc([128, 8, N], F32, "ly%d" % i) for i in range(2)]
        rs, Brs = self.alloc([128, 8, N], F32, "lr")
        sq, Bsq = self.alloc([128, 8, N], F32, "lsq")
        xo = [self.alloc([128, 8, N], F32, "lxo%d" % i) for i in range(2)]
        st_ = [self.alloc([128, N], F32, "lst%d" % i) for i in range(4)]
        final = last and not first
        if first:
            u2 = [self.alloc([128, 8, N], BF16, "lu%d" % i) for i in range(2)]
            u2f, Bu2f = self.alloc([128, 8, N], F32, "luf")
            wr, Bwr = self.alloc([128, 8, NE], F32, "wr")
            brb, Bbrb = self.alloc([128, NE], F32, "brb")
            self.ld("sp", wr, self.w_router[l].rearrange("(k p) e -> p k e", p=128), Bwr)
            self.ld("sp", brb, self.b_router_bc[l], Bbrb)
            lg = [self.alloc([128, NE], F32, "lg%d" % i) for i in range(2)]
            sm = [self.alloc([128, 16], F32, "sm%d" % i) for i in range(2)]
            gts = [self.alloc([NE, N], BF16, "gts%d" % i) for i in range(2)]
        if final:
            ot = [self.alloc([128, D], F32, "lot%d" % i) for i in range(2)]
        ntok = L if last else NT
        ntile = (ntok + N - 1) // N
        for g in range(ntile):
            t0 = g * N
            n = min(N, ntok - t0)
            v = 0 if t0 < L else 1
            x, Bx = xs[g % 2]
            y, By = ys[g % 2]

            def ln_loads(gg):
                tt0 = gg * N
                nn = min(N, ntok - tt0)
                xx, Bxx = xs[gg % 2]
                yy, Byy = ys[gg % 2]
                self.ld("sp", xx[:, :, 0:nn], src_x[:, tt0:tt0 + nn].rearrange("(c p) t -> p c t", p=128), Bxx)
                self.ld("sp", yy[:, :, 0:nn], self.YT[:, tt0:tt0 + nn].rearrange("(c p) t -> p c t", p=128), Byy)
            if g == 0:
                ln_loads(0)
            if g + 1 < ntile:
                ln_loads(g + 1)
            for c in range(8):
                self.act(y[:, c, 0:n], y[:, c, 0:n], AF.Copy, [By, self.Bmodc], (By, c), scale=self.mcol(gate_w, c, v))
                self.op("dve", lambda e, c=c, x=x, y=y, n=n: e.scalar_tensor_tensor(
                    out=rs[:, c, 0:n], in0=x[:, c, 0:n], scalar=ALPHA, in1=y[:, c, 0:n], op0=ALU.mult, op1=ALU.add),
                    reads=[Bx, (By, c)], writes=[(Brs, c)])
                self.act(sq[:, c, 0:n], rs[:, c, 0:n], AF.Square, [(Brs, c)], (Bsq, c))
            bm, Bbm = self.bank()
            bq, Bbq = self.bank()
            for c in range(8):
                self.mm(bm[:, 0:n], self.ones_s, rs[:, c, 0:n], c == 0, c == 7, [self.Bones_s, (Brs, c)], Bbm)
            for c in range(8):
                self.mm(bq[:, 0:n], self.ones_s, sq[:, c, 0:n], c == 0, c == 7, [self.Bones_s, (Bsq, c)], Bbq)
            (mean, Bmean), (m2, Bm2), (var, Bvar), (rstd, Brstd) = st_
            self.act(mean[:, 0:n], bm[:, 0:n], AF.Copy, [Bbm], Bmean)
            self.tt("dve", m2[:, 0:n], bm[:, 0:n], mean[:, 0:n], ALU.mult, [Bbm, Bmean], Bm2)
            self.tt("dve", var[:, 0:n], bq[:, 0:n], m2[:, 0:n], ALU.subtract, [Bbq, Bm2], Bvar)
            self.ts("dve", var[:, 0:n], var[:, 0:n], LN_EPS, None, ALU.add, None, [Bvar], Bvar)
            self.act(var[:, 0:n], var[:, 0:n], AF.Sqrt, [Bvar], Bvar)
            self.op("dve", lambda e, n=n: e.reciprocal(out=rstd[:, 0:n], in_=var[:, 0:n]), reads=[Bvar], writes=[Brstd])
            o, Bo = xo[g % 2]
            for c in range(8):
                self.tt("pool", rs[:, c, 0:n], rs[:, c, 0:n], mean[:, 0:n], ALU.subtract, [(Brs, c), Bmean], (Brs, c))
                self.tt("dve", rs[:, c, 0:n], rs[:, c, 0:n], rstd[:, 0:n], ALU.mult, [(Brs, c), Brstd], (Brs, c))
                self.act(o[:, c, 0:n], rs[:, c, 0:n], AF.Identity, [(Brs, c), Blnc], (Bo, c),
                         scale=lnc[:, gi, c:c + 1], bias=lnc[:, bi, c:c + 1])
            if not final:
                self.st("sp", dst_x[:, t0:t0 + n].rearrange("(c p) t -> p c t", p=128), o[:, :, 0:n], Bo)
            if first:
                u, Bu = u2[g % 2]
                for c in range(8):
                    self.act(u[:, c, 0:n], o[:, c, 0:n], AF.Identity, [(Bo, c), self.Bmodc], (Bu, c),
                             scale=self.mcol(4, c, v), bias=self.mcol(3, c, v))
                    self.act(u2f[:, c, 0:n], o[:, c, 0:n], AF.Identity, [(Bo, c), self.Bmodc], (Bu2f, c),
                             scale=self.mcol(4, c, v), bias=self.mcol(3, c, v))
                self.st("sp", self.U2T[:, t0:t0 + n].rearrange("(c p) t -> p c t", p=128), u[:, :, 0:n], Bu)
                gtile, Bgt = gts[g % 2]
                for s in range(n // 128):
                    bk, Bbk = self.bank()
                    for c in range(8):
                        self.mm(bk[:, 0:NE], u2f[:, c, s * 128:(s + 1) * 128], wr[:, c, :], c == 0, c == 7,
                                [(Bu2f, c), Bwr], Bbk)
                    lgt, Blg = lg[s % 2]
                    smt, Bsm = sm[s % 2]
                    self.tt("dve", lgt, bk[:, 0:NE], brb, ALU.add, [Bbk, Bbrb], Blg)
                    self.op("dve", lambda e, smt=smt, lgt=lgt: e.max(out=smt[:, 0:8], in_=lgt), reads=[Blg], writes=[Bsm])
                    self.ts("dve", smt[:, 8:9], smt[:, 0:1], -1.0, None, ALU.mult, None, [Bsm], Bsm)
                    bk2, Bbk2 = self.bank()
                    self.ts("dve", bk2[:, 0:NE], lgt, smt[:, 3:4], None, ALU.is_ge, None, [Blg, Bsm], Bbk2)
                    self.act(lgt, lgt, AF.Exp, [Blg, Bsm], Blg, bias=smt[:, 8:9])
                    self.tt("dve", lgt, lgt, bk2[:, 0:NE], ALU.mult, [Blg, Bbk2], Blg)
                    self.op("dve", lambda e, smt=smt, lgt=lgt: e.tensor_reduce(out=smt[:, 9:10], in_=lgt, axis=AX.X, op=ALU.add),
                            reads=[Blg], writes=[Bsm])
                    self.op("dve", lambda e, smt=smt: e.reciprocal(out=smt[:, 10:11], in_=smt[:, 9:10]), reads=[Bsm], writes=[Bsm])
                    self.ts("dve", lgt, lgt, smt[:, 10:11], None, ALU.mult, None, [Blg, Bsm], Blg)
                    bk3, Bbk3 = self.bank()
                    self.op("pe", lambda e, bk3=bk3, lgt=lgt: e.transpose(out=bk3[0:NE, 0:128], in_=lgt, identity=self.ident),
                            reads=[Blg, self.Bident], writes=[Bbk3])
                    self.act(gtile[:, s * 128:(s + 1) * 128], bk3[0:NE, 0:128], AF.Copy, [Bbk3], (Bgt, s))
                self.st("sp", self.GT[:, t0:t0 + n], gtile[:, 0:n], Bgt)
            if final:
                for s in range(n // 128):
                    ott, Bott = ot[s % 2]
                    for half in range(2):
                        bk, Bbk = self.bank()
                        for i in range(4):
                            c = half * 4 + i
                            self.op("pe", lambda e, bk=bk, o=o, i=i, c=c, s=s: e.transpose(
                                out=bk[:, i * 128:(i + 1) * 128], in_=o[:, c, s * 128:(s + 1) * 128], identity=self.ident),
                                reads=[(Bo, c), self.Bident], writes=[Bbk], signal=(i == 3))
                        if half == 0:
                            self.act(ott[:, 0:512], bk, AF.Copy, [Bbk], (Bott, 0))
                        else:
                            self.op("dve", lambda e, ott=ott, bk=bk: e.tensor_copy(out=ott[:, 512:1024], in_=bk), reads=[Bbk], writes=[(Bott, 1)])
                    self.st("sp", self.out[t0 + s * 128:t0 + (s + 1) * 128, :], ott, Bott)
        self.phase_end()

    def phase_moe(self, l, last):
        self.phase_begin()
        T = 2048
        bgu, Bbgu = self.alloc([128, NE, 16], F32, "bgu")
        bdn, Bbdn = self.alloc([NE, D], BF16, "bdn")
        sel, Bsel = self.alloc([NE, NE, 128], BF16, "sel")
        self.ld("sp", bgu, self.b_gu_c[l], Bbgu)
        bgu1, Bbgu1 = self.alloc([128, NE, 8], F32, "bgu1")
        self.ts("dve", bgu1, bgu[:, :, 8:16], 1.0, None, ALU.add, None, [Bbgu], Bbgu1)
        self.ld("pool", bdn, self.b_dn[l], Bbdn)
        self.ld("pool", sel, self.sel, Bsel)
        uT, BuT = self.alloc([128, 8, T], BF16, "mu")
        fT, BfT = self.alloc([128, 8, T], F32, "mf")
        aT, BaT = self.alloc([128, 8, T], BF16, "ma")
        gbc, Bgbc = self.alloc([128, T], BF16, "gbc")
        gT, BgT = self.alloc([NE, T], BF16, "mgT")
        wg = [self.alloc([128, 8, 256], BF16, "wg%d" % i) for i in range(4)]
        wd = [self.alloc([128, 8, 256], BF16, "wd%d" % i) for i in range(4)]
        tf = [self.alloc([128, 512], F32, "mt%d" % i) for i in range(9)]
        ntf = 0
        ntok = L if last else NT
        sts = [(t0, min(T, ntok - t0)) for t0 in range(0, ntok, T)]
        units = []
        for si in range(len(sts)):
            for e in range(NE):
                for c in range(8):
                    units.append(("g", si, e, c))
                for jj in range(4):
                    units.append(("d", si, e, jj))
        cnt = {"g": 0, "d": 0}
        slots = {}
        issued = 0

        def issue(upto):
            nonlocal issued
            while issued < min(upto, len(units)):
                kind, si, e, i = units[issued]
                if kind == "g":
                    w, Bw = wg[cnt["g"] % 4]
                    cnt["g"] += 1
                    self.ld("pool", w, self.w_gu[l, e, i].rearrange("(k p) n -> p k n", p=128), Bw)
                else:
                    w, Bw = wd[cnt["d"] % 4]
                    cnt["d"] += 1
                    self.ld("pool", w, self.w_dn[l, e, i].rearrange("(k p) n -> p k n", p=128), Bw)
                slots[issued] = (w, Bw)
                issued += 1
        ui = 0
        LOOK = 4
        for si, (s0, sn) in enumerate(sts):
            tiles = [(o, min(512, sn - o)) for o in range(0, sn, 512)]
            self.ld("sp", uT[:, :, 0:sn], self.U2T[:, s0:s0 + sn].rearrange("(c p) t -> p c t", p=128), BuT)
            self.ld("sp", gT[:, 0:sn], self.GT[:, s0:s0 + sn], BgT)
            for (o, n) in tiles:
                for j in range(8):
                    bk, Bbk = self.bank()
                    self.mm(bk[:, 0:n], bdn[:, j * 128:(j + 1) * 128], gT[:, o:o + n], True, True, [Bbdn, BgT], Bbk)
                    self.act(fT[:, j, o:o + n], bk[:, 0:n], AF.Copy, [Bbk], (BfT, (j, o)))
            for e in range(NE):
                for (o, n) in tiles:
                    bk, Bbk = self.bank()
                    self.mm(bk[:, 0:n], sel[:, e, :], gT[:, o:o + n], True, True, [Bsel, BgT], Bbk)
                    self.act(gbc[:, o:o + n], bk[:, 0:n], AF.Copy, [Bbk], (Bgbc, o), scale=1.0 / 1.702)
                for c in range(8):
                    issue(ui + LOOK)
                    w, Bw = slots.pop(ui)
                    ui += 1
                    for (o, n) in tiles:
                        bg, Bbg = self.bank()
                        bl, Bbl = self.bank()
                        for k in range(8):
                            self.mm(bg[:, 0:n], w[:, k, 0:128], uT[:, k, o:o + n], k == 0, k == 7, [Bw, BuT], Bbg)
                        for k in range(8):
                            self.mm(bl[:, 0:n], w[:, k, 128:256], uT[:, k, o:o + n], k == 0, k == 7, [Bw, BuT], Bbl)
                        (glu, Bglu), (sig, Bsig), (lin, Blin) = tf[ntf % 9], tf[(ntf + 1) % 9], tf[(ntf + 2) % 9]
                        ntf += 3
                        self.ts("dve", glu[:, 0:n], bg[:, 0:n], bgu[:, e, c:c + 1], 7.0, ALU.add, ALU.min, [Bbg, Bbgu], Bglu)
                        self.act(sig[:, 0:n], glu[:, 0:n], AF.Silu, [Bglu], Bsig, scale=1.702)
                        self.ts("dve", lin[:, 0:n], bl[:, 0:n], bgu1[:, e, c:c + 1], 8.0, ALU.add, ALU.min, [Bbl, Bbgu1], Blin)
                        self.op("dve", lambda e_, lin=lin, sig=sig, n=n: e_.scalar_tensor_tensor(
                            out=lin[:, 0:n], in0=lin[:, 0:n], scalar=-6.0, in1=sig[:, 0:n], op0=ALU.max, op1=ALU.mult),
                            reads=[Blin, Bsig], writes=[Blin])
                        self.tt("pool", aT[:, c, o:o + n], lin[:, 0:n], gbc[:, o:o + n], ALU.mult, [Blin, (Bgbc, o)], (BaT, (c, o)))
                for jj in range(4):
                    issue(ui + LOOK)
                    w, Bw = slots.pop(ui)
                    ui += 1
                    for jl in range(2):
                        j = jj * 2 + jl
                        for (o, n) in tiles:
                            bk, Bbk = self.bank()
                            for c in range(8):
                                self.mm(bk[:, 0:n], w[:, c, jl * 128:(jl + 1) * 128], aT[:, c, o:o + n], c == 0, c == 7,
                                        [Bw, (BaT, (c, o))], Bbk)
                            self.tt("dve", fT[:, j, o:o + n], bk[:, 0:n], fT[:, j, o:o + n], ALU.add,
                                    [Bbk, (BfT, (j, o))], (BfT, (j, o)))
            self.st("sp", self.YT[:, s0:s0 + sn].rearrange("(c p) t -> p c t", p=128), fT[:, :, 0:sn], BfT)
        self.phase_end()


def _cols(v, nchunk):
    return np.ascontiguousarray(np.asarray(v, np.float32).reshape(nchunk, 128).T)


def _consts():
    c = {}
    n_freq = 16
    freqs = (np.float32(10000.0) ** (-np.arange(n_freq, dtype=np.float32) / np.float32(n_freq))).astype(np.float32)
    pos = np.arange(L)
    row = (pos // 64).astype(np.float32)
    col = (pos % 64).astype(np.float32)
    ang_r = (row[:, None] * freqs[None, :]).astype(np.float32)
    ang_c = (col[:, None] * freqs[None, :]).astype(np.float32)
    cos = np.ones((64, NT), np.float32)
    sin = np.zeros((64, NT), np.float32)
    for d in range(64):
        ang = ang_r if d < 32 else ang_c
        j = d % 16
        sgn = -1.0 if (d % 32) < 16 else 1.0
        cos[d, :L] = np.cos(ang[:, j])
        sin[d, :L] = sgn * np.sin(ang[:, j])
    c["cos2"] = np.ascontiguousarray(np.concatenate([cos, cos], 0))
    c["sin2"] = np.ascontiguousarray(np.concatenate([sin, sin], 0))
    k = np.arange(128)
    tri = np.zeros((128, 2, 128), np.float32)
    tri[:, 0, :] = (k[:, None] >= k[None, :])
    tri[:, 1, :] = (k[:, None] <= k[None, :])
    c["tri"] = tri
    a = 2.0 * np.pi * np.outer(k, k) / 128.0
    c["w128"] = np.concatenate([np.cos(a), -np.sin(a)], 1).astype(np.float32)
    l2 = np.arange(64)
    k1 = np.arange(128)
    k2 = np.arange(64)
    kk = k1[:, None] + 128 * k2[None, :]
    th = 2.0 * np.pi * (l2[:, None, None] * kk[None, :, :] % L) / L
    fr, fi = np.cos(th), -np.sin(th)
    fab = np.zeros((128, 128, 128), np.float32)
    fab[0:64, :, 0:64] = fr
    fab[0:64, :, 64:128] = fi
    fab[64:128, :, 0:64] = -fi
    fab[64:128, :, 64:128] = fr
    c["fab"] = fab
    n = np.arange(256)
    a = 2.0 * np.pi * (np.outer(n, n) % 256) / 256.0
    C, S = np.cos(a), np.sin(a)
    cs = np.zeros((128, 2, 2, 256), np.float32)
    csn = np.zeros((128, 2, 512), np.float32)
    for kc in range(2):
        cs[:, kc, 0, :] = C[kc * 128:(kc + 1) * 128]
        cs[:, kc, 1, :] = S[kc * 128:(kc + 1) * 128]
        csn[:, kc, 0:256] = C[kc * 128:(kc + 1) * 128]
        csn[:, kc, 256:512] = -S[kc * 128:(kc + 1) * 128]
    c["cs256"] = cs
    c["csn256"] = csn
    sel = np.zeros((NE, NE, 128), np.float32)
    for e in range(NE):
        sel[e, e, :] = 1.0
    c["sel"] = sel
    return c


def _prep_shared(inp):
    f = lambda a: np.asarray(a, np.float32)
    sh = {}
    sh["w_mod"] = f(inp["w_mod"])
    sh["b_mod_c"] = np.stack([_cols(inp["b_mod"][l], 48) for l in range(2)])
    w_in = f(inp["w_in"])
    perm = np.array([(d + 16) if (d % 32) < 16 else (d - 16) for d in range(64)])
    qperm = np.concatenate([h * 64 + perm for h in range(16)])
    kperm = np.concatenate([h * 64 + perm for h in range(4)])
    q, k = w_in[:, :, 0:1024], w_in[:, :, 1024:1280]
    sh["w_in"] = np.ascontiguousarray(np.concatenate(
        [q, q[:, :, qperm], k, k[:, :, kperm], w_in[:, :, 1280:]], axis=2))
    sh["sink64"] = np.ascontiguousarray(np.broadcast_to(f(inp["attn_sink"])[:, None, :], (2, 64, 16)))
    for nm in ("w_o_attn", "w_o_rg", "w_o_four", "w_out", "w_merge"):
        sh[nm] = f(inp[nm])
    sh["b_merge_c"] = np.stack([_cols(inp["b_merge"][l], 24) for l in range(2)])
    rgc = np.zeros((2, 128, 8, 11), np.float32)
    for l in range(2):
        for t in range(4):
            rgc[l, :, :, t] = _cols(inp["conv_w"][l, t], 8)
        rgc[l, :, :, 4] = _cols(inp["conv_b"][l], 8)
        for d in range(2):
            rgc[l, :, :, 5 + d] = _cols(inp["rg_b_a"][l, d], 8)
            rgc[l, :, :, 7 + d] = _cols(inp["rg_b_i"][l, d], 8)
            rgc[l, :, :, 9 + d] = _cols(inp["rg_lambda"][l, d], 8)
    sh["rgcols"] = rgc
    wbd = np.zeros((2, 8, 128, 4, 128), np.float32)
    for l in range(2):
        for gi, nm in enumerate(("rg_w_a", "rg_w_i")):
            w = f(inp[nm])
            for d in range(2):
                for c in range(8):
                    for hb in range(2):
                        wbd[l, c, hb * 64:(hb + 1) * 64, gi * 2 + d, hb * 64:(hb + 1) * 64] = w[l, d, 2 * c + hb]
    sh["rg_wbd"] = wbd
    lnc = np.zeros((2, 128, 4, 8), np.float32)
    for l in range(2):
        for i, nm in enumerate(("ln1_g", "ln1_b", "ln2_g", "ln2_b")):
            lnc[l, :, i, :] = _cols(inp[nm][l], 8)
    sh["lncols"] = lnc
    sh["w_router"] = f(inp["w_router"])
    sh["b_router_bc"] = np.ascontiguousarray(np.broadcast_to(f(inp["b_router"])[:, None, :], (2, 128, NE)))
    wgu = f(inp["w_gate_up"]).reshape(2, NE, D, 2, 8, 128)
    sh["w_gu"] = np.ascontiguousarray(wgu.transpose(0, 1, 4, 2, 3, 5)).reshape(2, NE, 8, D, 256)
    bgu = f(inp["b_gate_up"]).reshape(2, NE, 16, 128)
    sh["b_gu_c"] = np.ascontiguousarray(bgu.transpose(0, 3, 1, 2))
    wdn = f(inp["w_down"]).reshape(2, NE, D, 4, 256)
    sh["w_dn"] = np.ascontiguousarray(wdn.transpose(0, 1, 3, 2, 4))
    sh["b_dn"] = f(inp["b_down"])
    sh.update(_consts())
    return sh


def _core_inputs(inp, b, sh):
    m = dict(sh)
    m["x"] = np.ascontiguousarray(np.asarray(inp["x"][b], np.float32))
    m["ctx"] = np.ascontiguousarray(np.asarray(inp["ctx"][b], np.float32))
    cv = np.stack([np.asarray(inp["c"][b], np.float32), np.asarray(inp["c_ctx"], np.float32)], 0)
    m["cvecT"] = np.ascontiguousarray(cv.reshape(2, 8, 128).transpose(2, 1, 0))
    return m


def kernel(**inputs):
    sh = _prep_shared(inputs)
    nc = K().build()
    in_maps = [_core_inputs(inputs, b, sh) for b in range(8)]
    res = run_bass_kernel_spmd(nc, in_maps, core_ids=list(range(8)))
    return np.stack([np.asarray(r["out"], np.float32) for r in res.results], 0)
```
